# Optimizing a Trainium2 kernel written in Bass

```python
import math
import jax, jax.numpy as jnp
from jax import lax
import numpy as np

D_MODEL = 1024
BATCH = 16
SEQ = 2048
DEPTH = 2

MLA_HEADS = 4
MLA_Q_RANK = 256
MLA_KV_RANK = 128
MLA_NOPE = 128
MLA_ROPE = 64
MLA_V = 128
ROPE_THETA = 10000.0
ATTN_BLOCK = 128
MASK_VALUE = -1e30
GDN_HEADS = 4
GDN_DK = 128
GDN_DV = 128
GDN_CONV = 4
GDN_CHUNK = 64
POOL_WINDOWS = (2, 4, 8, 16)
POOL_GROUP = 128
HGRN_HEADS = 4
HGRN_DK = 128
HGRN_DV = 128
HGRN_CHUNK = 32
N_BRANCH = 4
BRANCH_WIDTH = 512
D_FF = 2816
N_EXPERTS = 8
TOP_K = 2
D_FF_EXPERT = 3584
MOE_BLOCK = 512
N_DENSE = (DEPTH + 1) // 2
N_MOE = DEPTH // 2
DEEPNORM_ALPHA = (2 * DEPTH) ** 0.25
DEEPNORM_BETA = (8 * DEPTH) ** -0.25
LN_EPS = 1e-5
RMS_EPS = 1e-6

SPLIT_SIZES = (MLA_Q_RANK, MLA_KV_RANK, MLA_ROPE,
               GDN_HEADS * GDN_DK, GDN_HEADS * GDN_DK, GDN_HEADS * GDN_DV, GDN_HEADS * GDN_DV, GDN_HEADS, GDN_HEADS,
               len(POOL_WINDOWS) * POOL_GROUP,
               HGRN_HEADS * HGRN_DK, HGRN_HEADS * HGRN_DK, HGRN_HEADS * HGRN_DV, HGRN_HEADS * HGRN_DV,
               N_BRANCH * D_MODEL)
D_IN = sum(SPLIT_SIZES)

kernel_name = 'hybrid_gated_mla_gdn_pool_hgrn2_moe'


def layer_norm(x, g, b):
    xf = x.astype(jnp.float32)
    mu = jnp.mean(xf, -1, keepdims=True)
    var = jnp.mean(jnp.square(xf - mu), -1, keepdims=True)
    y = (xf - mu) * lax.rsqrt(var + LN_EPS) * g.astype(jnp.float32) + b.astype(jnp.float32)
    return y.astype(x.dtype)


def rms_norm(x, g):
    xf = x.astype(jnp.float32)
    y = xf * lax.rsqrt(jnp.mean(xf * xf, -1, keepdims=True) + RMS_EPS) * g.astype(jnp.float32)
    return y.astype(x.dtype)


def l2_norm(x):
    xf = x.astype(jnp.float32)
    return xf * lax.rsqrt(jnp.sum(xf * xf, -1, keepdims=True) + RMS_EPS)


def masked_exp(mask, diff):
    return jnp.where(mask, jnp.exp(jnp.where(mask, diff, 0.0)), 0.0)


def rope(x, pos):
    half = x.shape[-1] // 2
    inv = ROPE_THETA ** (-jnp.arange(half, dtype=jnp.float32) / half)
    ang = pos.astype(jnp.float32)[..., None] * inv
    ang = ang.reshape(ang.shape[:2] + (1,) * (x.ndim - 3) + (half,))
    cos, sin = jnp.cos(ang), jnp.sin(ang)
    xf = x.astype(jnp.float32)
    x1, x2 = xf[..., :half], xf[..., half:]
    return jnp.concatenate([x1 * cos - x2 * sin, x2 * cos + x1 * sin], -1).astype(x.dtype)


def swiglu(x, wg, wu, wd):
    return (jax.nn.silu(x @ wg) * (x @ wu)) @ wd


def mla_mixer(cq, ckv, kr, pos, g_q, w_uq, g_kv, w_ukv):
    B, S, _ = cq.shape
    H = MLA_HEADS
    q = (rms_norm(cq, g_q) @ w_uq).reshape(B, S, H, MLA_NOPE + MLA_ROPE)
    q = jnp.concatenate([q[..., :MLA_NOPE], rope(q[..., MLA_NOPE:], pos)], -1)
    kv = (rms_norm(ckv, g_kv) @ w_ukv).reshape(B, S, H, MLA_NOPE + MLA_V)
    k_nope, v = kv[..., :MLA_NOPE], kv[..., MLA_NOPE:]
    k_rope = rope(kr, pos)
    k = jnp.concatenate([k_nope, jnp.broadcast_to(k_rope[:, :, None, :], (B, S, H, MLA_ROPE))], -1)
    scale = (MLA_NOPE + MLA_ROPE) ** -0.5
    outs = []
    for start in range(0, S, ATTN_BLOCK):
        end = start + ATTN_BLOCK
        s = jnp.einsum('bqhd,bkhd->bhqk', q[:, start:end], k[:, :end]).astype(jnp.float32) * scale
        mask = (start + jnp.arange(ATTN_BLOCK))[:, None] >= jnp.arange(end)[None, :]
        p = jax.nn.softmax(jnp.where(mask, s, MASK_VALUE), axis=-1).astype(v.dtype)
        outs.append(jnp.einsum('bhqk,bkhd->bqhd', p, v[:, :end]))
    return jnp.concatenate(outs, 1).reshape(B, S, H * MLA_V)


def causal_dwconv(x, w):
    K, C = w.shape
    return lax.conv_general_dilated(x, w[:, None, :].astype(x.dtype), window_strides=(1,),
                                    padding=[(K - 1, 0)], dimension_numbers=('NWC', 'WIO', 'NWC'),
                                    feature_group_count=C)


def gated_delta_rule(q, k, v, g, beta):
    B, H, S, DK = q.shape
    DV = v.shape[-1]
    C = GDN_CHUNK
    N = S // C
    q = q * DK ** -0.5
    q, k = q.reshape(B, H, N, C, DK), k.reshape(B, H, N, C, DK)
    v = v.reshape(B, H, N, C, DV)
    g, beta = g.reshape(B, H, N, C), beta.reshape(B, H, N, C)
    kb, vb = k * beta[..., None], v * beta[..., None]
    gc = jnp.cumsum(g, -1)
    tri = jnp.tril(jnp.ones((C, C), bool))
    stri = jnp.tril(jnp.ones((C, C), bool), -1)
    decay = masked_exp(tri, gc[..., :, None] - gc[..., None, :])
    a = jnp.where(stri, jnp.einsum('bhnid,bhnjd->bhnij', kb, k) * decay, 0.0)
    eye = jnp.eye(C, dtype=jnp.float32)
    t_inv = lax.linalg.triangular_solve(eye + a, jnp.broadcast_to(eye, a.shape), left_side=True,
                                        lower=True, unit_diagonal=True)
    u = t_inv @ vb
    w = t_inv @ (kb * jnp.exp(gc)[..., None])
    qk = jnp.einsum('bhnid,bhnjd->bhnij', q, k) * decay

    def step(state, xs):
        q_c, k_c, u_c, w_c, qk_c, gc_c = xs
        v_new = u_c - w_c @ state
        o = (q_c * jnp.exp(gc_c)[..., None]) @ state + qk_c @ v_new
        g_last = gc_c[..., -1]
        state = state * jnp.exp(g_last)[..., None, None] + jnp.einsum(
            'bhcd,bhcv->bhdv', k_c * jnp.exp(g_last[..., None] - gc_c)[..., None], v_new)
        return state, o

    xs = tuple(jnp.moveaxis(t, 2, 0) for t in (q, k, u, w, qk, gc))
    _, o = lax.scan(step, jnp.zeros((B, H, DK, DV), jnp.float32), xs)
    return jnp.moveaxis(o, 0, 2).reshape(B, H, S, DV)


def gdn_mixer(q, k, v, z, a, b, conv_w, a_log, dt_bias, norm_g):
    B, S, _ = q.shape
    H = GDN_HEADS
    qkv = jax.nn.silu(causal_dwconv(jnp.concatenate([q, k, v], -1), conv_w))
    q, k, v = jnp.split(qkv, [H * GDN_DK, 2 * H * GDN_DK], axis=-1)
    qh = l2_norm(q.reshape(B, S, H, GDN_DK)).transpose(0, 2, 1, 3)
    kh = l2_norm(k.reshape(B, S, H, GDN_DK)).transpose(0, 2, 1, 3)
    vh = v.astype(jnp.float32).reshape(B, S, H, GDN_DV).transpose(0, 2, 1, 3)
    beta = jax.nn.sigmoid(b.astype(jnp.float32)).transpose(0, 2, 1)
    g = -jnp.exp(a_log.astype(jnp.float32)) * jax.nn.softplus(a.astype(jnp.float32) + dt_bias.astype(jnp.float32))
    o = gated_delta_rule(qh, kh, vh, g.transpose(0, 2, 1), beta).transpose(0, 2, 1, 3)
    o = rms_norm(o, norm_g) * jax.nn.silu(z.astype(jnp.float32).reshape(B, S, H, GDN_DV))
    return o.reshape(B, S, H * GDN_DV).astype(q.dtype)


def pool_mixer(u, w_group, scale):
    B, S, _ = u.shape
    ug = u.astype(jnp.float32).reshape(B, S, len(POOL_WINDOWS), POOL_GROUP)
    cs = jnp.cumsum(ug, axis=1)
    t = jnp.arange(S)
    outs = []
    for gi, win in enumerate(POOL_WINDOWS):
        c = cs[:, :, gi]
        lag = jnp.pad(c, ((0, 0), (win, 0), (0, 0)))[:, :S]
        cnt = jnp.minimum(t + 1, win).astype(jnp.float32)[None, :, None]
        outs.append((c - lag) / cnt - ug[:, :, gi])
    pooled = jnp.stack(outs, 2)
    y = jnp.einsum('bsgc,gcd->bsgd', pooled, w_group.astype(jnp.float32))
    return (y.reshape(B, S, -1) * scale.astype(jnp.float32)).astype(u.dtype)


def gla_chunk_scan(q, k, v, log_f):
    B, H, S, DK = q.shape
    DV = v.shape[-1]
    C = HGRN_CHUNK
    N = S // C
    b = jnp.cumsum(log_f.reshape(B, H, N, C, DK), axis=3)
    tri = jnp.tril(jnp.ones((C, C), bool))[:, :, None]

    def step(state, xs):
        q_c, k_c, v_c, b_c = xs
        dec = masked_exp(tri, b_c[:, :, :, None, :] - b_c[:, :, None, :, :])
        att = jnp.einsum('bhid,bhjd,bhijd->bhij', q_c, k_c, dec)
        o = att @ v_c + jnp.einsum('bhid,bhdv->bhiv', q_c * jnp.exp(b_c), state)
        b_last = b_c[:, :, -1]
        state = state * jnp.exp(b_last)[..., None] + jnp.einsum(
            'bhjd,bhjv->bhdv', k_c * jnp.exp(b_last[:, :, None, :] - b_c), v_c)
        return state, o

    rs = lambda t: jnp.moveaxis(t.reshape(B, H, N, C, t.shape[-1]), 2, 0)
    xs = (rs(q), rs(k), rs(v), jnp.moveaxis(b, 2, 0))
    _, o = lax.scan(step, jnp.zeros((B, H, DK, DV), jnp.float32), xs)
    return jnp.moveaxis(o, 0, 2).reshape(B, H, S, DV)


def hgrn2_mixer(q, f_pre, i, g, lb, norm_g):
    B, S, _ = q.shape
    H = HGRN_HEADS
    heads = lambda t, d: t.astype(jnp.float32).reshape(B, S, H, d).transpose(0, 2, 1, 3)
    qh = heads(jax.nn.silu(q), HGRN_DK)
    fp = heads(f_pre, HGRN_DK)
    lbh = lb.astype(jnp.float32).reshape(H, 1, HGRN_DK)
    log_f = jnp.log(lbh + (1.0 - lbh) * jax.nn.sigmoid(fp))
    kh = (1.0 - lbh) * jax.nn.sigmoid(-fp)
    vh = heads(i, HGRN_DV)
    o = gla_chunk_scan(qh, kh, vh, log_f).transpose(0, 2, 1, 3)
    o = rms_norm(o, norm_g) * jax.nn.sigmoid(g.astype(jnp.float32).reshape(B, S, H, HGRN_DV))
    return o.reshape(B, S, H * HGRN_DV).astype(q.dtype)


def moe_swiglu(x, w_router, w_gate, w_up, w_down):
    B, S, D = x.shape
    T = B * S
    xt = x.reshape(T, D)
    logits = (xt @ w_router).astype(jnp.float32)
    top_val, top_idx = lax.top_k(logits, TOP_K)
    gate = jax.nn.softmax(top_val, axis=-1)
    e_flat = top_idx.reshape(-1)
    tok_flat = jnp.repeat(jnp.arange(T, dtype=jnp.int32), TOP_K)
    w_flat = gate.reshape(-1)
    order = jnp.argsort(e_flat)
    e_s, tok_s, w_s = e_flat[order], tok_flat[order], w_flat[order]
    counts = jnp.zeros((N_EXPERTS,), jnp.int32).at[e_flat].add(1)
    starts = jnp.cumsum(counts) - counts
    padded = (counts + MOE_BLOCK - 1) // MOE_BLOCK * MOE_BLOCK
    pends = jnp.cumsum(padded)
    pstarts = pends - padded
    dest = pstarts[e_s] + (jnp.arange(T * TOP_K, dtype=jnp.int32) - starts[e_s])
    n_rows = ((T * TOP_K + MOE_BLOCK - 1) // MOE_BLOCK + N_EXPERTS) * MOE_BLOCK
    row_tok = jnp.full((n_rows,), T, jnp.int32).at[dest].set(tok_s)
    row_w = jnp.zeros((n_rows,), jnp.float32).at[dest].set(w_s)
    n_blk = n_rows // MOE_BLOCK
    blk_exp = jnp.minimum(jnp.searchsorted(pends, jnp.arange(n_blk, dtype=jnp.int32) * MOE_BLOCK, side='right'),
                          N_EXPERTS - 1)
    x_pad = jnp.concatenate([xt, jnp.zeros((1, D), xt.dtype)], 0)
    xb = x_pad[row_tok].reshape(n_blk, MOE_BLOCK, D)
    yb = lax.map(lambda a: swiglu(a[0], w_gate[a[1]], w_up[a[1]], w_down[a[1]]), (xb, blk_exp))
    yb = yb.reshape(n_rows, D) * row_w[:, None].astype(yb.dtype)
    y = jax.ops.segment_sum(yb, row_tok, num_segments=T + 1)[:T]
    return y.reshape(B, S, D)


def setup_inputs(seed: int = 0) -> dict:
    key = jax.random.key(seed)
    ks = iter(list(jax.random.split(key, 40)))
    f32 = jnp.float32
    L = DEPTH

    def nrm(shape, scale):
        return jax.random.normal(next(ks), shape, f32) * scale

    def gain(shape):
        return 1.0 + 0.02 * jax.random.normal(next(ks), shape, f32)

    x = jax.random.normal(next(ks), (BATCH, SEQ, D_MODEL), f32)
    positions = jnp.broadcast_to(jnp.arange(SEQ, dtype=jnp.int32), (BATCH, SEQ))
    dt = jnp.exp(jax.random.uniform(next(ks), (L, GDN_HEADS), f32, math.log(1e-3), math.log(1e-1)))
    return {
        'x': x,
        'positions': positions,
        'w_in': nrm((L, D_MODEL, D_IN), D_MODEL ** -0.5),
        'mla_q_norm': gain((L, MLA_Q_RANK)),
        'mla_w_uq': nrm((L, MLA_Q_RANK, MLA_HEADS * (MLA_NOPE + MLA_ROPE)), MLA_Q_RANK ** -0.5),
        'mla_kv_norm': gain((L, MLA_KV_RANK)),
        'mla_w_ukv': nrm((L, MLA_KV_RANK, MLA_HEADS * (MLA_NOPE + MLA_V)), MLA_KV_RANK ** -0.5),
        'gdn_conv': nrm((L, GDN_CONV, GDN_HEADS * (2 * GDN_DK + GDN_DV)), GDN_CONV ** -0.5),
        'gdn_a_log': jnp.log(jax.random.uniform(next(ks), (L, GDN_HEADS), f32, 1.0, 16.0)),
        'gdn_dt_bias': dt + jnp.log(-jnp.expm1(-dt)),
        'gdn_norm': gain((L, GDN_DV)),
        'pool_w': nrm((L, len(POOL_WINDOWS), POOL_GROUP, POOL_GROUP), POOL_GROUP ** -0.5),
        'pool_scale': gain((L, len(POOL_WINDOWS) * POOL_GROUP)),
        'hgrn_lb_logits': nrm((L, HGRN_HEADS * HGRN_DK), 0.5),
        'hgrn_norm': gain((L, HGRN_DV)),
        'w_branch': nrm((L, N_BRANCH, BRANCH_WIDTH, D_MODEL), BRANCH_WIDTH ** -0.5 * DEEPNORM_BETA),
        'w_out': nrm((L, D_MODEL, D_MODEL), D_MODEL ** -0.5 * DEEPNORM_BETA),
        'ln_mix_g': gain((L, D_MODEL)),
        'ln_mix_b': nrm((L, D_MODEL), 0.02),
        'ffn_w_gate': nrm((N_DENSE, D_MODEL, D_FF), D_MODEL ** -0.5),
        'ffn_w_up': nrm((N_DENSE, D_MODEL, D_FF), D_MODEL ** -0.5),
        'ffn_w_down': nrm((N_DENSE, D_FF, D_MODEL), D_FF ** -0.5 * DEEPNORM_BETA),
        'moe_router': nrm((N_MOE, D_MODEL, N_EXPERTS), D_MODEL ** -0.5),
        'moe_w_gate': nrm((N_MOE, N_EXPERTS, D_MODEL, D_FF_EXPERT), D_MODEL ** -0.5),
        'moe_w_up': nrm((N_MOE, N_EXPERTS, D_MODEL, D_FF_EXPERT), D_MODEL ** -0.5),
        'moe_w_down': nrm((N_MOE, N_EXPERTS, D_FF_EXPERT, D_MODEL), D_FF_EXPERT ** -0.5 * DEEPNORM_BETA),
        'ln_ffn_g': gain((L, D_MODEL)),
        'ln_ffn_b': nrm((L, D_MODEL), 0.02),
    }


def reference(x, positions, w_in, mla_q_norm, mla_w_uq, mla_kv_norm, mla_w_ukv, gdn_conv, gdn_a_log,
              gdn_dt_bias, gdn_norm, pool_w, pool_scale, hgrn_lb_logits, hgrn_norm, w_branch, w_out,
              ln_mix_g, ln_mix_b, ffn_w_gate, ffn_w_up, ffn_w_down, moe_router, moe_w_gate, moe_w_up,
              moe_w_down, ln_ffn_g, ln_ffn_b):
    B, S, D = x.shape
    cuts = []
    acc = 0
    for size in SPLIT_SIZES[:-1]:
        acc += size
        cuts.append(acc)
    p_lb = jax.nn.softmax(hgrn_lb_logits.astype(jnp.float32), axis=0)
    lower_bounds = jnp.cumsum(p_lb, axis=0) - p_lb[0]
    for l in range(DEPTH):
        proj = x @ w_in[l]
        (cq, ckv, kr, gq, gk, gv, gz, ga, gb, pu, hq, hf, hi, hg, gate_pre) = jnp.split(proj, cuts, axis=-1)
        y_a = mla_mixer(cq, ckv, kr, positions, mla_q_norm[l], mla_w_uq[l], mla_kv_norm[l], mla_w_ukv[l])
        y_b = gdn_mixer(gq, gk, gv, gz, ga, gb, gdn_conv[l], gdn_a_log[l], gdn_dt_bias[l], gdn_norm[l])
        y_c = pool_mixer(pu, pool_w[l], pool_scale[l])
        y_d = hgrn2_mixer(hq, hf, hi, hg, lower_bounds[l], hgrn_norm[l])
        gates = jax.nn.sigmoid(gate_pre.reshape(B, S, N_BRANCH, D))
        merged = None
        for m, y in enumerate((y_a, y_b, y_c, y_d)):
            term = gates[:, :, m] * (y @ w_branch[l, m])
            merged = term if merged is None else merged + term
        x = layer_norm(DEEPNORM_ALPHA * x + merged @ w_out[l], ln_mix_g[l], ln_mix_b[l])
        j = l // 2
        if l % 2 == 0:
            ffn = swiglu(x, ffn_w_gate[j], ffn_w_up[j], ffn_w_down[j])
        else:
            ffn = moe_swiglu(x, moe_router[j], moe_w_gate[j], moe_w_up[j], moe_w_down[j])
        x = layer_norm(DEEPNORM_ALPHA * x + ffn, ln_ffn_g[l], ln_ffn_b[l])
    return x
```

```python
import math
from contextlib import ExitStack

import numpy as np
import concourse.bass as bass
import concourse.mybir as mybir
from concourse.bass_utils import run_bass_kernel_spmd

F32 = mybir.dt.float32
BF16 = mybir.dt.bfloat16
I32 = mybir.dt.int32
ALU = mybir.AluOpType
AF = mybir.ActivationFunctionType
AX = mybir.AxisListType

D = 1024
S = 2048
NSEQ = 2
DEPTH = 2
NT = S // 128
D_IN = 9160
OFF = dict(cq=0, ckv=256, kr=384, gq=448, gk=960, gv=1472, gz=1984, ga=2496, gb=2500, pu=2504,
           hq=3016, hf=3528, hi=4040, hg=4552, gate=5064)
D_FF = 2816
D_FFE = 3584
NEXP = 8
ALPHA = (2 * DEPTH) ** 0.25
LN_EPS = 1e-5
RMS_EPS = 1e-6
ATT_SCALE = (128 + 64) ** -0.5

ENGS = ("pe", "dve", "act", "pool", "sp")
DBG = {}
MARKS = []
SELF_NOSYNC = ()
EPOCH = 5000
NDMASEM = 8


class Res:
    __slots__ = ("name", "w", "r")

    def __init__(self, name):
        self.name = name
        self.w = None
        self.r = {}


class Tile:
    def __init__(self, t, name):
        self.t = t
        self.name = name
        self.res = Res(name)

    def __getitem__(self, idx):
        return self.t[idx]


class FW:
    def __init__(self, nc, stack):
        self.nc = nc
        self.stack = stack
        self.cnt = {e: 0 for e in ENGS}
        self.known = {e: {} for e in ENGS}
        self.semh = {}
        self.dma_rr = {e: 0 for e in ENGS}
        self.dma_use = {}
        self.nsem = 0
        self.last = {}
        self.eobj = {"pe": nc.tensor, "dve": nc.vector, "act": nc.scalar, "pool": nc.gpsimd, "sp": nc.sync}
        self.ninst = 0
        self.per = {}
        self.pending_inc = {}
        self.pe_mode = None
        self.pe_safe = False

    def sbuf(self, name, shape, dtype, stack=None):
        self.nuniq = getattr(self, "nuniq", 0) + 1
        name = "%s_%d" % (name, self.nuniq)
        t = (stack or self.stack).enter_context(self.nc.sbuf_tensor(name, list(shape), dtype))
        return Tile(t, name)

    def psum(self, name, shape, dtype=F32):
        t = self.stack.enter_context(self.nc.psum_tensor(name, list(shape), dtype))
        return Tile(t, name)

    def dram(self, name, shape, dtype, kind=None):
        if kind is None:
            t = self.nc.dram_tensor(name, list(shape), dtype)
        else:
            t = self.nc.dram_tensor(name, list(shape), dtype, kind=kind)
        return Tile(t, name)

    def _sem(self, key):
        h = self.semh.get(key)
        if h is None:
            h = self.stack.enter_context(self.nc.semaphore("s%d" % self.nsem))
            self.nsem += 1
            self.semh[key] = h
        return h

    def _need(self, eng, ev):
        if ev is None:
            return
        key, val = ev
        if key[0] == "c" and key[1] == eng and eng in SELF_NOSYNC:
            return
        if eng == "pe" and key[0] == "c" and key[1] == "pe" and not self.pe_safe:
            return
        if self.known[eng].get(key, 0) >= val:
            return
        self.known[eng][key] = val
        self.eobj[eng].wait_ge(self._sem(key), val)
        self.ninst += 1
        self.per[eng] = self.per.get(eng, 0) + 1

    def emit(self, eng, fn, reads=(), writes=(), dma=False, inc=True, pe_mode=None):
        rr = [x.res if isinstance(x, Tile) else x for x in reads]
        ww = [x.res if isinstance(x, Tile) else x for x in writes]
        if eng == "pe" and self.pe_safe:
            inc = True
        if pe_mode is not None:
            if False and pe_mode != self.pe_mode and self.cnt["pe"] > 0:
                assert not self.pending_inc.get("pe"), "tiling-mode switch inside an open PE group"
                c = self.cnt["pe"] - 1
                key = ("c", "pe", c // EPOCH)
                self.eobj["pe"].wait_ge(self._sem(key), c % EPOCH + 1)
                self.ninst += 1
                self.ndrain = getattr(self, "ndrain", 0) + 1
            self.pe_mode = pe_mode
        for r in rr:
            self._need(eng, r.w)
        for w in ww:
            self._need(eng, w.w)
            for k, v in list(w.r.items()):
                self._need(eng, (k, v))
        if dma:
            i = self.dma_rr[eng]
            self.dma_rr[eng] = (i + 1) % NDMASEM
            key = ("d", eng, i)
            used = self.dma_use.get(key, 0)
            if used:
                self._need(eng, (key, 16 * used))
            self.dma_use[key] = used + 1
            ev = (key, 16 * (used + 1))
            inc = 16
        else:
            c = self.cnt[eng]
            key = ("c", eng, c // EPOCH)
            ev = (key, c % EPOCH + 1)
            if inc:
                self.cnt[eng] = c + 1
            self.pending_inc[eng] = not inc
        if dma or inc:
            fn(self.eobj[eng]).then_inc(self._sem(key), 16 if dma else 1)
        else:
            fn(self.eobj[eng])
        self.ninst += 1
        self.per[eng] = self.per.get(eng, 0) + 1
        self.last[key] = ev
        for r in rr:
            if r.r.get(key, 0) < ev[1]:
                r.r[key] = ev[1]
        for w in ww:
            w.w = ev
            w.r = {}
        return ev

    def barrier(self):
        assert not self.pending_inc.get("pe"), "PE group left open at a barrier"
        evs = list(self.last.values())
        for ev in evs:
            self._need("sp", ev)
        ev = self.emit("sp", lambda e: e.nop())
        for eng in ENGS:
            if eng != "sp":
                self._need(eng, ev)

    def finish(self):
        for ev in list(self.last.values()):
            self._need("sp", ev)


def make_consts():
    c = {}
    c["ident"] = np.eye(128, dtype=np.float32)
    i = np.arange(128)
    c["m_causal"] = (i[None, :] >= i[:, None]).astype(np.float32)
    blk64 = (i[:, None] // 64) == (i[None, :] // 64)
    blk32 = (i[:, None] // 32) == (i[None, :] // 32)
    c["m_low64"] = (blk64 & (i[None, :] <= i[:, None])).astype(np.float32)
    c["m_slow64"] = (blk64 & (i[None, :] < i[:, None])).astype(np.float32)
    c["m_up64"] = (blk64 & (i[None, :] >= i[:, None])).astype(np.float32)
    c["m_up32"] = (blk32 & (i[None, :] >= i[:, None])).astype(np.float32)
    c["m_chunk32"] = ((i[:, None] // 32) == np.arange(4)[None, :]).astype(np.float32)
    c["m_chunk64"] = ((i[:, None] // 64) == np.arange(2)[None, :]).astype(np.float32)
    t = np.arange(S)
    c["scan32"] = np.broadcast_to((t % 32 != 0).astype(np.float32)[None, :], (128, S)).copy()
    c["scan64"] = np.broadcast_to((t % 64 != 0).astype(np.float32)[None, :], (128, S)).copy()
    half = 32
    inv = (10000.0 ** (-np.arange(half, dtype=np.float32) / half)).astype(np.float32)
    rp = np.zeros((128, 4), np.float32)
    rp[:64, 0] = np.concatenate([inv, inv])
    rp[:64, 1] = np.concatenate([-np.ones(32), np.ones(32)])
    c["ropep"] = rp
    ic = np.zeros((128, 4, 16), np.float32)
    for gi, win in enumerate((2, 4, 8, 16)):
        ic[:, gi, :] = 1.0 / np.minimum(np.arange(16) + 1, win)
    c["invcnt"] = ic.reshape(128, 64)
    sel = np.zeros((128, 4, 128), np.float32)
    for h in range(4):
        sel[h, h, :] = 1.0
    c["sel4"] = sel.reshape(128, 512)
    c["ones"] = np.ones((128, 128), np.float32)
    selx = np.zeros((128, 4, 128), np.float32)
    for base in (0, 32, 64):
        for h in range(4):
            selx[base + h, h, :] = 1.0
    c["sel4x"] = selx.reshape(128, 512)
    c["m_sup64"] = (blk64 & (i[None, :] > i[:, None])).astype(np.float32)
    return c


CONST_SHAPES = {k: v.shape for k, v in make_consts().items()}


class View:
    __slots__ = ("tile", "ap")

    def __init__(self, tile, ap):
        self.tile = tile
        self.ap = ap

    def __getitem__(self, idx):
        return View(self.tile, self.ap[idx])

    def bitcast(self, dt):
        return View(self.tile, self.ap.bitcast(dt))

    def rearrange(self, s, **kw):
        return View(self.tile, self.ap.rearrange(s, **kw))

    def bcast(self, shape):
        return View(self.tile, self.ap.broadcast_to(list(shape)))


def V(tile):
    return View(tile, tile.t.ap() if hasattr(tile.t, "ap") and callable(tile.t.ap) else tile.t[:])


def _ap(x):
    return x.ap if isinstance(x, View) else x


def _tiles(*xs):
    return [x.tile for x in xs if isinstance(x, View)]


def _r32(n):
    return 32 if n <= 32 else (64 if n <= 64 else 128)


def _pe_mode(ap, is_tr):
    shp = list(ap.shape)
    k = shp[0]
    m = 1
    for d in shp[1:]:
        m *= d
    return (_r32(k), _r32(m), str(ap.dtype) == str(F32), is_tr and str(ap.dtype) == str(F32))


class Ops:
    def __init__(self, fw):
        self.fw = fw

    def mm(self, out, lhsT, rhs, start=True, stop=True, inc=None):
        if inc is None:
            inc = stop
        return self.fw.emit("pe", lambda e: e.matmul(out.ap, lhsT.ap, rhs.ap, start=start, stop=stop),
                            reads=_tiles(lhsT, rhs), writes=_tiles(out), inc=inc, pe_mode=_pe_mode(lhsT.ap, False))

    def tr(self, out, in_, ident, inc=True):
        return self.fw.emit("pe", lambda e: e.transpose(out.ap, in_.ap, ident.ap),
                            reads=_tiles(in_, ident), writes=_tiles(out), inc=inc, pe_mode=_pe_mode(in_.ap, True))

    def act(self, out, in_, func, bias=None, scale=None, accum=None):
        kw = {}
        if bias is not None:
            kw["bias"] = _ap(bias)
        if scale is not None:
            kw["scale"] = _ap(scale)
        if accum is not None:
            kw["accum_out"] = accum.ap
        return self.fw.emit("act", lambda e: e.activation(out.ap, in_.ap, func, **kw),
                            reads=_tiles(in_, bias, scale), writes=_tiles(out, accum))

    def ts(self, out, in0, s1, s2, op0, op1=None, eng="dve", accum=None):
        kw = {}
        if op1 is not None:
            kw["op1"] = op1
        if accum is not None:
            kw["accum_out"] = accum.ap
        return self.fw.emit(eng, lambda e: e.tensor_scalar(out.ap, in0.ap, _ap(s1), _ap(s2), op0, **kw),
                            reads=_tiles(in0, s1, s2), writes=_tiles(out, accum))

    def tt(self, out, a, b, op, eng="dve"):
        return self.fw.emit(eng, lambda e: e.tensor_tensor(out.ap, a.ap, b.ap, op),
                            reads=_tiles(a, b), writes=_tiles(out))

    def stt(self, out, in0, scalar, in1, op0, op1, accum=None):
        kw = {}
        if accum is not None:
            kw["accum_out"] = accum.ap
        return self.fw.emit("dve", lambda e: e.scalar_tensor_tensor(out.ap, in0.ap, _ap(scalar), in1.ap, op0, op1, **kw),
                            reads=_tiles(in0, scalar, in1), writes=_tiles(out, accum))

    def copy(self, out, in_, eng="dve"):
        if eng == "act":
            return self.act(out, in_, AF.Copy)
        return self.fw.emit(eng, lambda e: e.tensor_copy(out.ap, in_.ap), reads=_tiles(in_), writes=_tiles(out))

    def recip(self, out, in_):
        return self.fw.emit("dve", lambda e: e.reciprocal(out.ap, in_.ap), reads=_tiles(in_), writes=_tiles(out))

    def scan(self, out, d0, d1, init, op0, op1):
        return self.fw.emit("dve", lambda e: e.tensor_tensor_scan(out.ap, d0.ap, d1.ap, _ap(init), op0, op1),
                            reads=_tiles(d0, d1, init), writes=_tiles(out))

    def memset(self, out, val, eng="dve"):
        return self.fw.emit(eng, lambda e: e.memset(out.ap, val), writes=_tiles(out))

    def dma(self, out, in_, eng="sp", slow=False):
        kw = {"allow_slow_non_contiguous": True} if slow else {}
        return self.fw.emit(eng, lambda e: e.dma_start(out=out.ap, in_=in_.ap, **kw),
                            reads=_tiles(in_), writes=_tiles(out), dma=True)

    def wrap(self, out, in_, shift):
        return self.fw.emit("dve", lambda e: e.add_range_wrap(out.ap, in_.ap, shift, math.pi, 2 * math.pi),
                            reads=_tiles(in_), writes=_tiles(out))


def V(tile):
    return View(tile, tile.t[:])


WEIGHT_SPECS = [
    ("w_in", (2, 1024, 9160)), ("mla_q_norm", (2, 256)), ("mla_w_uq", (2, 256, 768)),
    ("mla_kv_norm", (2, 128)), ("mla_w_ukv", (2, 128, 1024)), ("gdn_conv", (2, 4, 1536)),
    ("gdn_a_log", (2, 4)), ("gdn_dt_bias", (2, 4)), ("gdn_norm", (2, 128)), ("pool_w", (2, 4, 128, 128)),
    ("pool_scale", (2, 512)), ("hgrn_lb_logits", (2, 512)), ("hgrn_norm", (2, 128)),
    ("w_branch", (2, 4, 512, 1024)), ("w_out", (2, 1024, 1024)), ("ln_mix_g", (2, 1024)),
    ("ln_mix_b", (2, 1024)), ("ffn_w_gate", (1, 1024, 2816)), ("ffn_w_up", (1, 1024, 2816)),
    ("ffn_w_down", (1, 2816, 1024)), ("moe_router", (1, 1024, 8)), ("moe_w_gate", (1, 8, 1024, 3584)),
    ("moe_w_up", (1, 8, 1024, 3584)), ("moe_w_down", (1, 8, 3584, 1024)), ("ln_ffn_g", (2, 1024)),
    ("ln_ffn_b", (2, 1024)), ("moe_router_T", (1, 8, 1024)),
]


def weight_array(inputs, name):
    if name == "moe_router_T":
        return np.ascontiguousarray(np.asarray(inputs["moe_router"], dtype=np.float32).transpose(0, 2, 1))
    return np.ascontiguousarray(inputs[name], dtype=np.float32)


class Kern:
    def __init__(self, nc, stack, nseq=NSEQ, dbg=None):
        self.nc = nc
        self.fw = fw = FW(nc, stack)
        self.o = Ops(fw)
        self.nseq = nseq
        self.dbg = dbg or {}
        self.x_in = V(fw.dram("x", [nseq, S, D], F32, kind="ExternalInput"))
        self.pos = V(fw.dram("positions", [nseq, S], I32, kind="ExternalInput"))
        self.W = {}
        for name, shp in WEIGHT_SPECS:
            self.W[name] = V(fw.dram(name, list(shp), F32, kind="ExternalInput"))
        self.Cd = {}
        for name, shp in CONST_SHAPES.items():
            self.Cd[name] = V(fw.dram("c_" + name, list(shp), F32, kind="ExternalInput"))
        self.out = V(fw.dram("out", [nseq, S, D], F32, kind="ExternalOutput"))
        self.xres = V(fw.dram("xres", [nseq, S, D], F32))
        self.pb = [V(fw.psum("pb%d" % i, [128, 512], F32)) for i in range(8)]
        self.prr = 0
        sb = fw.sbuf
        o = self.o
        C = self.C = {}
        for name in ("ident", "m_causal", "m_up64", "m_sup64", "m_up32", "ones"):
            C[name] = V(sb("k_" + name, [128, 128], F32))
            o.dma(C[name], self.Cd[name])
        for name, w in (("m_chunk32", 4), ("m_chunk64", 2), ("ropep", 4), ("invcnt", 64)):
            C[name] = V(sb("k_" + name, [128, w], F32))
            o.dma(C[name], self.Cd[name])
        C["scan32"] = V(sb("k_scan32", [128, S], F32))
        o.dma(C["scan32"], self.Cd["scan32"])
        C["scan64x"] = V(sb("k_scan64x", [68, S], F32))
        o.dma(C["scan64x"], self.Cd["scan64"][0:68, :])
        C["sel4x"] = V(sb("k_sel4x", [68, 512], F32))
        o.dma(C["sel4x"], self.Cd["sel4x"][0:68, :])
        C["ident_b"] = V(sb("k_ident_b", [128, 128], BF16))
        o.copy(C["ident_b"], C["ident"])
        C["ones_b"] = V(sb("k_ones_b", [128, 128], BF16))
        o.copy(C["ones_b"], C["ones"])
        C["causal_b"] = V(sb("k_causal_b", [128, 128], BF16))
        o.copy(C["causal_b"], C["m_causal"])
        self.gates = V(sb("moe_gates", [128, NT, 8], F32))
        self.xT = V(sb("xT", [128, 8, S], BF16))
        self.yT = [V(sb("yT%d" % m, [128, 4, S], BF16)) for m in range(4)]

    def ps(self, lo=0, hi=5):
        n = hi - lo
        b = self.pb[lo + self.prr % n]
        self.prr += 1
        return b

    def scope(self):
        return _Scope(self)

    def load_xT(self, src):
        o = self.o
        with self.scope() as sc:
            xin = [V(sc.sbuf("xa_in%d" % i, [128, D], F32)) for i in range(2)]
            xb = [V(sc.sbuf("xa_b%d" % i, [128, D], BF16)) for i in range(2)]
            for ti in range(NT):
                a, b = xin[ti % 2], xb[ti % 2]
                o.dma(a, src[ti * 128:(ti + 1) * 128, :])
                o.copy(b, a, eng="act")
                self.transpose_into_xT(b, ti)

    def transpose_into_xT(self, xb, ti):
        o = self.o
        p = self.ps().bitcast(BF16)
        for k in range(8):
            o.tr(p[:, k * 128:(k + 1) * 128], xb[:, k * 128:(k + 1) * 128], self.C["ident_b"], inc=(k == 7))
        o.copy(self.xT[:, :, ti * 128:(ti + 1) * 128], p.rearrange("p (k t) -> p k t", k=8))

    def wload(self, dst, src):
        return self.o.dma(dst, src, eng="pool")

    def w_in_cols(self, l, c0, n):
        return self.W["w_in"][l, :, c0:c0 + n].rearrange("(k p) n -> p k n", p=128)

    def proj_fm(self, wt, ncols, cb, blocks=range(4), bw=512):
        o = self.o
        for tb in blocks:
            ps = self.ps()
            for k in range(8):
                o.mm(ps[0:ncols, 0:bw], wt[:, k, 0:ncols], self.xT[:, k, tb * bw:(tb + 1) * bw],
                     start=(k == 0), stop=(k == 7))
            cb(ps[0:ncols, 0:bw], tb)

    def proj_tm(self, wt, ncols, cb, tiles=range(NT)):
        o = self.o
        for ti in tiles:
            ps = self.ps()
            for k in range(8):
                o.mm(ps[:, 0:ncols], self.xT[:, k, ti * 128:(ti + 1) * 128], wt[:, k, 0:ncols],
                     start=(k == 0), stop=(k == 7))
            cb(ps[:, 0:ncols], ti)

    def gated_norm_out(self, sc_tiles, oT, wg, ng_col, func, dst):
        o = self.o
        sq, rs, gs, tmp = sc_tiles
        for tb in range(4):
            blk = slice(tb * 512, (tb + 1) * 512)
            o.act(sq, oT[:, blk], AF.Square)
            p = self.ps()
            o.mm(p, self.C["ones_b"], sq)
            o.act(rs, p, AF.Sqrt, bias=RMS_EPS, scale=1.0 / 128)
            o.recip(rs, rs)
            pg = self.ps()
            for k in range(8):
                o.mm(pg, wg[:, k, :], self.xT[:, k, blk], start=(k == 0), stop=(k == 7))
            o.act(gs, pg, func)
            o.stt(tmp, oT[:, blk], ng_col, rs, ALU.mult, ALU.mult)
            o.tt(dst[:, blk], tmp, gs, ALU.mult)


class _Scope:
    def __init__(self, kern):
        self.k = kern
        self.stack = ExitStack()

    def __enter__(self):
        self.stack.__enter__()
        return self

    def sbuf(self, name, shape, dtype):
        return self.k.fw.sbuf(name, shape, dtype, stack=self.stack)

    def __exit__(self, *a):
        if a[0] is None:
            self.k.fw.barrier()
        return self.stack.__exit__(*a)


class KernMix(Kern):
    def pool_mixer(self, l):
        o = self.o
        C = self.C
        with self.scope() as sc:
            wt = V(sc.sbuf("pl_w", [128, 8, 512], BF16))
            self.wload(wt, self.w_in_cols(l, OFF["pu"], 512))
            pw = V(sc.sbuf("pl_pw", [128, 4, 128], BF16))
            self.wload(pw, self.W["pool_w"][l].rearrange("g c d -> c g d"))
            psc = V(sc.sbuf("pl_sc", [128, 4], F32))
            o.dma(psc, self.W["pool_scale"][l].rearrange("(g p) -> p g", p=128), slow=True)
            u = V(sc.sbuf("pl_u", [128, 4, S], F32))
            tmp = [V(sc.sbuf("pl_t%d" % i, [128, S], F32)) for i in range(2)]
            pooled = V(sc.sbuf("pl_pooled", [128, S], BF16))
            t16 = V(sc.sbuf("pl_t16", [128, 16], F32))
            for gi in range(4):
                self.proj_fm(wt[:, :, gi * 128:(gi + 1) * 128], 128,
                             lambda ps, tb, gi=gi: o.copy(u[:, gi, tb * 512:(tb + 1) * 512], ps, eng="act"))
            for gi in range(4):
                win = 2 ** (gi + 1)
                cur = u[:, gi, :]
                sh = 1
                for step in range(gi + 1):
                    nxt = tmp[step % 2]
                    o.tt(nxt[:, sh:S], cur[:, sh:S], cur[:, 0:S - sh], ALU.add)
                    o.copy(nxt[:, 0:sh], cur[:, 0:sh])
                    cur = nxt
                    sh *= 2
                o.stt(pooled[:, :], cur, 1.0 / win, u[:, gi, :], ALU.mult, ALU.subtract)
                o.tt(t16, cur[:, 0:16], C["invcnt"][:, gi * 16:(gi + 1) * 16], ALU.mult)
                o.tt(pooled[:, 0:16], t16, u[:, gi, 0:16], ALU.subtract)
                for tb in range(4):
                    blk = slice(tb * 512, (tb + 1) * 512)
                    p = self.ps()
                    o.mm(p, pw[:, gi, :], pooled[:, blk])
                    o.ts(self.yT[2][:, gi, blk], p, psc[:, gi:gi + 1], None, ALU.mult)

    def hgrn_prep(self, l):
        o = self.o
        if hasattr(self, "_lb"):
            return
        sb = self.fw.sbuf
        lg = V(sb("hg_lg", [128, 2, 4], F32))
        o.dma(lg, self.W["hgrn_lb_logits"].rearrange("l (h p) -> p l h", p=128), slow=True)
        lb = V(sb("hg_lb", [128, 2, 4], F32))
        oml = V(sb("hg_oml", [128, 2, 4], F32))
        noml = V(sb("hg_noml", [128, 2, 4], F32))
        d = V(sb("hg_d", [128, 4], F32))
        o.tt(d, lg[:, 1, :], lg[:, 0, :], ALU.subtract)
        o.memset(lb[:, 0, :], 0.0)
        o.act(lb[:, 1, :], d, AF.Sigmoid)
        o.ts(oml, lb, -1.0, 1.0, ALU.mult, ALU.add)
        o.ts(noml, oml, -1.0, None, ALU.mult)
        self._lb, self._oml, self._noml = lb, oml, noml
        ng = V(sb("hg_ng", [128, 2], F32))
        o.dma(ng, self.W["hgrn_norm"].rearrange("l p -> p l"), slow=True)
        self._hng = ng

    def hgrn_mixer(self, l):
        o = self.o
        C = self.C
        self.hgrn_prep(l)
        for h in range(4):
            with self.scope() as sc:
                sb = sc.sbuf
                wq = V(sb("h_wq", [128, 8, 128], BF16))
                wf = V(sb("h_wf", [128, 8, 128], BF16))
                wi = V(sb("h_wi", [128, 8, 128], BF16))
                wg = V(sb("h_wg", [128, 8, 128], BF16))
                self.wload(wq, self.w_in_cols(l, OFF["hq"] + h * 128, 128))
                self.wload(wf, self.w_in_cols(l, OFF["hf"] + h * 128, 128))
                self.wload(wi, self.w_in_cols(l, OFF["hi"] + h * 128, 128))
                self.wload(wg, self.w_in_cols(l, OFF["hg"] + h * 128, 128))
                lbc = self._lb[:, l, h:h + 1]
                omlc = self._oml[:, l, h:h + 1]
                nomlc = self._noml[:, l, h:h + 1]
                q0T = V(sb("h_q0T", [128, S], BF16))
                qmT = V(sb("h_qmT", [128, S], BF16))
                kmT = V(sb("h_kmT", [128, S], BF16))
                khT = V(sb("h_khT", [128, S], BF16))
                vsb = V(sb("h_v", [128, NT, 128], BF16))
                oT = V(sb("h_oT", [128, S], F32))
                eb = V(sb("h_eb", [128, 64], F32))
                with self.scope() as s1:
                    s1b = s1.sbuf
                    qs = V(s1b("h_qs", [128, S], F32))
                    kT = V(s1b("h_kT", [128, S], F32))
                    lf = V(s1b("h_lf", [128, S], F32))
                    b = V(s1b("h_b", [128, S], F32))
                    e = V(s1b("h_e", [128, S], F32))
                    self.proj_fm(wq, 128, lambda ps, tb: o.act(qs[:, tb * 512:(tb + 1) * 512], ps, AF.Silu))

                    def f_cb(ps, tb):
                        blk = slice(tb * 512, (tb + 1) * 512)
                        o.act(e[:, blk], ps, AF.Sigmoid)
                        o.ts(lf[:, blk], e[:, blk], omlc, lbc, ALU.mult, ALU.add)
                        o.ts(kT[:, blk], e[:, blk], nomlc, omlc, ALU.mult, ALU.add)
                        o.act(lf[:, blk], lf[:, blk], AF.Ln)
                    self.proj_fm(wf, 128, f_cb)
                    self.proj_tm(wi, 128, lambda ps, ti: o.copy(vsb[:, ti, :], ps, eng="act"))
                    o.scan(b, C["scan32"], lf, 0.0, ALU.mult, ALU.add)
                    b3 = b.rearrange("p (c t) -> p c t", t=32)
                    o.act(e, b, AF.Exp)
                    e3 = e.rearrange("p (c t) -> p c t", t=32)
                    o.copy(eb, e3[:, :, 31])
                    o.tt(q0T, qs, e, ALU.mult)
                    lf3 = lf.rearrange("p (c t) -> p c t", t=32)
                    o.tt(lf3, b3, b3[:, :, 15:16].bcast([128, 64, 32]), ALU.subtract)
                    o.act(e, lf, AF.Exp)
                    o.tt(qmT, qs, e, ALU.mult)
                    o.act(e, lf, AF.Exp, scale=-1.0)
                    o.tt(kmT, kT, e, ALU.mult)
                    o.tt(lf3, b3, b3[:, :, 31:32].bcast([128, 64, 32]), ALU.subtract)
                    o.act(e, lf, AF.Exp, scale=-1.0)
                    o.tt(khT, kT, e, ALU.mult)
                att = V(sb("h_att", [128, NT, 128], BF16))
                khm = V(sb("h_khm", [128, NT, 4, 128], BF16))
                Sts = [V(sb("h_S%d" % i, [128, 128], F32)) for i in range(2)]
                Sb = [V(sb("h_Sb%d" % i, [128, 128], BF16)) for i in range(2)]
                g_sq = V(sb("h_gsq", [128, 512], BF16))
                g_rs = V(sb("h_grs", [128, 512], F32))
                g_gs = V(sb("h_ggs", [128, 512], F32))
                g_tmp = V(sb("h_gtmp", [128, 512], F32))
                for ti in range(NT):
                    tl = slice(ti * 128, (ti + 1) * 128)
                    p = self.ps(0, 4)
                    o.mm(p[:, 0:128], kmT[:, tl], qmT[:, tl])
                    o.tt(att[:, ti, :], p[:, 0:128], C["m_up32"], ALU.mult)
                    pt = self.ps(0, 4).bitcast(BF16)
                    o.tr(pt[:, 0:128], khT[:, tl], C["ident_b"])
                    for c in range(4):
                        o.act(khm[:, ti, c, :], pt[:, 0:128], AF.Copy, scale=C["m_chunk32"][:, c:c + 1])
                o.memset(Sts[0], 0.0)
                o.memset(Sb[0], 0.0)
                cur = 0
                pOs = [self.pb[4], self.pb[5]]
                pSs = [self.pb[6], self.pb[7]]
                def emit_pS(ti):
                    for c in range(4):
                        o.mm(pSs[ti % 2][:, c * 128:(c + 1) * 128], khm[:, ti, c, :], vsb[:, ti, :])

                emit_pS(0)
                for ti in range(NT):
                    tl = slice(ti * 128, (ti + 1) * 128)
                    pO, pS = pOs[ti % 2], pSs[ti % 2]
                    if ti + 1 < NT:
                        emit_pS(ti + 1)
                    o.mm(pO[:, 0:128], vsb[:, ti, :], att[:, ti, :], start=True, stop=False)
                    for c in range(4):
                        cs = slice(ti * 128 + c * 32, ti * 128 + (c + 1) * 32)
                        o.mm(pO[:, c * 32:(c + 1) * 32], Sb[cur], q0T[:, cs], start=False, stop=(c == 3), inc=True)
                        o.stt(Sts[1 - cur], Sts[cur], eb[:, ti * 4 + c:ti * 4 + c + 1], pS[:, c * 128:(c + 1) * 128], ALU.mult, ALU.add)
                        cur = 1 - cur
                        o.copy(Sb[cur], Sts[cur], eng="act")
                    o.copy(oT[:, tl], pO[:, 0:128], eng="act")
                self.gated_norm_out((g_sq, g_rs, g_gs, g_tmp), oT, wg, self._hng[:, l:l + 1], AF.Sigmoid,
                                    self.yT[3][:, h, :])


class KernFull0(KernMix):
    pass


def build_debug(stages, nseq=1):
    nc = bass.Bass("TRN2", target_bir_lowering=False)
    stack = ExitStack()
    with stack:
        k = KERN_CLS(nc, stack, nseq=nseq)
        o = k.o
        dump = V(k.fw.dram("dump", [4, 128, 4, S], BF16, kind="ExternalOutput"))
        l = stages.get("layer", 0)
        k.load_xT(k.x_in[0])
        for m, name in enumerate(("mla", "gdn", "pool", "hgrn")):
            if name in stages["mixers"]:
                getattr(k, name + "_mixer")(l, 0) if name == "mla" else getattr(k, name + "_mixer")(l)
                o.dma(dump[m], k.yT[m])
        k.fw.finish()
        print("instructions:", k.fw.ninst, "sems:", k.fw.nsem, "sbuf left:", nc.sbuf_bytes_remaining)
    return nc


class KernMLA(KernMix):
    def rope_tables(self, sc, s):
        o = self.o
        C = self.C
        cosT = V(sc.sbuf("rp_cos", [64, S], BF16))
        sinT = V(sc.sbuf("rp_sin", [64, S], BF16))
        with self.scope() as s2:
            posi = V(s2.sbuf("rp_pi", [64, S], I32))
            ang = V(s2.sbuf("rp_ang", [64, S], F32))
            t = V(s2.sbuf("rp_t", [64, S], F32))
            ki = V(s2.sbuf("rp_ki", [64, S], I32))
            ang2 = V(s2.sbuf("rp_ang2", [64, S], F32))
            o.dma(posi, self.pos[s:s + 1, :].bcast([64, S]))
            o.copy(ang, posi)
            o.ts(ang, ang, C["ropep"][0:64, 0:1], None, ALU.mult)
            for shift, dst, scale in ((0.0, sinT, C["ropep"][0:64, 1:2]), (math.pi / 2, cosT, None)):
                o.ts(t, ang, shift, 1.0 / (2 * math.pi), ALU.add, ALU.mult)
                o.copy(ki, t)
                o.copy(t, ki)
                o.stt(t, t, -2 * math.pi, ang, ALU.mult, ALU.add)
                if shift != 0.0:
                    o.ts(t, t, shift, None, ALU.add)
                o.ts(ang2, t, math.pi, 2 * math.pi, ALU.is_gt, ALU.mult)
                o.tt(t, t, ang2, ALU.subtract)
                o.ts(ang2, t, -math.pi, 2 * math.pi, ALU.is_lt, ALU.mult)
                o.tt(t, t, ang2, ALU.add)
                o.ts(t, t, 3.141592, -3.141592, ALU.min, ALU.max)
                o.act(dst, t, AF.Sin, scale=scale)
        return cosT, sinT

    def mla_mixer(self, l, s):
        o = self.o
        C = self.C
        with self.scope() as sc:
            sb = sc.sbuf
            cosT, sinT = self.rope_tables(sc, s)
            wcq = V(sb("a_wcq", [128, 8, 256], BF16))
            wckv = V(sb("a_wckv", [128, 8, 128], BF16))
            wkrA = V(sb("a_wkrA", [128, 8, 64], BF16))
            wkrB = V(sb("a_wkrB", [128, 8, 64], BF16))
            wuq = V(sb("a_wuq", [128, 2, 768], BF16))
            wuqB = V(sb("a_wuqB", [128, 2, 4, 64], BF16))
            wukv = V(sb("a_wukv", [128, 1024], BF16))
            gq = V(sb("a_gq", [128, 2], F32))
            gkv = V(sb("a_gkv", [128, 1], F32))
            self.wload(wcq, self.w_in_cols(l, OFF["cq"], 256))
            self.wload(wckv, self.w_in_cols(l, OFF["ckv"], 128))
            self.wload(wkrA, self.w_in_cols(l, OFF["kr"], 64))
            self.wload(wkrB[:, :, 0:32], self.w_in_cols(l, OFF["kr"] + 32, 32))
            self.wload(wkrB[:, :, 32:64], self.w_in_cols(l, OFF["kr"], 32))
            uq = self.W["mla_w_uq"][l].rearrange("(c p) n -> p c n", p=128)
            self.wload(wuq, uq)
            for h in range(4):
                self.wload(wuqB[:, :, h, 0:32], uq[:, :, h * 192 + 160:h * 192 + 192])
                self.wload(wuqB[:, :, h, 32:64], uq[:, :, h * 192 + 128:h * 192 + 160])
            self.wload(wukv, self.W["mla_w_ukv"][l])
            o.dma(gq, self.W["mla_q_norm"][l].rearrange("(c p) -> p c", p=128), slow=True)
            o.dma(gkv, self.W["mla_kv_norm"][l].rearrange("(p o) -> p o", o=1), slow=True)
            cqn = V(sb("a_cqn", [128, 2, S], BF16))
            ckvn = V(sb("a_ckvn", [128, S], BF16))
            krT = V(sb("a_krT", [64, S], BF16))
            cqf = V(sb("a_cqf", [128, 2, 512], F32))
            sq = V(sb("a_sq", [128, 2, 512], BF16))
            rq = V(sb("a_rq", [128, 512], F32))
            t1 = V(sb("a_t1", [64, 512], F32))
            t2 = V(sb("a_t2", [64, 512], F32))

            def rope_combine(dst, psA, psB, blk):
                o.tt(t1, psA, cosT[:, blk], ALU.mult)
                o.tt(t2, psB, sinT[:, blk], ALU.mult)
                o.tt(dst, t1, t2, ALU.add)

            for tb in range(4):
                blk = slice(tb * 512, (tb + 1) * 512)
                for c in range(2):
                    ps = self.ps()
                    for k in range(8):
                        o.mm(ps, wcq[:, k, c * 128:(c + 1) * 128], self.xT[:, k, blk], start=(k == 0), stop=(k == 7))
                    o.copy(cqf[:, c, :], ps, eng="act")
                    o.act(sq[:, c, :], ps, AF.Square)
                pss = self.ps()
                for c in range(2):
                    o.mm(pss, C["ones_b"], sq[:, c, :], start=(c == 0), stop=(c == 1))
                o.act(rq, pss, AF.Sqrt, bias=RMS_EPS, scale=1.0 / 256)
                o.recip(rq, rq)
                for c in range(2):
                    o.stt(cqn[:, c, blk], cqf[:, c, :], gq[:, c:c + 1], rq, ALU.mult, ALU.mult)
                ps = self.ps()
                for k in range(8):
                    o.mm(ps, wckv[:, k, :], self.xT[:, k, blk], start=(k == 0), stop=(k == 7))
                o.copy(cqf[:, 0, :], ps, eng="act")
                o.act(sq[:, 0, :], ps, AF.Square)
                pss = self.ps()
                o.mm(pss, C["ones_b"], sq[:, 0, :])
                o.act(rq, pss, AF.Sqrt, bias=RMS_EPS, scale=1.0 / 128)
                o.recip(rq, rq)
                o.stt(ckvn[:, blk], cqf[:, 0, :], gkv[:, 0:1], rq, ALU.mult, ALU.mult)
                psA = self.ps()
                psB = self.ps()
                for k in range(8):
                    o.mm(psA[0:64, :], wkrA[:, k, :], self.xT[:, k, blk], start=(k == 0), stop=(k == 7))
                for k in range(8):
                    o.mm(psB[0:64, :], wkrB[:, k, :], self.xT[:, k, blk], start=(k == 0), stop=(k == 7))
                rope_combine(krT[:, blk], psA[0:64, :], psB[0:64, :], blk)

            qnT = V(sb("a_qnT", [128, S], BF16))
            qrT = V(sb("a_qrT", [64, S], BF16))
            knT = V(sb("a_knT", [128, S], BF16))
            vsb = V(sb("a_v", [128, NT, 128], BF16))
            pts = [V(sb("a_pt%d" % i, [128, 512], BF16)) for i in range(3)]
            rd = V(sb("a_rd", [128, 512], F32))
            pO = self.pb[5]
            pD = self.pb[6]
            for h in range(4):
                for tb in range(4):
                    blk = slice(tb * 512, (tb + 1) * 512)
                    ps = self.ps()
                    for c in range(2):
                        o.mm(ps, wuq[:, c, h * 192:h * 192 + 128], cqn[:, c, blk], start=(c == 0), stop=(c == 1))
                    o.copy(qnT[:, blk], ps, eng="act")
                    psA = self.ps()
                    psB = self.ps()
                    for c in range(2):
                        o.mm(psA[0:64, :], wuq[:, c, h * 192 + 128:h * 192 + 192], cqn[:, c, blk], start=(c == 0), stop=(c == 1))
                    for c in range(2):
                        o.mm(psB[0:64, :], wuqB[:, c, h, :], cqn[:, c, blk], start=(c == 0), stop=(c == 1))
                    rope_combine(qrT[:, blk], psA[0:64, :], psB[0:64, :], blk)
                    ps = self.ps()
                    o.mm(ps, wukv[:, h * 256:h * 256 + 128], ckvn[:, blk])
                    o.copy(knT[:, blk], ps, eng="act")
                for ti in range(NT):
                    ps = self.ps()
                    o.mm(ps[:, 0:128], ckvn[:, ti * 128:(ti + 1) * 128], wukv[:, h * 256 + 128:h * 256 + 256])
                    o.copy(vsb[:, ti, :], ps[:, 0:128], eng="act")
                it = 0
                for a in range(4):
                    nj = 4 * a + 4

                    def geom(j, a=a):
                        qlo = max(j * 128, a * 512)
                        qhi = (a + 1) * 512
                        return qlo, qhi, qhi - qlo, qlo - a * 512, slice(j * 128, (j + 1) * 128)

                    def scores(j):
                        qlo, qhi, wd, off, ks = geom(j)
                        ps = self.ps()
                        o.mm(ps[:, 0:wd], knT[:, ks], qnT[:, qlo:qhi], start=True, stop=False)
                        o.mm(ps[:, 0:wd], krT[:, ks], qrT[:, qlo:qhi], start=False, stop=True)
                        return ps

                    ps_next = scores(0)
                    for j in range(nj):
                        qlo, qhi, wd, off, ks = geom(j)
                        ps = ps_next
                        if j + 1 < nj:
                            ps_next = scores(j + 1)
                        pt = pts[it % 3]
                        it += 1
                        o.act(pt[:, 0:wd], ps[:, 0:wd], AF.Exp, scale=ATT_SCALE)
                        if j >= 4 * a:
                            o.tt(pt[:, 0:128], pt[:, 0:128], C["causal_b"], ALU.mult)
                        o.mm(pO[:, off:512], vsb[:, j, :], pt[:, 0:wd], start=(j == 0), stop=(j == nj - 1))
                        o.mm(pD[:, off:512], C["ones_b"], pt[:, 0:wd], start=(j == 0), stop=(j == nj - 1))
                    o.recip(rd, pD)
                    o.tt(self.yT[0][:, h, a * 512:(a + 1) * 512], pO, rd, ALU.mult)


class KernGDN(KernMLA):
    def gdn_mixer(self, l):
        self.fw.pe_safe = True
        try:
            self._gdn_mixer(l)
        finally:
            self.fw.pe_safe = False

    def _gdn_mixer(self, l):
        o = self.o
        C = self.C
        with self.scope() as sc:
            sb = sc.sbuf
            scal = V(sb("g_scal", [68, S], F32))
            tok = V(sb("g_tok", [128, NT, 16], F32))
            egl = V(sb("g_egl", [68, 32], F32))
            bge = V(sb("g_bge", [128, NT, 4], F32))
            with self.scope() as s2:
                w3 = V(s2.sbuf("g_w3", [128, 8, 68], BF16))
                par = V(s2.sbuf("g_par", [68, 4], F32))
                tmp = V(s2.sbuf("g_tmp", [68, 512], F32))
                edT = V(s2.sbuf("g_edT", [4, S], F32))
                o.memset(w3, 0.0)
                o.memset(par, 0.0)
                self.wload(w3[:, :, 0:4], self.w_in_cols(l, OFF["ga"], 4))
                self.wload(w3[:, :, 32:36], self.w_in_cols(l, OFF["gb"], 4))
                self.wload(w3[:, :, 64:68], self.w_in_cols(l, OFF["ga"], 4))
                for base in (0, 64):
                    o.dma(par[base:base + 4, 0:1], self.W["gdn_dt_bias"][l].rearrange("(p o) -> p o", o=1), slow=True)
                    o.dma(par[base:base + 4, 1:2], self.W["gdn_a_log"][l].rearrange("(p o) -> p o", o=1), slow=True)
                o.act(par[:, 2:3], par[:, 1:2], AF.Exp)
                o.ts(par[:, 2:3], par[:, 2:3], -1.0, None, ALU.mult)
                for tb in range(4):
                    blk = slice(tb * 512, (tb + 1) * 512)
                    ps = self.ps()
                    for k in range(8):
                        o.mm(ps[0:68, :], w3[:, k, :], self.xT[:, k, blk], start=(k == 0), stop=(k == 7))
                    o.act(tmp, ps[0:68, :], AF.Exp, bias=par[:, 0:1])
                    o.act(tmp, tmp, AF.Ln, bias=1.0)
                    o.ts(tmp, tmp, par[:, 2:3], None, ALU.mult)
                    o.copy(scal[:, blk], tmp)
                    o.act(tmp[32:36, :], ps[32:36, :], AF.Sigmoid)
                    o.copy(scal[32:36, blk], tmp[32:36, :])
                for base in (0, 64):
                    for tb in range(4):
                        blk = slice(tb * 512, (tb + 1) * 512)
                        o.scan(tmp[base:base + 4, :], C["scan64x"][base:base + 4, blk], scal[base:base + 4, blk], 0.0, ALU.mult, ALU.add)
                        o.copy(scal[base:base + 4, blk], tmp[base:base + 4, :])
                gc3 = scal[0:4, :].rearrange("p (c t) -> p c t", t=64)
                ed3 = edT.rearrange("p (c t) -> p c t", t=64)
                o.tt(ed3, gc3[:, :, 63:64].bcast([4, 32, 64]), gc3, ALU.subtract)
                o.act(edT, edT, AF.Exp)
                o.act(scal[64:68, :], scal[64:68, :], AF.Exp)
                o.copy(egl[64:68, :], scal[64:68, :].rearrange("p (c t) -> p c t", t=64)[:, :, 63])
                for ti in range(NT):
                    tl = slice(ti * 128, (ti + 1) * 128)
                    ps = self.ps()
                    o.tr(ps[:, 0:4], scal[0:4, tl], C["ident"][0:4, 0:4])
                    o.tr(ps[:, 4:8], scal[32:36, tl], C["ident"][32:36, 32:36])
                    o.tr(ps[:, 8:12], scal[64:68, tl], C["ident"][64:68, 64:68])
                    o.tr(ps[:, 12:16], edT[0:4, tl], C["ident"][0:4, 0:4])
                    o.copy(tok[:, ti, :], ps[:, 0:16])
                o.tt(bge, tok[:, :, 4:8], tok[:, :, 8:12], ALU.mult)
            ng = V(sb("g_ng", [128, 1], F32))
            o.dma(ng, self.W["gdn_norm"][l].rearrange("(p o) -> p o", o=1), slow=True)
            for h in range(4):
                self.gdn_head(l, h, scal, tok, egl, bge, ng)

    def gdn_head(self, l, h, scal, tok, egl, bge, ng):
        MARKS.append(("gdn h%d start" % h, self.nc.get_next_instruction_name()))
        o = self.o
        C = self.C
        sel = C["sel4x"]
        hs = slice(h * 128, (h + 1) * 128)
        with self.scope() as sc:
            sb = sc.sbuf
            wz = V(sb("g_wz", [128, 8, 128], BF16))
            self.wload(wz, self.w_in_cols(l, OFF["gz"] + h * 128, 128))
            qT = V(sb("g_qT", [128, S], BF16))
            kT = V(sb("g_kT", [128, S], BF16))
            vT = V(sb("g_vT", [128, S], BF16))
            with self.scope() as s1:
                s1b = s1.sbuf
                wts = []
                for i, nm in enumerate(("gq", "gk", "gv")):
                    w = V(s1b("g_w" + nm, [128, 8, 128], BF16))
                    self.wload(w, self.w_in_cols(l, OFF[nm] + h * 128, 128))
                    wts.append(w)
                cw = V(s1b("g_cw", [128, 3, 4], F32))
                for i in range(3):
                    o.dma(cw[:, i, :], self.W["gdn_conv"][l][:, i * 512 + h * 128:i * 512 + (h + 1) * 128].rearrange("t c -> c t"), slow=True)
                xin = V(s1b("g_xin", [128, S], F32))
                acc = V(s1b("g_acc", [128, S], F32))
                sq = V(s1b("g_sq", [128, 512], BF16))
                rs = V(s1b("g_rs", [128, 512], F32))
                for i, dst in enumerate((qT, kT, vT)):
                    self.fw.pe_safe = False
                    self.proj_fm(wts[i], 128, lambda ps, tb: o.copy(xin[:, tb * 512:(tb + 1) * 512], ps, eng="act"))
                    self.fw.pe_safe = True
                    o.ts(acc, xin, cw[:, i, 3:4], None, ALU.mult)
                    for d in range(1, 4):
                        o.stt(acc[:, d:S], xin[:, 0:S - d], cw[:, i, 3 - d:4 - d], acc[:, d:S], ALU.mult, ALU.add)
                    o.act(acc, acc, AF.Silu)
                    if i == 2:
                        o.copy(vT, acc)
                        continue
                    for tb in range(4):
                        blk = slice(tb * 512, (tb + 1) * 512)
                        o.act(sq, acc[:, blk], AF.Square)
                        p = self.ps()
                        o.mm(p, C["ones_b"], sq)
                        o.act(rs, p, AF.Sqrt, bias=RMS_EPS, scale=1.0)
                        o.recip(rs, rs)
                        if i == 0:
                            o.stt(dst[:, blk], acc[:, blk], 128 ** -0.5, rs, ALU.mult, ALU.mult)
                        else:
                            o.tt(dst[:, blk], acc[:, blk], rs, ALU.mult)
            MARKS.append(("gdn h%d front-done" % h, self.nc.get_next_instruction_name()))
            u = V(sb("g_u", [128, NT, 128], F32))
            wT = V(sb("g_wT", [128, S], BF16))
            qgT = V(sb("g_qgT", [128, S], BF16))
            kdec = V(sb("g_kdec", [128, NT, 128], BF16))
            qkm = V(sb("g_qkm", [128, NT, 128], BF16))
            dtab = V(sb("g_dtab", [128, 32], F32))
            oT = V(sb("g_oT", [128, S], F32))
            LT = V(sb("g_LT", [128, 128], F32))
            dd = V(sb("g_dd", [128, 128], F32))
            t1 = V(sb("g_t1", [128, 128], F32))
            G = 4
            sets = []
            for i in range(G):
                sets.append(dict(
                    PT=[V(sb("g_PTa%d" % i, [128, 128], F32)), V(sb("g_PTb%d" % i, [128, 128], F32))],
                    P=[V(sb("g_Pa%d" % i, [128, 128], F32)), V(sb("g_Pb%d" % i, [128, 128], F32))],
                    Y=V(sb("g_Y%d" % i, [128, 256], F32))))
            p = self.ps()
            o.mm(p[:, 0:32], sel[64:68, hs], egl[64:68, :])
            o.copy(dtab, p[:, 0:32])
            for base in range(0, NT, G):
                for i in range(G):
                    ti = base + i
                    T = sets[i]
                    AT, Y = T["PT"][0], T["Y"]
                    tl = slice(ti * 128, (ti + 1) * 128)
                    gcc = tok[:, ti, h:h + 1]
                    pG = self.ps()
                    o.mm(pG[:, 0:128], sel[0:4, hs], scal[0:4, tl])
                    o.mm(pG[:, 128:256], sel[32:36, hs], scal[32:36, tl])
                    o.mm(pG[:, 256:384], sel[64:68, hs], scal[64:68, tl])
                    o.ts(dd, pG[:, 0:128], gcc, 0.0, ALU.subtract, ALU.min)
                    o.act(dd, dd, AF.Exp)
                    pK = self.ps()
                    o.mm(pK[:, 0:128], kT[:, tl], kT[:, tl])
                    o.mm(pK[:, 128:256], kT[:, tl], qT[:, tl])
                    pt = self.ps().bitcast(BF16)
                    o.tr(pt[:, 0:128], kT[:, tl], C["ident_b"])
                    o.tr(pt[:, 128:256], vT[:, tl], C["ident_b"])
                    o.tt(qgT[:, tl], qT[:, tl], pG[:, 256:384], ALU.mult)
                    o.ts(Y[:, 0:128], pt[:, 128:256], tok[:, ti, 4 + h:5 + h], None, ALU.mult)
                    o.ts(Y[:, 128:256], pt[:, 0:128], bge[:, ti, h:h + 1], None, ALU.mult)
                    o.ts(kdec[:, ti, :], pt[:, 0:128], tok[:, ti, 12 + h:13 + h], None, ALU.mult)
                    o.tt(LT, dd, C["m_up64"], ALU.mult)
                    o.tt(t1, dd, C["m_sup64"], ALU.mult)
                    o.tt(qkm[:, ti, :], pK[:, 128:256], LT, ALU.mult)
                    o.tt(t1, pK[:, 0:128], t1, ALU.mult)
                    o.tt(AT, t1, pG[:, 128:256], ALU.mult)
                    pa = self.ps()
                    o.tr(pa[:, 0:128], AT, C["ident"])
                    pa2 = self.ps()
                    o.mm(pa2[:, 0:256], AT, Y)
                    o.copy(T["P"][0], pa[:, 0:128], eng="act")
                    o.tt(Y, Y, pa2[:, 0:256], ALU.subtract)
                cur = 0
                for k in range(5):
                    nxt = 1 - cur
                    pps = [self.pb[i] for i in range(G)]
                    pqs = [self.pb[4 + i] for i in range(G)]
                    for i in range(G):
                        T = sets[i]
                        o.mm(pps[i][:, 0:128], T["P"][cur], T["PT"][cur])
                        if k < 4:
                            o.mm(pps[i][:, 128:256], T["PT"][cur], T["P"][cur])
                    for i in range(G):
                        T = sets[i]
                        o.copy(T["PT"][nxt], pps[i][:, 0:128], eng="act")
                        if k < 4:
                            o.copy(T["P"][nxt], pps[i][:, 128:256], eng="act")
                    for i in range(G):
                        T = sets[i]
                        o.mm(pqs[i][:, 0:256], T["PT"][nxt], T["Y"])
                    for i in range(G):
                        T = sets[i]
                        o.tt(T["Y"], T["Y"], pqs[i][:, 0:256], ALU.add)
                    cur = nxt
                for i in range(G):
                    ti = base + i
                    T = sets[i]
                    tl = slice(ti * 128, (ti + 1) * 128)
                    wtok = T["P"][0]
                    o.copy(u[:, ti, :], T["Y"][:, 0:128], eng="act")
                    o.copy(wtok, T["Y"][:, 128:256], eng="act")
                    pw = self.ps()
                    o.tr(pw[:, 0:128], wtok, C["ident"])
                    o.copy(wT[:, tl], pw[:, 0:128], eng="act")
            MARKS.append(("gdn h%d prep-done" % h, self.nc.get_next_instruction_name()))
            Sts = [V(sb("g_S%d" % i, [128, 128], F32)) for i in range(2)]
            Sbs = [V(sb("g_Sb%d" % i, [128, 128], BF16)) for i in range(2)]
            vn = [V(sb("g_vn%d" % i, [128, 128], BF16)) for i in range(2)]
            o.memset(Sts[0], 0.0)
            o.memset(Sbs[0], 0.0)
            pO = self.pb[5]
            pSs = [self.pb[6], self.pb[6]]
            pV = self.pb[7]
            cur = 0
            for n in range(32):
                ti, half = n // 2, n % 2
                rows = slice(half * 64, half * 64 + 64)
                cols = slice(n * 64, n * 64 + 64)
                v = vn[ti % 2]
                pS = pSs[n % 2]
                St, Sb = Sts[cur], Sbs[cur]
                o.mm(pV[rows, 0:128], wT[:, cols], Sb)
                o.tt(v[rows, :], u[rows, ti, :], pV[rows, 0:128], ALU.subtract)
                oc = slice(half * 64, half * 64 + 64)
                o.mm(pO[:, oc], Sb, qgT[:, cols], start=True, stop=False, inc=True)
                o.mm(pO[:, oc], v[rows, :], qkm[rows, ti, half * 64:half * 64 + 64], start=False, stop=True)
                o.mm(pS[:, 0:128], kdec[rows, ti, :], v[rows, :])
                o.stt(Sbs[1 - cur], St, dtab[:, n:n + 1], pS[:, 0:128], ALU.mult, ALU.add)
                o.stt(Sts[1 - cur], St, dtab[:, n:n + 1], pS[:, 0:128], ALU.mult, ALU.add)
                cur = 1 - cur
                if half == 1:
                    o.copy(oT[:, ti * 128:(ti + 1) * 128], pO[:, 0:128], eng="act")
            MARKS.append(("gdn h%d scan-done" % h, self.nc.get_next_instruction_name()))
            g_sq = V(sb("g_gsq", [128, 512], BF16))
            g_rs = V(sb("g_grs", [128, 512], F32))
            g_gs = V(sb("g_ggs", [128, 512], F32))
            g_tmp = V(sb("g_gtmp", [128, 512], F32))
            self.fw.pe_safe = False
            self.gated_norm_out((g_sq, g_rs, g_gs, g_tmp), oT, wz, ng[:, 0:1], AF.Silu, self.yT[1][:, h, :])
            self.fw.pe_safe = True


class KernFull(KernGDN):
    def xacc(self, ti):
        v = self.yT[ti // 4].rearrange("p a s -> p (a s)").bitcast(F32)
        return v[:, (ti % 4) * 1024:(ti % 4 + 1) * 1024]

    def bc_row(self, sc, name, src_row):
        t = V(sc.sbuf(name, [128, D], F32))
        self.o.dma(t, src_row.rearrange("(o d) -> o d", o=1).bcast([128, D]))
        return t

    def ln_tile(self, r, gbc, bbc, st, junk):
        o = self.o
        o.memset(st[:, 0:2], 0.0)
        o.act(junk, r, AF.Copy, accum=st[:, 0:1])
        o.act(junk, r, AF.Square, accum=st[:, 1:2])
        o.ts(st[:, 2:4], st[:, 0:2], 1.0 / D, None, ALU.mult)
        o.tt(st[:, 4:5], st[:, 2:3], st[:, 2:3], ALU.mult)
        o.tt(st[:, 5:6], st[:, 3:4], st[:, 4:5], ALU.subtract)
        o.act(st[:, 6:7], st[:, 5:6], AF.Sqrt, bias=LN_EPS)
        o.recip(st[:, 7:8], st[:, 6:7])
        o.ts(r, r, st[:, 2:3], st[:, 7:8], ALU.subtract, ALU.mult)
        o.tt(r, r, gbc, ALU.mult)
        o.tt(r, r, bbc, ALU.add)

    def ln_tiles(self, items, gbc, bbc):
        o = self.o
        for r, st, junk in items:
            o.memset(st[:, 0:2], 0.0)
        for r, st, junk in items:
            o.act(junk, r, AF.Copy, accum=st[:, 0:1])
        for r, st, junk in items:
            o.act(junk, r, AF.Square, accum=st[:, 1:2])
        for r, st, junk in items:
            o.ts(st[:, 2:4], st[:, 0:2], 1.0 / D, None, ALU.mult)
        for r, st, junk in items:
            o.tt(st[:, 4:5], st[:, 2:3], st[:, 2:3], ALU.mult)
        for r, st, junk in items:
            o.tt(st[:, 5:6], st[:, 3:4], st[:, 4:5], ALU.subtract)
        for r, st, junk in items:
            o.act(st[:, 6:7], st[:, 5:6], AF.Sqrt, bias=LN_EPS)
        for r, st, junk in items:
            o.recip(st[:, 7:8], st[:, 6:7])
        for r, st, junk in items:
            o.ts(r, r, st[:, 2:3], st[:, 7:8], ALU.subtract, ALU.mult)
        for r, st, junk in items:
            o.tt(r, r, gbc, ALU.mult)
        for r, st, junk in items:
            o.tt(r, r, bbc, ALU.add)

    def merge_outproj(self, l, s, moe_next):
        o = self.o
        C = self.C
        with self.scope() as sc:
            sb = sc.sbuf
            mT = V(sb("m_mT", [128, 8, S], BF16))
            with self.scope() as s1:
                wgo = [V(s1.sbuf("m_wgo%d" % i, [128, 4, 8, 128], BF16)) for i in range(2)]
                wbo = [V(s1.sbuf("m_wbo%d" % i, [128, 4, 4, 128], BF16)) for i in range(2)]
                acc = [V(s1.sbuf("m_acc%d" % i, [128, 512], F32)) for i in range(4)]
                gs = [V(s1.sbuf("m_gs%d" % i, [128, 512], F32)) for i in range(2)]
                tt_ = V(s1.sbuf("m_t", [128, 512], F32))
                it = 0
                for oc in range(8):
                    wg, wb = wgo[oc % 2], wbo[oc % 2]
                    for m in range(4):
                        self.wload(wg[:, m], self.w_in_cols(l, OFF["gate"] + m * 1024 + oc * 128, 128))
                        self.wload(wb[:, m], self.W["w_branch"][l, m][:, oc * 128:(oc + 1) * 128].rearrange("(c p) n -> p c n", p=128))
                    for m in range(4):
                        for tb in range(4):
                            blk = slice(tb * 512, (tb + 1) * 512)
                            pg = self.ps()
                            for k in range(8):
                                o.mm(pg, wg[:, m, k, :], self.xT[:, k, blk], start=(k == 0), stop=(k == 7))
                            g = gs[it % 2]
                            it += 1
                            o.act(g, pg, AF.Sigmoid)
                            pbr = self.ps()
                            for c in range(4):
                                o.mm(pbr, wb[:, m, c, :], self.yT[m][:, c, blk], start=(c == 0), stop=(c == 3))
                            if m == 0:
                                o.tt(acc[tb], g, pbr, ALU.mult)
                            else:
                                o.tt(tt_, g, pbr, ALU.mult)
                                o.tt(mT[:, oc, blk] if m == 3 else acc[tb], acc[tb], tt_, ALU.add)
            wout = V(sb("m_wout", [128, 8, D], BF16))
            self.wload(wout, self.W["w_out"][l].rearrange("(k p) n -> p k n", p=128))
            gbc = self.bc_row(sc, "m_gbc", self.W["ln_mix_g"][l])
            bbc = self.bc_row(sc, "m_bbc", self.W["ln_mix_b"][l])
            xr = [V(sb("m_xr%d" % i, [128, D], F32)) for i in range(2)]
            rr = [V(sb("m_rr%d" % i, [128, D], F32)) for i in range(2)]
            xb = [V(sb("m_xb%d" % i, [128, D], BF16)) for i in range(2)]
            junk2 = [V(sb("m_junk%d" % i, [128, D], BF16)) for i in range(2)]
            st2 = [V(sb("m_st%d" % i, [128, 8], F32)) for i in range(2)]
            src = self.x_in[s] if l == 0 else self.xres[s]
            for tp in range(0, NT, 2):
                for k_ in range(2):
                    ti = tp + k_
                    tl = slice(ti * 128, (ti + 1) * 128)
                    x_, r_ = xr[k_], rr[k_]
                    o.dma(x_, src[tl, :])
                    for half in range(2):
                        hs = slice(half * 512, (half + 1) * 512)
                        ps = self.ps()
                        for k in range(8):
                            o.mm(ps, mT[:, k, tl], wout[:, k, hs], start=(k == 0), stop=(k == 7))
                        o.stt(r_[:, hs], x_[:, hs], ALPHA, ps, ALU.mult, ALU.add)
                self.ln_tiles([(rr[0], st2[0], junk2[0]), (rr[1], st2[1], junk2[1])], gbc, bbc)
                for k_ in range(2):
                    ti = tp + k_
                    r_, b_ = rr[k_], xb[k_]
                    o.ts(self.xacc(ti), r_, ALPHA, None, ALU.mult)
                    o.copy(b_, r_, eng="act")
                    self.transpose_into_xT(b_, ti)

    def router(self):
        o = self.o
        with self.scope() as sc:
            wrb = [V(sc.sbuf("r_wrb%d" % i, [128, D], F32)) for i in range(2)]
            junk = V(sc.sbuf("r_junk", [128, D], F32))
            lgs = V(sc.sbuf("r_lgs", [128, NT, 8], F32))
            rt = V(sc.sbuf("r_rt", [128, 64], F32))
            o.memset(lgs, 0.0)
            for e in range(NEXP):
                w = wrb[e % 2]
                o.dma(w, self.W["moe_router_T"][0, e:e + 1, :].bcast([128, D]))
                for ti in range(NT):
                    o.stt(junk, self.xacc(ti), 1.0 / ALPHA, w, ALU.mult, ALU.mult, accum=lgs[:, ti, e:e + 1])
            for ti in range(NT):
                lg = lgs[:, ti, :]
                m1, m2, nm1, den = rt[:, 8:9], rt[:, 9:10], rt[:, 10:11], rt[:, 11:12]
                eq, l2, sel, ex = rt[:, 16:24], rt[:, 24:32], rt[:, 32:40], rt[:, 40:48]
                o.fw.emit("dve", lambda e_: e_.reduce_max(m1.ap, lg.ap, AX.X), reads=[lgs.tile], writes=[rt.tile])
                o.ts(eq, lg, m1, None, ALU.is_equal)
                o.stt(l2, eq, -1e30, lg, ALU.mult, ALU.add)
                o.fw.emit("dve", lambda e_: e_.reduce_max(m2.ap, l2.ap, AX.X), reads=[rt.tile], writes=[rt.tile])
                o.ts(sel, lg, m2, None, ALU.is_ge)
                o.ts(nm1, m1, -1.0, None, ALU.mult)
                o.act(ex, lg, AF.Exp, bias=nm1)
                o.tt(ex, ex, sel, ALU.mult)
                o.fw.emit("dve", lambda e_: e_.reduce_sum(den.ap, ex.ap, AX.X), reads=[rt.tile], writes=[rt.tile])
                o.recip(den, den)
                o.ts(self.gates[:, ti, :], ex, den, None, ALU.mult)

    def ffn_stage(self, l, s, last):
        o = self.o
        moe = (l % 2 == 1)
        j = l // 2
        if moe:
            self.router()
        with self.scope() as sc:
            sb = sc.sbuf
            hT = V(sb("f_hT", [128, 4, S], BF16))
            wg = [V(sb("f_wg%d" % i, [128, 8, 512], BF16)) for i in range(2)]
            wu = [V(sb("f_wu%d" % i, [128, 8, 512], BF16)) for i in range(2)]
            wd = [V(sb("f_wd%d" % i, [128, 4, D], BF16)) for i in range(2)]
            hs_ = [V(sb("f_hs%d" % i, [128, 512], F32)) for i in range(2)]
            if moe:
                jobs = [(e, fb, 4) for e in range(NEXP) for fb in range(D_FFE // 512)]
            else:
                jobs = [(None, fb, 4) for fb in range(D_FF // 512)] + [(None, D_FF // 512, (D_FF % 512) // 128)]
            it = 0
            if DBG.get("max_jobs") is not None:
                jobs = jobs[:DBG["max_jobs"]]
            for ji, (e, fb, nch) in enumerate(jobs):
                g_, u_, d_ = wg[ji % 2], wu[ji % 2], wd[ji % 2]
                f0 = fb * 512
                nf = nch * 128
                if moe:
                    Wg, Wu, Wd = self.W["moe_w_gate"][j, e], self.W["moe_w_up"][j, e], self.W["moe_w_down"][j, e]
                else:
                    Wg, Wu, Wd = self.W["ffn_w_gate"][j], self.W["ffn_w_up"][j], self.W["ffn_w_down"][j]
                self.wload(g_[:, :, 0:nf], Wg[:, f0:f0 + nf].rearrange("(k p) n -> p k n", p=128))
                self.wload(u_[:, :, 0:nf], Wu[:, f0:f0 + nf].rearrange("(k p) n -> p k n", p=128))
                self.wload(d_[:, 0:nch, :], Wd[f0:f0 + nf, :].rearrange("(c p) n -> p c n", p=128))
                for tb in range(4):
                    blk = slice(tb * 512, (tb + 1) * 512)
                    for ch in range(nch):
                        pg = self.ps()
                        for k in range(8):
                            o.mm(pg, g_[:, k, ch * 128:(ch + 1) * 128], self.xT[:, k, blk], start=(k == 0), stop=(k == 7))
                        pu = self.ps()
                        for k in range(8):
                            o.mm(pu, u_[:, k, ch * 128:(ch + 1) * 128], self.xT[:, k, blk], start=(k == 0), stop=(k == 7))
                        h_ = hs_[it % 2]
                        it += 1
                        o.act(h_, pg, AF.Silu)
                        o.tt(hT[:, ch, blk], h_, pu, ALU.mult)
                for ti in range(NT):
                    tl = slice(ti * 128, (ti + 1) * 128)
                    xa = self.xacc(ti)
                    for half in range(2):
                        hs = slice(half * 512, (half + 1) * 512)
                        ps = self.ps()
                        for ch in range(nch):
                            o.mm(ps, hT[:, ch, tl], d_[:, ch, hs], start=(ch == 0), stop=(ch == nch - 1))
                        if moe:
                            o.stt(xa[:, hs], ps, self.gates[:, ti, e:e + 1], xa[:, hs], ALU.mult, ALU.add)
                        else:
                            o.tt(xa[:, hs], xa[:, hs], ps, ALU.add)
            gbc = self.bc_row(sc, "f_gbc", self.W["ln_ffn_g"][l])
            bbc = self.bc_row(sc, "f_bbc", self.W["ln_ffn_b"][l])
            junk2 = [V(sb("f_junk%d" % i, [128, D], BF16)) for i in range(2)]
            st2 = [V(sb("f_st%d" % i, [128, 8], F32)) for i in range(2)]
            xb = [V(sb("f_xb%d" % i, [128, D], BF16)) for i in range(2)]
            dst = self.out[s] if last else self.xres[s]
            for g in range(2):
                for i in range(4):
                    pair = (g * 8 + i, g * 8 + i + 4)
                    self.ln_tiles([(self.xacc(pair[0]), st2[0], junk2[0]), (self.xacc(pair[1]), st2[1], junk2[1])],
                                  gbc, bbc)
                    for k_, ti in enumerate(pair):
                        tl = slice(ti * 128, (ti + 1) * 128)
                        xa = self.xacc(ti)
                        o.dma(dst[tl, :], xa)
                        if not last:
                            b_ = xb[k_]
                            o.copy(b_, xa, eng="act")
                            self.transpose_into_xT(b_, ti)

    def forward(self, depth=DEPTH, dbg_stop=None):
        for s in range(self.nseq):
            self.load_xT(self.x_in[s])
            for l in range(depth):
                on = DBG.get("l1") if (l == 1 and DBG.get("l1") is not None) else ("mla", "gdn", "pool", "hgrn", "merge", "ffn")
                for st in ("mla", "gdn", "pool", "hgrn", "merge", "ffn"):
                    if st not in on:
                        continue
                    MARKS.append(("s%d l%d %s" % (s, l, st), self.nc.get_next_instruction_name()))
                    if st == "mla":
                        self.mla_mixer(l, s)
                    elif st == "gdn":
                        self.gdn_mixer(l)
                    elif st == "pool":
                        self.pool_mixer(l)
                    elif st == "hgrn":
                        self.hgrn_mixer(l)
                    elif st == "merge":
                        self.merge_outproj(l, s, moe_next=False)
                    else:
                        self.ffn_stage(l, s, last=(l == depth - 1))
        MARKS.append(("end", self.nc.get_next_instruction_name()))
        self.fw.finish()


def build_full(nseq=NSEQ, depth=DEPTH):
    nc = bass.Bass("TRN2", target_bir_lowering=False)
    stack = ExitStack()
    with stack:
        k = KernFull(nc, stack, nseq=nseq)
        k.forward(depth)
        print("instructions:", k.fw.ninst, k.fw.per, "sems:", k.fw.nsem, "sbuf left:", nc.sbuf_bytes_remaining)
    return nc


_CACHE = {}


def kernel(**inputs):
    n_cores = 8
    if "nc" not in _CACHE:
        _CACHE["nc"] = build_full()
    nc = _CACHE["nc"]
    consts = make_consts()
    x = np.ascontiguousarray(inputs["x"], dtype=np.float32)
    pos = np.ascontiguousarray(inputs["positions"], dtype=np.int32)
    in_maps = []
    warr = {name: weight_array(inputs, name) for name, _ in WEIGHT_SPECS}
    for c in range(n_cores):
        m = {"x": x[c * NSEQ:(c + 1) * NSEQ], "positions": pos[c * NSEQ:(c + 1) * NSEQ]}
        for name, shp in WEIGHT_SPECS:
            m[name] = warr[name]
        for k_, v_ in consts.items():
            m["c_" + k_] = v_
        in_maps.append(m)
    res = run_bass_kernel_spmd(nc, in_maps, core_ids=list(range(n_cores)))
    out = np.concatenate([np.asarray(r["out"]) for r in res.results], axis=0)
    return out.astype(np.float32)


KERN_CLS = KernFull
```

```python
import math
from contextlib import ExitStack

import numpy as np
import concourse.bass as bass
import concourse.mybir as mybir
from concourse.bass_utils import run_bass_kernel_spmd

F32 = mybir.dt.float32
BF16 = mybir.dt.bfloat16
I32 = mybir.dt.int32
ALU = mybir.AluOpType
AF = mybir.ActivationFunctionType
AX = mybir.AxisListType

D = 1024
S = 2048
NSEQ = 2
DEPTH = 2
NT = S // 128
D_IN = 9160
OFF = dict(cq=0, ckv=256, kr=384, gq=448, gk=960, gv=1472, gz=1984, ga=2496, gb=2500, pu=2504,
           hq=3016, hf=3528, hi=4040, hg=4552, gate=5064)
D_FF = 2816
D_FFE = 3584
NEXP = 8
ALPHA = (2 * DEPTH) ** 0.25
LN_EPS = 1e-5
RMS_EPS = 1e-6
ATT_SCALE = (128 + 64) ** -0.5

ENGS = ("pe", "dve", "act", "pool", "sp")
DBG = {}
MARKS = []
SELF_NOSYNC = ()
EPOCH = 5000
NDMASEM = 8


class Res:
    __slots__ = ("name", "w", "r")

    def __init__(self, name):
        self.name = name
        self.w = None
        self.r = {}


class Tile:
    def __init__(self, t, name):
        self.t = t
        self.name = name
        self.res = Res(name)

    def __getitem__(self, idx):
        return self.t[idx]


class FW:
    def __init__(self, nc, stack):
        self.nc = nc
        self.stack = stack
        self.cnt = {e: 0 for e in ENGS}
        self.known = {e: {} for e in ENGS}
        self.semh = {}
        self.dma_rr = {e: 0 for e in ENGS}
        self.dma_use = {}
        self.nsem = 0
        self.last = {}
        self.eobj = {"pe": nc.tensor, "dve": nc.vector, "act": nc.scalar, "pool": nc.gpsimd, "sp": nc.sync}
        self.ninst = 0
        self.per = {}
        self.pending_inc = {}
        self.pe_mode = None
        self.pe_safe = False

    def sbuf(self, name, shape, dtype, stack=None):
        self.nuniq = getattr(self, "nuniq", 0) + 1
        name = "%s_%d" % (name, self.nuniq)
        t = (stack or self.stack).enter_context(self.nc.sbuf_tensor(name, list(shape), dtype))
        return Tile(t, name)

    def psum(self, name, shape, dtype=F32):
        t = self.stack.enter_context(self.nc.psum_tensor(name, list(shape), dtype))
        return Tile(t, name)

    def dram(self, name, shape, dtype, kind=None):
        if kind is None:
            t = self.nc.dram_tensor(name, list(shape), dtype)
        else:
            t = self.nc.dram_tensor(name, list(shape), dtype, kind=kind)
        return Tile(t, name)

    def _sem(self, key):
        h = self.semh.get(key)
        if h is None:
            h = self.stack.enter_context(self.nc.semaphore("s%d" % self.nsem))
            self.nsem += 1
            self.semh[key] = h
        return h

    def _need(self, eng, ev):
        if ev is None:
            return
        key, val = ev
        if key[0] == "c" and key[1] == eng and eng in SELF_NOSYNC:
            return
        if eng == "pe" and key[0] == "c" and key[1] == "pe" and not self.pe_safe:
            return
        if self.known[eng].get(key, 0) >= val:
            return
        self.known[eng][key] = val
        self.eobj[eng].wait_ge(self._sem(key), val)
        self.ninst += 1
        self.per[eng] = self.per.get(eng, 0) + 1

    def emit(self, eng, fn, reads=(), writes=(), dma=False, inc=True, pe_mode=None):
        rr = [x.res if isinstance(x, Tile) else x for x in reads]
        ww = [x.res if isinstance(x, Tile) else x for x in writes]
        if eng == "pe" and self.pe_safe:
            inc = True
        if pe_mode is not None:
            if False and pe_mode != self.pe_mode and self.cnt["pe"] > 0:
                assert not self.pending_inc.get("pe"), "tiling-mode switch inside an open PE group"
                c = self.cnt["pe"] - 1
                key = ("c", "pe", c // EPOCH)
                self.eobj["pe"].wait_ge(self._sem(key), c % EPOCH + 1)
                self.ninst += 1
                self.ndrain = getattr(self, "ndrain", 0) + 1
            self.pe_mode = pe_mode
        for r in rr:
            self._need(eng, r.w)
        for w in ww:
            self._need(eng, w.w)
            for k, v in list(w.r.items()):
                self._need(eng, (k, v))
        if dma:
            i = self.dma_rr[eng]
            self.dma_rr[eng] = (i + 1) % NDMASEM
            key = ("d", eng, i)
            used = self.dma_use.get(key, 0)
            if used:
                self._need(eng, (key, 16 * used))
            self.dma_use[key] = used + 1
            ev = (key, 16 * (used + 1))
            inc = 16
        else:
            c = self.cnt[eng]
            key = ("c", eng, c // EPOCH)
            ev = (key, c % EPOCH + 1)
            if inc:
                self.cnt[eng] = c + 1
            self.pending_inc[eng] = not inc
        if dma or inc:
            fn(self.eobj[eng]).then_inc(self._sem(key), 16 if dma else 1)
        else:
            fn(self.eobj[eng])
        self.ninst += 1
        self.per[eng] = self.per.get(eng, 0) + 1
        self.last[key] = ev
        for r in rr:
            if r.r.get(key, 0) < ev[1]:
                r.r[key] = ev[1]
        for w in ww:
            w.w = ev
            w.r = {}
        return ev

    def barrier(self):
        assert not self.pending_inc.get("pe"), "PE group left open at a barrier"
        evs = list(self.last.values())
        for ev in evs:
            self._need("sp", ev)
        ev = self.emit("sp", lambda e: e.nop())
        for eng in ENGS:
            if eng != "sp":
                self._need(eng, ev)

    def finish(self):
        for ev in list(self.last.values()):
            self._need("sp", ev)


def make_consts():
    c = {}
    c["ident"] = np.eye(128, dtype=np.float32)
    i = np.arange(128)
    c["m_causal"] = (i[None, :] >= i[:, None]).astype(np.float32)
    blk64 = (i[:, None] // 64) == (i[None, :] // 64)
    blk32 = (i[:, None] // 32) == (i[None, :] // 32)
    c["m_low64"] = (blk64 & (i[None, :] <= i[:, None])).astype(np.float32)
    c["m_slow64"] = (blk64 & (i[None, :] < i[:, None])).astype(np.float32)
    c["m_up64"] = (blk64 & (i[None, :] >= i[:, None])).astype(np.float32)
    c["m_up32"] = (blk32 & (i[None, :] >= i[:, None])).astype(np.float32)
    c["m_chunk32"] = ((i[:, None] // 32) == np.arange(4)[None, :]).astype(np.float32)
    c["m_chunk64"] = ((i[:, None] // 64) == np.arange(2)[None, :]).astype(np.float32)
    t = np.arange(S)
    c["scan32"] = np.broadcast_to((t % 32 != 0).astype(np.float32)[None, :], (128, S)).copy()
    c["scan64"] = np.broadcast_to((t % 64 != 0).astype(np.float32)[None, :], (128, S)).copy()
    half = 32
    inv = (10000.0 ** (-np.arange(half, dtype=np.float32) / half)).astype(np.float32)
    rp = np.zeros((128, 4), np.float32)
    rp[:64, 0] = np.concatenate([inv, inv])
    rp[:64, 1] = np.concatenate([-np.ones(32), np.ones(32)])
    c["ropep"] = rp
    ic = np.zeros((128, 4, 16), np.float32)
    for gi, win in enumerate((2, 4, 8, 16)):
        ic[:, gi, :] = 1.0 / np.minimum(np.arange(16) + 1, win)
    c["invcnt"] = ic.reshape(128, 64)
    sel = np.zeros((128, 4, 128), np.float32)
    for h in range(4):
        sel[h, h, :] = 1.0
    c["sel4"] = sel.reshape(128, 512)
    c["ones"] = np.ones((128, 128), np.float32)
    selx = np.zeros((128, 4, 128), np.float32)
    for base in (0, 32, 64):
        for h in range(4):
            selx[base + h, h, :] = 1.0
    c["sel4x"] = selx.reshape(128, 512)
    c["m_sup64"] = (blk64 & (i[None, :] > i[:, None])).astype(np.float32)
    return c


CONST_SHAPES = {k: v.shape for k, v in make_consts().items()}


class View:
    __slots__ = ("tile", "ap")

    def __init__(self, tile, ap):
        self.tile = tile
        self.ap = ap

    def __getitem__(self, idx):
        return View(self.tile, self.ap[idx])

    def bitcast(self, dt):
        return View(self.tile, self.ap.bitcast(dt))

    def rearrange(self, s, **kw):
        return View(self.tile, self.ap.rearrange(s, **kw))

    def bcast(self, shape):
        return View(self.tile, self.ap.broadcast_to(list(shape)))


def V(tile):
    return View(tile, tile.t.ap() if hasattr(tile.t, "ap") and callable(tile.t.ap) else tile.t[:])


def _ap(x):
    return x.ap if isinstance(x, View) else x


def _tiles(*xs):
    return [x.tile for x in xs if isinstance(x, View)]


def _r32(n):
    return 32 if n <= 32 else (64 if n <= 64 else 128)


def _pe_mode(ap, is_tr):
    shp = list(ap.shape)
    k = shp[0]
    m = 1
    for d in shp[1:]:
        m *= d
    return (_r32(k), _r32(m), str(ap.dtype) == str(F32), is_tr and str(ap.dtype) == str(F32))


class Ops:
    def __init__(self, fw):
        self.fw = fw

    def mm(self, out, lhsT, rhs, start=True, stop=True, inc=None):
        if inc is None:
            inc = stop
        return self.fw.emit("pe", lambda e: e.matmul(out.ap, lhsT.ap, rhs.ap, start=start, stop=stop),
                            reads=_tiles(lhsT, rhs), writes=_tiles(out), inc=inc, pe_mode=_pe_mode(lhsT.ap, False))

    def tr(self, out, in_, ident, inc=True):
        return self.fw.emit("pe", lambda e: e.transpose(out.ap, in_.ap, ident.ap),
                            reads=_tiles(in_, ident), writes=_tiles(out), inc=inc, pe_mode=_pe_mode(in_.ap, True))

    def act(self, out, in_, func, bias=None, scale=None, accum=None):
        kw = {}
        if bias is not None:
            kw["bias"] = _ap(bias)
        if scale is not None:
            kw["scale"] = _ap(scale)
        if accum is not None:
            kw["accum_out"] = accum.ap
        return self.fw.emit("act", lambda e: e.activation(out.ap, in_.ap, func, **kw),
                            reads=_tiles(in_, bias, scale), writes=_tiles(out, accum))

    def ts(self, out, in0, s1, s2, op0, op1=None, eng="dve", accum=None):
        kw = {}
        if op1 is not None:
            kw["op1"] = op1
        if accum is not None:
            kw["accum_out"] = accum.ap
        return self.fw.emit(eng, lambda e: e.tensor_scalar(out.ap, in0.ap, _ap(s1), _ap(s2), op0, **kw),
                            reads=_tiles(in0, s1, s2), writes=_tiles(out, accum))

    def tt(self, out, a, b, op, eng="dve"):
        return self.fw.emit(eng, lambda e: e.tensor_tensor(out.ap, a.ap, b.ap, op),
                            reads=_tiles(a, b), writes=_tiles(out))

    def stt(self, out, in0, scalar, in1, op0, op1, accum=None):
        kw = {}
        if accum is not None:
            kw["accum_out"] = accum.ap
        return self.fw.emit("dve", lambda e: e.scalar_tensor_tensor(out.ap, in0.ap, _ap(scalar), in1.ap, op0, op1, **kw),
                            reads=_tiles(in0, scalar, in1), writes=_tiles(out, accum))

    def copy(self, out, in_, eng="dve"):
        if eng == "act":
            return self.act(out, in_, AF.Copy)
        return self.fw.emit(eng, lambda e: e.tensor_copy(out.ap, in_.ap), reads=_tiles(in_), writes=_tiles(out))

    def recip(self, out, in_):
        return self.fw.emit("dve", lambda e: e.reciprocal(out.ap, in_.ap), reads=_tiles(in_), writes=_tiles(out))

    def scan(self, out, d0, d1, init, op0, op1):
        return self.fw.emit("dve", lambda e: e.tensor_tensor_scan(out.ap, d0.ap, d1.ap, _ap(init), op0, op1),
                            reads=_tiles(d0, d1, init), writes=_tiles(out))

    def memset(self, out, val, eng="dve"):
        return self.fw.emit(eng, lambda e: e.memset(out.ap, val), writes=_tiles(out))

    def dma(self, out, in_, eng="sp", slow=False):
        kw = {"allow_slow_non_contiguous": True} if slow else {}
        return self.fw.emit(eng, lambda e: e.dma_start(out=out.ap, in_=in_.ap, **kw),
                            reads=_tiles(in_), writes=_tiles(out), dma=True)

    def wrap(self, out, in_, shift):
        return self.fw.emit("dve", lambda e: e.add_range_wrap(out.ap, in_.ap, shift, math.pi, 2 * math.pi),
                            reads=_tiles(in_), writes=_tiles(out))


def V(tile):
    return View(tile, tile.t[:])


WEIGHT_SPECS = [
    ("w_in", (2, 1024, 9160)), ("mla_q_norm", (2, 256)), ("mla_w_uq", (2, 256, 768)),
    ("mla_kv_norm", (2, 128)), ("mla_w_ukv", (2, 128, 1024)), ("gdn_conv", (2, 4, 1536)),
    ("gdn_a_log", (2, 4)), ("gdn_dt_bias", (2, 4)), ("gdn_norm", (2, 128)), ("pool_w", (2, 4, 128, 128)),
    ("pool_scale", (2, 512)), ("hgrn_lb_logits", (2, 512)), ("hgrn_norm", (2, 128)),
    ("w_branch", (2, 4, 512, 1024)), ("w_out", (2, 1024, 1024)), ("ln_mix_g", (2, 1024)),
    ("ln_mix_b", (2, 1024)), ("ffn_w_gate", (1, 1024, 2816)), ("ffn_w_up", (1, 1024, 2816)),
    ("ffn_w_down", (1, 2816, 1024)), ("moe_router", (1, 1024, 8)), ("moe_w_gate", (1, 8, 1024, 3584)),
    ("moe_w_up", (1, 8, 1024, 3584)), ("moe_w_down", (1, 8, 3584, 1024)), ("ln_ffn_g", (2, 1024)),
    ("ln_ffn_b", (2, 1024)), ("moe_router_T", (1, 8, 1024)),
]


def weight_array(inputs, name):
    if name == "moe_router_T":
        return np.ascontiguousarray(np.asarray(inputs["moe_router"], dtype=np.float32).transpose(0, 2, 1))
    return np.ascontiguousarray(inputs[name], dtype=np.float32)


class Kern:
    def __init__(self, nc, stack, nseq=NSEQ, dbg=None):
        self.nc = nc
        self.fw = fw = FW(nc, stack)
        self.o = Ops(fw)
        self.nseq = nseq
        self.dbg = dbg or {}
        self.x_in = V(fw.dram("x", [nseq, S, D], F32, kind="ExternalInput"))
        self.pos = V(fw.dram("positions", [nseq, S], I32, kind="ExternalInput"))
        self.W = {}
        for name, shp in WEIGHT_SPECS:
            self.W[name] = V(fw.dram(name, list(shp), F32, kind="ExternalInput"))
        self.Cd = {}
        for name, shp in CONST_SHAPES.items():
            self.Cd[name] = V(fw.dram("c_" + name, list(shp), F32, kind="ExternalInput"))
        self.out = V(fw.dram("out", [nseq, S, D], F32, kind="ExternalOutput"))
        self.xres = V(fw.dram("xres", [nseq, S, D], F32))
        self.pb = [V(fw.psum("pb%d" % i, [128, 512], F32)) for i in range(8)]
        self.prr = 0
        sb = fw.sbuf
        o = self.o
        C = self.C = {}
        for name in ("ident", "m_causal", "m_up64", "m_sup64", "m_up32", "ones"):
            C[name] = V(sb("k_" + name, [128, 128], F32))
            o.dma(C[name], self.Cd[name])
        for name, w in (("m_chunk32", 4), ("m_chunk64", 2), ("ropep", 4), ("invcnt", 64)):
            C[name] = V(sb("k_" + name, [128, w], F32))
            o.dma(C[name], self.Cd[name])
        C["scan32"] = V(sb("k_scan32", [128, S], F32))
        o.dma(C["scan32"], self.Cd["scan32"])
        C["scan64x"] = V(sb("k_scan64x", [68, S], F32))
        o.dma(C["scan64x"], self.Cd["scan64"][0:68, :])
        C["sel4x"] = V(sb("k_sel4x", [68, 512], F32))
        o.dma(C["sel4x"], self.Cd["sel4x"][0:68, :])
        C["ident_b"] = V(sb("k_ident_b", [128, 128], BF16))
        o.copy(C["ident_b"], C["ident"])
        C["ones_b"] = V(sb("k_ones_b", [128, 128], BF16))
        o.copy(C["ones_b"], C["ones"])
        C["causal_b"] = V(sb("k_causal_b", [128, 128], BF16))
        o.copy(C["causal_b"], C["m_causal"])
        self.gates = V(sb("moe_gates", [128, NT, 8], F32))
        self.xT = V(sb("xT", [128, 8, S], BF16))
        self.yT = [V(sb("yT%d" % m, [128, 4, S], BF16)) for m in range(4)]

    def ps(self, lo=0, hi=5):
        n = hi - lo
        b = self.pb[lo + self.prr % n]
        self.prr += 1
        return b

    def scope(self):
        return _Scope(self)

    def load_xT(self, src):
        o = self.o
        with self.scope() as sc:
            xin = [V(sc.sbuf("xa_in%d" % i, [128, D], F32)) for i in range(2)]
            xb = [V(sc.sbuf("xa_b%d" % i, [128, D], BF16)) for i in range(2)]
            for ti in range(NT):
                a, b = xin[ti % 2], xb[ti % 2]
                o.dma(a, src[ti * 128:(ti + 1) * 128, :])
                o.copy(b, a, eng="act")
                self.transpose_into_xT(b, ti)

    def transpose_into_xT(self, xb, ti):
        o = self.o
        p = self.ps().bitcast(BF16)
        for k in range(8):
            o.tr(p[:, k * 128:(k + 1) * 128], xb[:, k * 128:(k + 1) * 128], self.C["ident_b"], inc=(k == 7))
        o.copy(self.xT[:, :, ti * 128:(ti + 1) * 128], p.rearrange("p (k t) -> p k t", k=8))

    def wload(self, dst, src):
        return self.o.dma(dst, src, eng="pool")

    def w_in_cols(self, l, c0, n):
        return self.W["w_in"][l, :, c0:c0 + n].rearrange("(k p) n -> p k n", p=128)

    def proj_fm(self, wt, ncols, cb, blocks=range(4), bw=512):
        o = self.o
        for tb in blocks:
            ps = self.ps()
            for k in range(8):
                o.mm(ps[0:ncols, 0:bw], wt[:, k, 0:ncols], self.xT[:, k, tb * bw:(tb + 1) * bw],
                     start=(k == 0), stop=(k == 7))
            cb(ps[0:ncols, 0:bw], tb)

    def proj_tm(self, wt, ncols, cb, tiles=range(NT)):
        o = self.o
        for ti in tiles:
            ps = self.ps()
            for k in range(8):
                o.mm(ps[:, 0:ncols], self.xT[:, k, ti * 128:(ti + 1) * 128], wt[:, k, 0:ncols],
                     start=(k == 0), stop=(k == 7))
            cb(ps[:, 0:ncols], ti)

    def gated_norm_out(self, sc_tiles, oT, wg, ng_col, func, dst):
        o = self.o
        sets = [sc_tiles[i:i + 4] for i in range(0, len(sc_tiles), 4)]
        n = len(sets)
        for tb0 in range(0, 4, n):
            items = [(sets[i], slice((tb0 + i) * 512, (tb0 + i + 1) * 512)) for i in range(n)]
            for (sq, rs, gs, tmp), blk in items:
                o.act(sq, oT[:, blk], AF.Square)
            pss = []
            for (sq, rs, gs, tmp), blk in items:
                p = self.ps()
                o.mm(p, self.C["ones_b"], sq)
                pss.append(p)
            pgs = []
            for (sq, rs, gs, tmp), blk in items:
                pg = self.ps()
                for k in range(8):
                    o.mm(pg, wg[:, k, :], self.xT[:, k, blk], start=(k == 0), stop=(k == 7))
                pgs.append(pg)
            for i, ((sq, rs, gs, tmp), blk) in enumerate(items):
                o.act(rs, pss[i], AF.Sqrt, bias=RMS_EPS, scale=1.0 / 128)
            for i, ((sq, rs, gs, tmp), blk) in enumerate(items):
                o.act(gs, pgs[i], func)
            for (sq, rs, gs, tmp), blk in items:
                o.recip(rs, rs)
            for (sq, rs, gs, tmp), blk in items:
                o.stt(tmp, oT[:, blk], ng_col, rs, ALU.mult, ALU.mult)
            for (sq, rs, gs, tmp), blk in items:
                o.tt(dst[:, blk], tmp, gs, ALU.mult)


class _Scope:
    def __init__(self, kern):
        self.k = kern
        self.stack = ExitStack()

    def __enter__(self):
        self.stack.__enter__()
        return self

    def sbuf(self, name, shape, dtype):
        return self.k.fw.sbuf(name, shape, dtype, stack=self.stack)

    def __exit__(self, *a):
        if a[0] is None:
            self.k.fw.barrier()
        return self.stack.__exit__(*a)


class KernMix(Kern):
    def pool_mixer(self, l):
        o = self.o
        C = self.C
        with self.scope() as sc:
            wt = V(sc.sbuf("pl_w", [128, 8, 512], BF16))
            self.wload(wt, self.w_in_cols(l, OFF["pu"], 512))
            pw = V(sc.sbuf("pl_pw", [128, 4, 128], BF16))
            self.wload(pw, self.W["pool_w"][l].rearrange("g c d -> c g d"))
            psc = V(sc.sbuf("pl_sc", [128, 4], F32))
            o.dma(psc, self.W["pool_scale"][l].rearrange("(g p) -> p g", p=128), slow=True)
            u = V(sc.sbuf("pl_u", [128, 4, S], F32))
            tmp = [V(sc.sbuf("pl_t%d" % i, [128, S], F32)) for i in range(2)]
            pooled = V(sc.sbuf("pl_pooled", [128, S], BF16))
            t16 = V(sc.sbuf("pl_t16", [128, 16], F32))
            for gi in range(4):
                self.proj_fm(wt[:, :, gi * 128:(gi + 1) * 128], 128,
                             lambda ps, tb, gi=gi: o.copy(u[:, gi, tb * 512:(tb + 1) * 512], ps, eng="act"))
            for gi in range(4):
                win = 2 ** (gi + 1)
                cur = u[:, gi, :]
                sh = 1
                for step in range(gi + 1):
                    nxt = tmp[step % 2]
                    o.tt(nxt[:, sh:S], cur[:, sh:S], cur[:, 0:S - sh], ALU.add)
                    o.copy(nxt[:, 0:sh], cur[:, 0:sh])
                    cur = nxt
                    sh *= 2
                o.stt(pooled[:, :], cur, 1.0 / win, u[:, gi, :], ALU.mult, ALU.subtract)
                o.tt(t16, cur[:, 0:16], C["invcnt"][:, gi * 16:(gi + 1) * 16], ALU.mult)
                o.tt(pooled[:, 0:16], t16, u[:, gi, 0:16], ALU.subtract)
                for tb in range(4):
                    blk = slice(tb * 512, (tb + 1) * 512)
                    p = self.ps()
                    o.mm(p, pw[:, gi, :], pooled[:, blk])
                    o.ts(self.yT[2][:, gi, blk], p, psc[:, gi:gi + 1], None, ALU.mult)

    def hgrn_prep(self, l):
        o = self.o
        if hasattr(self, "_lb"):
            return
        sb = self.fw.sbuf
        lg = V(sb("hg_lg", [128, 2, 4], F32))
        o.dma(lg, self.W["hgrn_lb_logits"].rearrange("l (h p) -> p l h", p=128), slow=True)
        lb = V(sb("hg_lb", [128, 2, 4], F32))
        oml = V(sb("hg_oml", [128, 2, 4], F32))
        noml = V(sb("hg_noml", [128, 2, 4], F32))
        d = V(sb("hg_d", [128, 4], F32))
        o.tt(d, lg[:, 1, :], lg[:, 0, :], ALU.subtract)
        o.memset(lb[:, 0, :], 0.0)
        o.act(lb[:, 1, :], d, AF.Sigmoid)
        o.ts(oml, lb, -1.0, 1.0, ALU.mult, ALU.add)
        o.ts(noml, oml, -1.0, None, ALU.mult)
        self._lb, self._oml, self._noml = lb, oml, noml
        ng = V(sb("hg_ng", [128, 2], F32))
        o.dma(ng, self.W["hgrn_norm"].rearrange("l p -> p l"), slow=True)
        self._hng = ng

    def hgrn_mixer(self, l):
        o = self.o
        C = self.C
        self.hgrn_prep(l)
        for h in range(4):
            with self.scope() as sc:
                sb = sc.sbuf
                wq = V(sb("h_wq", [128, 8, 128], BF16))
                wf = V(sb("h_wf", [128, 8, 128], BF16))
                wi = V(sb("h_wi", [128, 8, 128], BF16))
                wg = V(sb("h_wg", [128, 8, 128], BF16))
                self.wload(wq, self.w_in_cols(l, OFF["hq"] + h * 128, 128))
                self.wload(wf, self.w_in_cols(l, OFF["hf"] + h * 128, 128))
                self.wload(wi, self.w_in_cols(l, OFF["hi"] + h * 128, 128))
                self.wload(wg, self.w_in_cols(l, OFF["hg"] + h * 128, 128))
                lbc = self._lb[:, l, h:h + 1]
                omlc = self._oml[:, l, h:h + 1]
                nomlc = self._noml[:, l, h:h + 1]
                q0T = V(sb("h_q0T", [128, S], BF16))
                qmT = V(sb("h_qmT", [128, S], BF16))
                kmT = V(sb("h_kmT", [128, S], BF16))
                khT = V(sb("h_khT", [128, S], BF16))
                vsb = V(sb("h_v", [128, NT, 128], BF16))
                oT = V(sb("h_oT", [128, S], F32))
                eb = V(sb("h_eb", [128, 64], F32))
                with self.scope() as s1:
                    s1b = s1.sbuf
                    qs = V(s1b("h_qs", [128, S], F32))
                    kT = V(s1b("h_kT", [128, S], F32))
                    lf = V(s1b("h_lf", [128, S], F32))
                    b = V(s1b("h_b", [128, S], F32))
                    e = V(s1b("h_e", [128, S], F32))
                    self.proj_fm(wq, 128, lambda ps, tb: o.act(qs[:, tb * 512:(tb + 1) * 512], ps, AF.Silu))

                    def f_cb(ps, tb):
                        blk = slice(tb * 512, (tb + 1) * 512)
                        o.act(e[:, blk], ps, AF.Sigmoid)
                        o.ts(lf[:, blk], e[:, blk], omlc, lbc, ALU.mult, ALU.add)
                        o.ts(kT[:, blk], e[:, blk], nomlc, omlc, ALU.mult, ALU.add)
                        o.act(lf[:, blk], lf[:, blk], AF.Ln)
                    self.proj_fm(wf, 128, f_cb)
                    self.proj_tm(wi, 128, lambda ps, ti: o.copy(vsb[:, ti, :], ps, eng="act"))
                    o.scan(b, C["scan32"], lf, 0.0, ALU.mult, ALU.add)
                    b3 = b.rearrange("p (c t) -> p c t", t=32)
                    o.act(e, b, AF.Exp)
                    e3 = e.rearrange("p (c t) -> p c t", t=32)
                    o.copy(eb, e3[:, :, 31])
                    o.tt(q0T, qs, e, ALU.mult)
                    lf3 = lf.rearrange("p (c t) -> p c t", t=32)
                    o.tt(lf3, b3, b3[:, :, 15:16].bcast([128, 64, 32]), ALU.subtract)
                    o.act(e, lf, AF.Exp)
                    o.tt(qmT, qs, e, ALU.mult)
                    o.act(e, lf, AF.Exp, scale=-1.0)
                    o.tt(kmT, kT, e, ALU.mult)
                    o.tt(lf3, b3, b3[:, :, 31:32].bcast([128, 64, 32]), ALU.subtract)
                    o.act(e, lf, AF.Exp, scale=-1.0)
                    o.tt(khT, kT, e, ALU.mult)
                att = V(sb("h_att", [128, NT, 128], BF16))
                khm = V(sb("h_khm", [128, NT, 4, 128], BF16))
                Sts = [V(sb("h_S%d" % i, [128, 128], F32)) for i in range(2)]
                Sb = [V(sb("h_Sb%d" % i, [128, 128], BF16)) for i in range(2)]
                g_sq = V(sb("h_gsq", [128, 512], BF16))
                g_rs = V(sb("h_grs", [128, 512], F32))
                g_gs = V(sb("h_ggs", [128, 512], F32))
                g_tmp = V(sb("h_gtmp", [128, 512], F32))
                g2 = (V(sb("h_gsq2", [128, 512], BF16)), V(sb("h_grs2", [128, 512], F32)),
                      V(sb("h_ggs2", [128, 512], F32)), V(sb("h_gtmp2", [128, 512], F32)))
                for ti in range(NT):
                    tl = slice(ti * 128, (ti + 1) * 128)
                    p = self.ps(0, 4)
                    o.mm(p[:, 0:128], kmT[:, tl], qmT[:, tl])
                    o.tt(att[:, ti, :], p[:, 0:128], C["m_up32"], ALU.mult)
                    pt = self.ps(0, 4).bitcast(BF16)
                    o.tr(pt[:, 0:128], khT[:, tl], C["ident_b"])
                    for c in range(4):
                        o.act(khm[:, ti, c, :], pt[:, 0:128], AF.Copy, scale=C["m_chunk32"][:, c:c + 1])
                o.memset(Sts[0], 0.0)
                o.memset(Sb[0], 0.0)
                cur = 0
                pOs = [self.pb[4], self.pb[5]]
                pSs = [self.pb[6], self.pb[7]]
                def emit_pS(ti):
                    for c in range(4):
                        o.mm(pSs[ti % 2][:, c * 128:(c + 1) * 128], khm[:, ti, c, :], vsb[:, ti, :])

                emit_pS(0)
                for ti in range(NT):
                    tl = slice(ti * 128, (ti + 1) * 128)
                    pO, pS = pOs[ti % 2], pSs[ti % 2]
                    if ti + 1 < NT:
                        emit_pS(ti + 1)
                    o.mm(pO[:, 0:128], vsb[:, ti, :], att[:, ti, :], start=True, stop=False)
                    for c in range(4):
                        cs = slice(ti * 128 + c * 32, ti * 128 + (c + 1) * 32)
                        o.mm(pO[:, c * 32:(c + 1) * 32], Sb[cur], q0T[:, cs], start=False, stop=(c == 3), inc=True)
                        o.stt(Sts[1 - cur], Sts[cur], eb[:, ti * 4 + c:ti * 4 + c + 1], pS[:, c * 128:(c + 1) * 128], ALU.mult, ALU.add)
                        cur = 1 - cur
                        o.copy(Sb[cur], Sts[cur], eng="act")
                    o.copy(oT[:, tl], pO[:, 0:128], eng="act")
                self.gated_norm_out((g_sq, g_rs, g_gs, g_tmp) + g2, oT, wg, self._hng[:, l:l + 1], AF.Sigmoid,
                                    self.yT[3][:, h, :])


class KernFull0(KernMix):
    pass


def build_debug(stages, nseq=1):
    nc = bass.Bass("TRN2", target_bir_lowering=False)
    stack = ExitStack()
    with stack:
        k = KERN_CLS(nc, stack, nseq=nseq)
        o = k.o
        dump = V(k.fw.dram("dump", [4, 128, 4, S], BF16, kind="ExternalOutput"))
        l = stages.get("layer", 0)
        k.load_xT(k.x_in[0])
        for m, name in enumerate(("mla", "gdn", "pool", "hgrn")):
            if name in stages["mixers"]:
                getattr(k, name + "_mixer")(l, 0) if name == "mla" else getattr(k, name + "_mixer")(l)
                o.dma(dump[m], k.yT[m])
        k.fw.finish()
        print("instructions:", k.fw.ninst, "sems:", k.fw.nsem, "sbuf left:", nc.sbuf_bytes_remaining)
    return nc


class KernMLA(KernMix):
    def rope_tables(self, sc, s):
        o = self.o
        C = self.C
        cosT = V(sc.sbuf("rp_cos", [64, S], BF16))
        sinT = V(sc.sbuf("rp_sin", [64, S], BF16))
        with self.scope() as s2:
            posi = V(s2.sbuf("rp_pi", [64, S], I32))
            ang = V(s2.sbuf("rp_ang", [64, S], F32))
            t = V(s2.sbuf("rp_t", [64, S], F32))
            ki = V(s2.sbuf("rp_ki", [64, S], I32))
            ang2 = V(s2.sbuf("rp_ang2", [64, S], F32))
            o.dma(posi, self.pos[s:s + 1, :].bcast([64, S]))
            o.copy(ang, posi)
            o.ts(ang, ang, C["ropep"][0:64, 0:1], None, ALU.mult)
            for shift, dst, scale in ((0.0, sinT, C["ropep"][0:64, 1:2]), (math.pi / 2, cosT, None)):
                o.ts(t, ang, shift, 1.0 / (2 * math.pi), ALU.add, ALU.mult)
                o.copy(ki, t)
                o.copy(t, ki)
                o.stt(t, t, -2 * math.pi, ang, ALU.mult, ALU.add)
                if shift != 0.0:
                    o.ts(t, t, shift, None, ALU.add)
                o.ts(ang2, t, math.pi, 2 * math.pi, ALU.is_gt, ALU.mult)
                o.tt(t, t, ang2, ALU.subtract)
                o.ts(ang2, t, -math.pi, 2 * math.pi, ALU.is_lt, ALU.mult)
                o.tt(t, t, ang2, ALU.add)
                o.ts(t, t, 3.141592, -3.141592, ALU.min, ALU.max)
                o.act(dst, t, AF.Sin, scale=scale)
        return cosT, sinT

    def mla_mixer(self, l, s):
        o = self.o
        C = self.C
        with self.scope() as sc:
            sb = sc.sbuf
            cosT, sinT = self.rope_tables(sc, s)
            wcq = V(sb("a_wcq", [128, 8, 256], BF16))
            wckv = V(sb("a_wckv", [128, 8, 128], BF16))
            wkrA = V(sb("a_wkrA", [128, 8, 64], BF16))
            wkrB = V(sb("a_wkrB", [128, 8, 64], BF16))
            wuq = V(sb("a_wuq", [128, 2, 768], BF16))
            wuqB = V(sb("a_wuqB", [128, 2, 4, 64], BF16))
            wukv = V(sb("a_wukv", [128, 1024], BF16))
            gq = V(sb("a_gq", [128, 2], F32))
            gkv = V(sb("a_gkv", [128, 1], F32))
            self.wload(wcq, self.w_in_cols(l, OFF["cq"], 256))
            self.wload(wckv, self.w_in_cols(l, OFF["ckv"], 128))
            self.wload(wkrA, self.w_in_cols(l, OFF["kr"], 64))
            self.wload(wkrB[:, :, 0:32], self.w_in_cols(l, OFF["kr"] + 32, 32))
            self.wload(wkrB[:, :, 32:64], self.w_in_cols(l, OFF["kr"], 32))
            uq = self.W["mla_w_uq"][l].rearrange("(c p) n -> p c n", p=128)
            self.wload(wuq, uq)
            for h in range(4):
                self.wload(wuqB[:, :, h, 0:32], uq[:, :, h * 192 + 160:h * 192 + 192])
                self.wload(wuqB[:, :, h, 32:64], uq[:, :, h * 192 + 128:h * 192 + 160])
            self.wload(wukv, self.W["mla_w_ukv"][l])
            o.dma(gq, self.W["mla_q_norm"][l].rearrange("(c p) -> p c", p=128), slow=True)
            o.dma(gkv, self.W["mla_kv_norm"][l].rearrange("(p o) -> p o", o=1), slow=True)
            cqn = V(sb("a_cqn", [128, 2, S], BF16))
            ckvn = V(sb("a_ckvn", [128, S], BF16))
            krT = V(sb("a_krT", [64, S], BF16))
            cqf = V(sb("a_cqf", [128, 2, 512], F32))
            sq = V(sb("a_sq", [128, 2, 512], BF16))
            rq = V(sb("a_rq", [128, 512], F32))
            t1 = V(sb("a_t1", [64, 512], F32))
            t2 = V(sb("a_t2", [64, 512], F32))

            def rope_combine(dst, psA, psB, blk):
                o.tt(t1, psA, cosT[:, blk], ALU.mult)
                o.tt(t2, psB, sinT[:, blk], ALU.mult)
                o.tt(dst, t1, t2, ALU.add)

            for tb in range(4):
                blk = slice(tb * 512, (tb + 1) * 512)
                for c in range(2):
                    ps = self.ps()
                    for k in range(8):
                        o.mm(ps, wcq[:, k, c * 128:(c + 1) * 128], self.xT[:, k, blk], start=(k == 0), stop=(k == 7))
                    o.copy(cqf[:, c, :], ps, eng="act")
                    o.act(sq[:, c, :], ps, AF.Square)
                pss = self.ps()
                for c in range(2):
                    o.mm(pss, C["ones_b"], sq[:, c, :], start=(c == 0), stop=(c == 1))
                o.act(rq, pss, AF.Sqrt, bias=RMS_EPS, scale=1.0 / 256)
                o.recip(rq, rq)
                for c in range(2):
                    o.stt(cqn[:, c, blk], cqf[:, c, :], gq[:, c:c + 1], rq, ALU.mult, ALU.mult)
                ps = self.ps()
                for k in range(8):
                    o.mm(ps, wckv[:, k, :], self.xT[:, k, blk], start=(k == 0), stop=(k == 7))
                o.copy(cqf[:, 0, :], ps, eng="act")
                o.act(sq[:, 0, :], ps, AF.Square)
                pss = self.ps()
                o.mm(pss, C["ones_b"], sq[:, 0, :])
                o.act(rq, pss, AF.Sqrt, bias=RMS_EPS, scale=1.0 / 128)
                o.recip(rq, rq)
                o.stt(ckvn[:, blk], cqf[:, 0, :], gkv[:, 0:1], rq, ALU.mult, ALU.mult)
                psA = self.ps()
                psB = self.ps()
                for k in range(8):
                    o.mm(psA[0:64, :], wkrA[:, k, :], self.xT[:, k, blk], start=(k == 0), stop=(k == 7))
                for k in range(8):
                    o.mm(psB[0:64, :], wkrB[:, k, :], self.xT[:, k, blk], start=(k == 0), stop=(k == 7))
                rope_combine(krT[:, blk], psA[0:64, :], psB[0:64, :], blk)

            qnT = V(sb("a_qnT", [128, S], BF16))
            qrT = V(sb("a_qrT", [64, S], BF16))
            knT = V(sb("a_knT", [128, S], BF16))
            vsb = V(sb("a_v", [128, NT, 128], BF16))
            pts = [V(sb("a_pt%d" % i, [128, 512], BF16)) for i in range(3)]
            rd = V(sb("a_rd", [128, 512], F32))
            pO = self.pb[5]
            pD = self.pb[6]
            for h in range(4):
                for tb in range(4):
                    blk = slice(tb * 512, (tb + 1) * 512)
                    ps = self.ps()
                    for c in range(2):
                        o.mm(ps, wuq[:, c, h * 192:h * 192 + 128], cqn[:, c, blk], start=(c == 0), stop=(c == 1))
                    o.copy(qnT[:, blk], ps, eng="act")
                    psA = self.ps()
                    psB = self.ps()
                    for c in range(2):
                        o.mm(psA[0:64, :], wuq[:, c, h * 192 + 128:h * 192 + 192], cqn[:, c, blk], start=(c == 0), stop=(c == 1))
                    for c in range(2):
                        o.mm(psB[0:64, :], wuqB[:, c, h, :], cqn[:, c, blk], start=(c == 0), stop=(c == 1))
                    rope_combine(qrT[:, blk], psA[0:64, :], psB[0:64, :], blk)
                    ps = self.ps()
                    o.mm(ps, wukv[:, h * 256:h * 256 + 128], ckvn[:, blk])
                    o.copy(knT[:, blk], ps, eng="act")
                for ti in range(NT):
                    ps = self.ps()
                    o.mm(ps[:, 0:128], ckvn[:, ti * 128:(ti + 1) * 128], wukv[:, h * 256 + 128:h * 256 + 256])
                    o.copy(vsb[:, ti, :], ps[:, 0:128], eng="act")
                it = 0
                for a in range(4):
                    nj = 4 * a + 4

                    def geom(j, a=a):
                        qlo = max(j * 128, a * 512)
                        qhi = (a + 1) * 512
                        return qlo, qhi, qhi - qlo, qlo - a * 512, slice(j * 128, (j + 1) * 128)

                    def scores(j):
                        qlo, qhi, wd, off, ks = geom(j)
                        ps = self.ps()
                        o.mm(ps[:, 0:wd], knT[:, ks], qnT[:, qlo:qhi], start=True, stop=False)
                        o.mm(ps[:, 0:wd], krT[:, ks], qrT[:, qlo:qhi], start=False, stop=True)
                        return ps

                    ps_next = scores(0)
                    for j in range(nj):
                        qlo, qhi, wd, off, ks = geom(j)
                        ps = ps_next
                        if j + 1 < nj:
                            ps_next = scores(j + 1)
                        pt = pts[it % 3]
                        it += 1
                        o.act(pt[:, 0:wd], ps[:, 0:wd], AF.Exp, scale=ATT_SCALE)
                        if j >= 4 * a:
                            o.tt(pt[:, 0:128], pt[:, 0:128], C["causal_b"], ALU.mult)
                        o.mm(pO[:, off:512], vsb[:, j, :], pt[:, 0:wd], start=(j == 0), stop=(j == nj - 1))
                        o.mm(pD[:, off:512], C["ones_b"], pt[:, 0:wd], start=(j == 0), stop=(j == nj - 1))
                    o.recip(rd, pD)
                    o.tt(self.yT[0][:, h, a * 512:(a + 1) * 512], pO, rd, ALU.mult)


class KernGDN(KernMLA):
    def gdn_mixer(self, l):
        self.fw.pe_safe = True
        try:
            self._gdn_mixer(l)
        finally:
            self.fw.pe_safe = False

    def _gdn_mixer(self, l):
        o = self.o
        C = self.C
        with self.scope() as sc:
            sb = sc.sbuf
            scal = V(sb("g_scal", [68, S], F32))
            tok = V(sb("g_tok", [128, NT, 16], F32))
            egl = V(sb("g_egl", [68, 32], F32))
            bge = V(sb("g_bge", [128, NT, 4], F32))
            with self.scope() as s2:
                w3 = V(s2.sbuf("g_w3", [128, 8, 68], BF16))
                par = V(s2.sbuf("g_par", [68, 4], F32))
                tmp = V(s2.sbuf("g_tmp", [68, 512], F32))
                edT = V(s2.sbuf("g_edT", [4, S], F32))
                o.memset(w3, 0.0)
                o.memset(par, 0.0)
                self.wload(w3[:, :, 0:4], self.w_in_cols(l, OFF["ga"], 4))
                self.wload(w3[:, :, 32:36], self.w_in_cols(l, OFF["gb"], 4))
                self.wload(w3[:, :, 64:68], self.w_in_cols(l, OFF["ga"], 4))
                for base in (0, 64):
                    o.dma(par[base:base + 4, 0:1], self.W["gdn_dt_bias"][l].rearrange("(p o) -> p o", o=1), slow=True)
                    o.dma(par[base:base + 4, 1:2], self.W["gdn_a_log"][l].rearrange("(p o) -> p o", o=1), slow=True)
                o.act(par[:, 2:3], par[:, 1:2], AF.Exp)
                o.ts(par[:, 2:3], par[:, 2:3], -1.0, None, ALU.mult)
                for tb in range(4):
                    blk = slice(tb * 512, (tb + 1) * 512)
                    ps = self.ps()
                    for k in range(8):
                        o.mm(ps[0:68, :], w3[:, k, :], self.xT[:, k, blk], start=(k == 0), stop=(k == 7))
                    o.act(tmp, ps[0:68, :], AF.Exp, bias=par[:, 0:1])
                    o.act(tmp, tmp, AF.Ln, bias=1.0)
                    o.ts(tmp, tmp, par[:, 2:3], None, ALU.mult)
                    o.copy(scal[:, blk], tmp)
                    o.act(tmp[32:36, :], ps[32:36, :], AF.Sigmoid)
                    o.copy(scal[32:36, blk], tmp[32:36, :])
                for base in (0, 64):
                    for tb in range(4):
                        blk = slice(tb * 512, (tb + 1) * 512)
                        o.scan(tmp[base:base + 4, :], C["scan64x"][base:base + 4, blk], scal[base:base + 4, blk], 0.0, ALU.mult, ALU.add)
                        o.copy(scal[base:base + 4, blk], tmp[base:base + 4, :])
                gc3 = scal[0:4, :].rearrange("p (c t) -> p c t", t=64)
                ed3 = edT.rearrange("p (c t) -> p c t", t=64)
                o.tt(ed3, gc3[:, :, 63:64].bcast([4, 32, 64]), gc3, ALU.subtract)
                o.act(edT, edT, AF.Exp)
                o.act(scal[64:68, :], scal[64:68, :], AF.Exp)
                o.copy(egl[64:68, :], scal[64:68, :].rearrange("p (c t) -> p c t", t=64)[:, :, 63])
                for ti in range(NT):
                    tl = slice(ti * 128, (ti + 1) * 128)
                    ps = self.ps()
                    o.tr(ps[:, 0:4], scal[0:4, tl], C["ident"][0:4, 0:4])
                    o.tr(ps[:, 4:8], scal[32:36, tl], C["ident"][32:36, 32:36])
                    o.tr(ps[:, 8:12], scal[64:68, tl], C["ident"][64:68, 64:68])
                    o.tr(ps[:, 12:16], edT[0:4, tl], C["ident"][0:4, 0:4])
                    o.copy(tok[:, ti, :], ps[:, 0:16])
                o.tt(bge, tok[:, :, 4:8], tok[:, :, 8:12], ALU.mult)
            ng = V(sb("g_ng", [128, 1], F32))
            o.dma(ng, self.W["gdn_norm"][l].rearrange("(p o) -> p o", o=1), slow=True)
            for h in range(4):
                self.gdn_head(l, h, scal, tok, egl, bge, ng)

    def gdn_head(self, l, h, scal, tok, egl, bge, ng):
        MARKS.append(("gdn h%d start" % h, self.nc.get_next_instruction_name()))
        o = self.o
        C = self.C
        sel = C["sel4x"]
        hs = slice(h * 128, (h + 1) * 128)
        with self.scope() as sc:
            sb = sc.sbuf
            wz = V(sb("g_wz", [128, 8, 128], BF16))
            self.wload(wz, self.w_in_cols(l, OFF["gz"] + h * 128, 128))
            qT = V(sb("g_qT", [128, S], BF16))
            kT = V(sb("g_kT", [128, S], BF16))
            vT = V(sb("g_vT", [128, S], BF16))
            with self.scope() as s1:
                s1b = s1.sbuf
                wts = []
                for i, nm in enumerate(("gq", "gk", "gv")):
                    w = V(s1b("g_w" + nm, [128, 8, 128], BF16))
                    self.wload(w, self.w_in_cols(l, OFF[nm] + h * 128, 128))
                    wts.append(w)
                cw = V(s1b("g_cw", [128, 3, 4], F32))
                for i in range(3):
                    o.dma(cw[:, i, :], self.W["gdn_conv"][l][:, i * 512 + h * 128:i * 512 + (h + 1) * 128].rearrange("t c -> c t"), slow=True)
                xin = V(s1b("g_xin", [128, S], F32))
                acc = V(s1b("g_acc", [128, S], F32))
                sq = V(s1b("g_sq", [128, 512], BF16))
                rs = V(s1b("g_rs", [128, 512], F32))
                for i, dst in enumerate((qT, kT, vT)):
                    self.fw.pe_safe = False
                    self.proj_fm(wts[i], 128, lambda ps, tb: o.copy(xin[:, tb * 512:(tb + 1) * 512], ps, eng="act"))
                    self.fw.pe_safe = True
                    o.ts(acc, xin, cw[:, i, 3:4], None, ALU.mult)
                    for d in range(1, 4):
                        o.stt(acc[:, d:S], xin[:, 0:S - d], cw[:, i, 3 - d:4 - d], acc[:, d:S], ALU.mult, ALU.add)
                    o.act(acc, acc, AF.Silu)
                    if i == 2:
                        o.copy(vT, acc)
                        continue
                    for tb in range(4):
                        blk = slice(tb * 512, (tb + 1) * 512)
                        o.act(sq, acc[:, blk], AF.Square)
                        p = self.ps()
                        o.mm(p, C["ones_b"], sq)
                        o.act(rs, p, AF.Sqrt, bias=RMS_EPS, scale=1.0)
                        o.recip(rs, rs)
                        if i == 0:
                            o.stt(dst[:, blk], acc[:, blk], 128 ** -0.5, rs, ALU.mult, ALU.mult)
                        else:
                            o.tt(dst[:, blk], acc[:, blk], rs, ALU.mult)
            MARKS.append(("gdn h%d front-done" % h, self.nc.get_next_instruction_name()))
            u = V(sb("g_u", [128, NT, 128], F32))
            wT = V(sb("g_wT", [128, S], BF16))
            qgT = V(sb("g_qgT", [128, S], BF16))
            kdec = V(sb("g_kdec", [128, NT, 128], BF16))
            qkm = V(sb("g_qkm", [128, NT, 128], BF16))
            dtab = V(sb("g_dtab", [128, 32], F32))
            oT = V(sb("g_oT", [128, S], F32))
            LT = V(sb("g_LT", [128, 128], F32))
            dd = V(sb("g_dd", [128, 128], F32))
            t1 = V(sb("g_t1", [128, 128], F32))
            G = 4
            sets = []
            for i in range(G):
                sets.append(dict(
                    PT=[V(sb("g_PTa%d" % i, [128, 128], F32)), V(sb("g_PTb%d" % i, [128, 128], F32))],
                    P=[V(sb("g_Pa%d" % i, [128, 128], F32)), V(sb("g_Pb%d" % i, [128, 128], F32))],
                    Y=V(sb("g_Y%d" % i, [128, 256], F32))))
            p = self.ps()
            o.mm(p[:, 0:32], sel[64:68, hs], egl[64:68, :])
            o.copy(dtab, p[:, 0:32])
            for base in range(0, NT, G):
                for i in range(G):
                    ti = base + i
                    T = sets[i]
                    AT, Y = T["PT"][0], T["Y"]
                    tl = slice(ti * 128, (ti + 1) * 128)
                    gcc = tok[:, ti, h:h + 1]
                    pG = self.ps()
                    o.mm(pG[:, 0:128], sel[0:4, hs], scal[0:4, tl])
                    o.mm(pG[:, 128:256], sel[32:36, hs], scal[32:36, tl])
                    o.mm(pG[:, 256:384], sel[64:68, hs], scal[64:68, tl])
                    o.ts(dd, pG[:, 0:128], gcc, 0.0, ALU.subtract, ALU.min)
                    o.act(dd, dd, AF.Exp)
                    pK = self.ps()
                    o.mm(pK[:, 0:128], kT[:, tl], kT[:, tl])
                    o.mm(pK[:, 128:256], kT[:, tl], qT[:, tl])
                    pt = self.ps().bitcast(BF16)
                    o.tr(pt[:, 0:128], kT[:, tl], C["ident_b"])
                    o.tr(pt[:, 128:256], vT[:, tl], C["ident_b"])
                    o.tt(qgT[:, tl], qT[:, tl], pG[:, 256:384], ALU.mult)
                    o.ts(Y[:, 0:128], pt[:, 128:256], tok[:, ti, 4 + h:5 + h], None, ALU.mult)
                    o.ts(Y[:, 128:256], pt[:, 0:128], bge[:, ti, h:h + 1], None, ALU.mult)
                    o.ts(kdec[:, ti, :], pt[:, 0:128], tok[:, ti, 12 + h:13 + h], None, ALU.mult)
                    o.tt(LT, dd, C["m_up64"], ALU.mult)
                    o.tt(t1, dd, C["m_sup64"], ALU.mult)
                    o.tt(qkm[:, ti, :], pK[:, 128:256], LT, ALU.mult)
                    o.tt(t1, pK[:, 0:128], t1, ALU.mult)
                    o.tt(AT, t1, pG[:, 128:256], ALU.mult)
                    pa = self.ps()
                    o.tr(pa[:, 0:128], AT, C["ident"])
                    pa2 = self.ps()
                    o.mm(pa2[:, 0:256], AT, Y)
                    o.copy(T["P"][0], pa[:, 0:128], eng="act")
                    o.tt(Y, Y, pa2[:, 0:256], ALU.subtract)
                cur = 0
                for k in range(5):
                    nxt = 1 - cur
                    pps = [self.pb[i] for i in range(G)]
                    pqs = [self.pb[4 + i] for i in range(G)]
                    for i in range(G):
                        T = sets[i]
                        o.mm(pps[i][:, 0:128], T["P"][cur], T["PT"][cur])
                        if k < 4:
                            o.mm(pps[i][:, 128:256], T["PT"][cur], T["P"][cur])
                    for i in range(G):
                        T = sets[i]
                        o.copy(T["PT"][nxt], pps[i][:, 0:128], eng="act")
                        if k < 4:
                            o.copy(T["P"][nxt], pps[i][:, 128:256], eng="act")
                    for i in range(G):
                        T = sets[i]
                        o.mm(pqs[i][:, 0:256], T["PT"][nxt], T["Y"])
                    for i in range(G):
                        T = sets[i]
                        o.tt(T["Y"], T["Y"], pqs[i][:, 0:256], ALU.add)
                    cur = nxt
                for i in range(G):
                    ti = base + i
                    T = sets[i]
                    tl = slice(ti * 128, (ti + 1) * 128)
                    wtok = T["P"][0]
                    o.copy(u[:, ti, :], T["Y"][:, 0:128], eng="act")
                    o.copy(wtok, T["Y"][:, 128:256], eng="act")
                    pw = self.ps()
                    o.tr(pw[:, 0:128], wtok, C["ident"])
                    o.copy(wT[:, tl], pw[:, 0:128], eng="act")
            MARKS.append(("gdn h%d prep-done" % h, self.nc.get_next_instruction_name()))
            Sts = [V(sb("g_S%d" % i, [128, 128], F32)) for i in range(2)]
            Sbs = [V(sb("g_Sb%d" % i, [128, 128], BF16)) for i in range(2)]
            vn = [V(sb("g_vn%d" % i, [128, 128], BF16)) for i in range(2)]
            o.memset(Sts[0], 0.0)
            o.memset(Sbs[0], 0.0)
            pO = self.pb[5]
            pSs = [self.pb[6], self.pb[6]]
            pV = self.pb[7]
            cur = 0
            for n in range(32):
                ti, half = n // 2, n % 2
                rows = slice(half * 64, half * 64 + 64)
                cols = slice(n * 64, n * 64 + 64)
                v = vn[ti % 2]
                pS = pSs[n % 2]
                St, Sb = Sts[cur], Sbs[cur]
                o.mm(pV[rows, 0:128], wT[:, cols], Sb)
                o.tt(v[rows, :], u[rows, ti, :], pV[rows, 0:128], ALU.subtract)
                oc = slice(half * 64, half * 64 + 64)
                o.mm(pO[:, oc], Sb, qgT[:, cols], start=True, stop=False, inc=True)
                o.mm(pO[:, oc], v[rows, :], qkm[rows, ti, half * 64:half * 64 + 64], start=False, stop=True)
                o.mm(pS[:, 0:128], kdec[rows, ti, :], v[rows, :])
                o.stt(Sbs[1 - cur], St, dtab[:, n:n + 1], pS[:, 0:128], ALU.mult, ALU.add)
                o.stt(Sts[1 - cur], St, dtab[:, n:n + 1], pS[:, 0:128], ALU.mult, ALU.add)
                cur = 1 - cur
                if half == 1:
                    o.copy(oT[:, ti * 128:(ti + 1) * 128], pO[:, 0:128], eng="act")
            MARKS.append(("gdn h%d scan-done" % h, self.nc.get_next_instruction_name()))
            g_sq = V(sb("g_gsq", [128, 512], BF16))
            g_rs = V(sb("g_grs", [128, 512], F32))
            g_gs = V(sb("g_ggs", [128, 512], F32))
            g_tmp = V(sb("g_gtmp", [128, 512], F32))
            self.fw.pe_safe = False
            self.gated_norm_out((g_sq, g_rs, g_gs, g_tmp), oT, wz, ng[:, 0:1], AF.Silu, self.yT[1][:, h, :])
            self.fw.pe_safe = True


class KernFull(KernGDN):
    def xacc(self, ti):
        v = self.yT[ti // 4].rearrange("p a s -> p (a s)").bitcast(F32)
        return v[:, (ti % 4) * 1024:(ti % 4 + 1) * 1024]

    def bc_row(self, sc, name, src_row):
        t = V(sc.sbuf(name, [128, D], F32))
        self.o.dma(t, src_row.rearrange("(o d) -> o d", o=1).bcast([128, D]))
        return t

    def ln_tile(self, r, gbc, bbc, st, junk):
        o = self.o
        o.memset(st[:, 0:2], 0.0)
        o.act(junk, r, AF.Copy, accum=st[:, 0:1])
        o.act(junk, r, AF.Square, accum=st[:, 1:2])
        o.ts(st[:, 2:4], st[:, 0:2], 1.0 / D, None, ALU.mult)
        o.tt(st[:, 4:5], st[:, 2:3], st[:, 2:3], ALU.mult)
        o.tt(st[:, 5:6], st[:, 3:4], st[:, 4:5], ALU.subtract)
        o.act(st[:, 6:7], st[:, 5:6], AF.Sqrt, bias=LN_EPS)
        o.recip(st[:, 7:8], st[:, 6:7])
        o.ts(r, r, st[:, 2:3], st[:, 7:8], ALU.subtract, ALU.mult)
        o.tt(r, r, gbc, ALU.mult)
        o.tt(r, r, bbc, ALU.add)

    def ln_tiles(self, items, gbc, bbc):
        o = self.o
        for r, st, junk in items:
            o.memset(st[:, 0:2], 0.0)
        for r, st, junk in items:
            o.act(junk, r, AF.Copy, accum=st[:, 0:1])
        for r, st, junk in items:
            o.act(junk, r, AF.Square, accum=st[:, 1:2])
        for r, st, junk in items:
            o.ts(st[:, 2:4], st[:, 0:2], 1.0 / D, None, ALU.mult)
        for r, st, junk in items:
            o.tt(st[:, 4:5], st[:, 2:3], st[:, 2:3], ALU.mult)
        for r, st, junk in items:
            o.tt(st[:, 5:6], st[:, 3:4], st[:, 4:5], ALU.subtract)
        for r, st, junk in items:
            o.act(st[:, 6:7], st[:, 5:6], AF.Sqrt, bias=LN_EPS)
        for r, st, junk in items:
            o.recip(st[:, 7:8], st[:, 6:7])
        for r, st, junk in items:
            o.ts(r, r, st[:, 2:3], st[:, 7:8], ALU.subtract, ALU.mult)
        for r, st, junk in items:
            o.tt(r, r, gbc, ALU.mult)
        for r, st, junk in items:
            o.tt(r, r, bbc, ALU.add)

    def merge_outproj(self, l, s, moe_next):
        o = self.o
        C = self.C
        with self.scope() as sc:
            sb = sc.sbuf
            mT = V(sb("m_mT", [128, 8, S], BF16))
            with self.scope() as s1:
                wgo = [V(s1.sbuf("m_wgo%d" % i, [128, 4, 8, 128], BF16)) for i in range(2)]
                wbo = [V(s1.sbuf("m_wbo%d" % i, [128, 4, 4, 128], BF16)) for i in range(2)]
                acc = [V(s1.sbuf("m_acc%d" % i, [128, 512], F32)) for i in range(4)]
                gs = [V(s1.sbuf("m_gs%d" % i, [128, 512], F32)) for i in range(2)]
                tt_ = V(s1.sbuf("m_t", [128, 512], F32))
                it = 0
                for oc in range(8):
                    wg, wb = wgo[oc % 2], wbo[oc % 2]
                    for m in range(4):
                        self.wload(wg[:, m], self.w_in_cols(l, OFF["gate"] + m * 1024 + oc * 128, 128))
                        self.wload(wb[:, m], self.W["w_branch"][l, m][:, oc * 128:(oc + 1) * 128].rearrange("(c p) n -> p c n", p=128))
                    for m in range(4):
                        for tb in range(4):
                            blk = slice(tb * 512, (tb + 1) * 512)
                            pg = self.ps()
                            for k in range(8):
                                o.mm(pg, wg[:, m, k, :], self.xT[:, k, blk], start=(k == 0), stop=(k == 7))
                            g = gs[it % 2]
                            it += 1
                            o.act(g, pg, AF.Sigmoid)
                            pbr = self.ps()
                            for c in range(4):
                                o.mm(pbr, wb[:, m, c, :], self.yT[m][:, c, blk], start=(c == 0), stop=(c == 3))
                            if m == 0:
                                o.tt(acc[tb], g, pbr, ALU.mult)
                            else:
                                o.tt(tt_, g, pbr, ALU.mult)
                                o.tt(mT[:, oc, blk] if m == 3 else acc[tb], acc[tb], tt_, ALU.add)
            wout = V(sb("m_wout", [128, 8, D], BF16))
            self.wload(wout, self.W["w_out"][l].rearrange("(k p) n -> p k n", p=128))
            gbc = self.bc_row(sc, "m_gbc", self.W["ln_mix_g"][l])
            bbc = self.bc_row(sc, "m_bbc", self.W["ln_mix_b"][l])
            xr = [V(sb("m_xr%d" % i, [128, D], F32)) for i in range(2)]
            rr = [V(sb("m_rr%d" % i, [128, D], F32)) for i in range(2)]
            xb = [V(sb("m_xb%d" % i, [128, D], BF16)) for i in range(2)]
            junk2 = [V(sb("m_junk%d" % i, [128, D], BF16)) for i in range(2)]
            st2 = [V(sb("m_st%d" % i, [128, 8], F32)) for i in range(2)]
            src = self.x_in[s] if l == 0 else self.xres[s]
            for tp in range(0, NT, 2):
                for k_ in range(2):
                    ti = tp + k_
                    tl = slice(ti * 128, (ti + 1) * 128)
                    x_, r_ = xr[k_], rr[k_]
                    o.dma(x_, src[tl, :])
                    for half in range(2):
                        hs = slice(half * 512, (half + 1) * 512)
                        ps = self.ps()
                        for k in range(8):
                            o.mm(ps, mT[:, k, tl], wout[:, k, hs], start=(k == 0), stop=(k == 7))
                        o.stt(r_[:, hs], x_[:, hs], ALPHA, ps, ALU.mult, ALU.add)
                self.ln_tiles([(rr[0], st2[0], junk2[0]), (rr[1], st2[1], junk2[1])], gbc, bbc)
                for k_ in range(2):
                    ti = tp + k_
                    r_, b_ = rr[k_], xb[k_]
                    o.ts(self.xacc(ti), r_, ALPHA, None, ALU.mult)
                    o.copy(b_, r_, eng="act")
                    self.transpose_into_xT(b_, ti)

    def router(self):
        o = self.o
        with self.scope() as sc:
            wrb = [V(sc.sbuf("r_wrb%d" % i, [128, D], F32)) for i in range(2)]
            junk = V(sc.sbuf("r_junk", [128, D], F32))
            lgs = V(sc.sbuf("r_lgs", [128, NT, 8], F32))
            rt = V(sc.sbuf("r_rt", [128, 64], F32))
            o.memset(lgs, 0.0)
            for e in range(NEXP):
                w = wrb[e % 2]
                o.dma(w, self.W["moe_router_T"][0, e:e + 1, :].bcast([128, D]))
                for ti in range(NT):
                    o.stt(junk, self.xacc(ti), 1.0 / ALPHA, w, ALU.mult, ALU.mult, accum=lgs[:, ti, e:e + 1])
            for ti in range(NT):
                lg = lgs[:, ti, :]
                m1, m2, nm1, den = rt[:, 8:9], rt[:, 9:10], rt[:, 10:11], rt[:, 11:12]
                eq, l2, sel, ex = rt[:, 16:24], rt[:, 24:32], rt[:, 32:40], rt[:, 40:48]
                o.fw.emit("dve", lambda e_: e_.reduce_max(m1.ap, lg.ap, AX.X), reads=[lgs.tile], writes=[rt.tile])
                o.ts(eq, lg, m1, None, ALU.is_equal)
                o.stt(l2, eq, -1e30, lg, ALU.mult, ALU.add)
                o.fw.emit("dve", lambda e_: e_.reduce_max(m2.ap, l2.ap, AX.X), reads=[rt.tile], writes=[rt.tile])
                o.ts(sel, lg, m2, None, ALU.is_ge)
                o.ts(nm1, m1, -1.0, None, ALU.mult)
                o.act(ex, lg, AF.Exp, bias=nm1)
                o.tt(ex, ex, sel, ALU.mult)
                o.fw.emit("dve", lambda e_: e_.reduce_sum(den.ap, ex.ap, AX.X), reads=[rt.tile], writes=[rt.tile])
                o.recip(den, den)
                o.ts(self.gates[:, ti, :], ex, den, None, ALU.mult)

    def ffn_stage(self, l, s, last):
        o = self.o
        moe = (l % 2 == 1)
        j = l // 2
        if moe:
            self.router()
        with self.scope() as sc:
            sb = sc.sbuf
            hT = V(sb("f_hT", [128, 4, S], BF16))
            wg = [V(sb("f_wg%d" % i, [128, 8, 512], BF16)) for i in range(2)]
            wu = [V(sb("f_wu%d" % i, [128, 8, 512], BF16)) for i in range(2)]
            wd = [V(sb("f_wd%d" % i, [128, 4, D], BF16)) for i in range(2)]
            hs_ = [V(sb("f_hs%d" % i, [128, 512], F32)) for i in range(2)]
            if moe:
                jobs = [(e, fb, 4) for e in range(NEXP) for fb in range(D_FFE // 512)]
            else:
                jobs = [(None, fb, 4) for fb in range(D_FF // 512)] + [(None, D_FF // 512, (D_FF % 512) // 128)]
            it = 0
            if DBG.get("max_jobs") is not None:
                jobs = jobs[:DBG["max_jobs"]]
            for ji, (e, fb, nch) in enumerate(jobs):
                g_, u_, d_ = wg[ji % 2], wu[ji % 2], wd[ji % 2]
                f0 = fb * 512
                nf = nch * 128
                if moe:
                    Wg, Wu, Wd = self.W["moe_w_gate"][j, e], self.W["moe_w_up"][j, e], self.W["moe_w_down"][j, e]
                else:
                    Wg, Wu, Wd = self.W["ffn_w_gate"][j], self.W["ffn_w_up"][j], self.W["ffn_w_down"][j]
                self.wload(g_[:, :, 0:nf], Wg[:, f0:f0 + nf].rearrange("(k p) n -> p k n", p=128))
                self.wload(u_[:, :, 0:nf], Wu[:, f0:f0 + nf].rearrange("(k p) n -> p k n", p=128))
                self.wload(d_[:, 0:nch, :], Wd[f0:f0 + nf, :].rearrange("(c p) n -> p c n", p=128))
                for tb in range(4):
                    blk = slice(tb * 512, (tb + 1) * 512)
                    for ch in range(nch):
                        pg = self.ps()
                        for k in range(8):
                            o.mm(pg, g_[:, k, ch * 128:(ch + 1) * 128], self.xT[:, k, blk], start=(k == 0), stop=(k == 7))
                        pu = self.ps()
                        for k in range(8):
                            o.mm(pu, u_[:, k, ch * 128:(ch + 1) * 128], self.xT[:, k, blk], start=(k == 0), stop=(k == 7))
                        h_ = hs_[it % 2]
                        it += 1
                        o.act(h_, pg, AF.Silu)
                        o.tt(hT[:, ch, blk], h_, pu, ALU.mult)
                for ti in range(NT):
                    tl = slice(ti * 128, (ti + 1) * 128)
                    xa = self.xacc(ti)
                    for half in range(2):
                        hs = slice(half * 512, (half + 1) * 512)
                        ps = self.ps()
                        for ch in range(nch):
                            o.mm(ps, hT[:, ch, tl], d_[:, ch, hs], start=(ch == 0), stop=(ch == nch - 1))
                        if moe:
                            o.stt(xa[:, hs], ps, self.gates[:, ti, e:e + 1], xa[:, hs], ALU.mult, ALU.add)
                        else:
                            o.tt(xa[:, hs], xa[:, hs], ps, ALU.add)
            gbc = self.bc_row(sc, "f_gbc", self.W["ln_ffn_g"][l])
            bbc = self.bc_row(sc, "f_bbc", self.W["ln_ffn_b"][l])
            junk2 = [V(sb("f_junk%d" % i, [128, D], BF16)) for i in range(2)]
            st2 = [V(sb("f_st%d" % i, [128, 8], F32)) for i in range(2)]
            xb = [V(sb("f_xb%d" % i, [128, D], BF16)) for i in range(2)]
            dst = self.out[s] if last else self.xres[s]
            for g in range(2):
                for i in range(4):
                    pair = (g * 8 + i, g * 8 + i + 4)
                    self.ln_tiles([(self.xacc(pair[0]), st2[0], junk2[0]), (self.xacc(pair[1]), st2[1], junk2[1])],
                                  gbc, bbc)
                    for k_, ti in enumerate(pair):
                        tl = slice(ti * 128, (ti + 1) * 128)
                        xa = self.xacc(ti)
                        o.dma(dst[tl, :], xa)
                        if not last:
                            b_ = xb[k_]
                            o.copy(b_, xa, eng="act")
                            self.transpose_into_xT(b_, ti)

    def forward(self, depth=DEPTH, dbg_stop=None):
        for s in range(self.nseq):
            self.load_xT(self.x_in[s])
            for l in range(depth):
                on = DBG.get("l1") if (l == 1 and DBG.get("l1") is not None) else ("mla", "gdn", "pool", "hgrn", "merge", "ffn")
                for st in ("mla", "gdn", "pool", "hgrn", "merge", "ffn"):
                    if st not in on:
                        continue
                    MARKS.append(("s%d l%d %s" % (s, l, st), self.nc.get_next_instruction_name()))
                    if st == "mla":
                        self.mla_mixer(l, s)
                    elif st == "gdn":
                        self.gdn_mixer(l)
                    elif st == "pool":
                        self.pool_mixer(l)
                    elif st == "hgrn":
                        self.hgrn_mixer(l)
                    elif st == "merge":
                        self.merge_outproj(l, s, moe_next=False)
                    else:
                        self.ffn_stage(l, s, last=(l == depth - 1))
        MARKS.append(("end", self.nc.get_next_instruction_name()))
        self.fw.finish()


def build_full(nseq=NSEQ, depth=DEPTH):
    nc = bass.Bass("TRN2", target_bir_lowering=False)
    stack = ExitStack()
    with stack:
        k = KernFull(nc, stack, nseq=nseq)
        k.forward(depth)
        print("instructions:", k.fw.ninst, k.fw.per, "sems:", k.fw.nsem, "sbuf left:", nc.sbuf_bytes_remaining)
    return nc


_CACHE = {}


def kernel(**inputs):
    n_cores = 8
    if "nc" not in _CACHE:
        _CACHE["nc"] = build_full()
    nc = _CACHE["nc"]
    consts = make_consts()
    x = np.ascontiguousarray(inputs["x"], dtype=np.float32)
    pos = np.ascontiguousarray(inputs["positions"], dtype=np.int32)
    in_maps = []
    warr = {name: weight_array(inputs, name) for name, _ in WEIGHT_SPECS}
    for c in range(n_cores):
        m = {"x": x[c * NSEQ:(c + 1) * NSEQ], "positions": pos[c * NSEQ:(c + 1) * NSEQ]}
        for name, shp in WEIGHT_SPECS:
            m[name] = warr[name]
        for k_, v_ in consts.items():
            m["c_" + k_] = v_
        in_maps.append(m)
    res = run_bass_kernel_spmd(nc, in_maps, core_ids=list(range(n_cores)))
    out = np.concatenate([np.asarray(r["out"]) for r in res.results], axis=0)
    return out.astype(np.float32)


KERN_CLS = KernFull
```

```python
import math
from contextlib import ExitStack

import numpy as np
import concourse.bass as bass
import concourse.mybir as mybir
from concourse.bass_utils import run_bass_kernel_spmd

F32 = mybir.dt.float32
BF16 = mybir.dt.bfloat16
I32 = mybir.dt.int32
ALU = mybir.AluOpType
AF = mybir.ActivationFunctionType
AX = mybir.AxisListType

D = 1024
S = 2048
NSEQ = 2
DEPTH = 2
NT = S // 128
D_IN = 9160
OFF = dict(cq=0, ckv=256, kr=384, gq=448, gk=960, gv=1472, gz=1984, ga=2496, gb=2500, pu=2504,
           hq=3016, hf=3528, hi=4040, hg=4552, gate=5064)
D_FF = 2816
D_FFE = 3584
NEXP = 8
ALPHA = (2 * DEPTH) ** 0.25
LN_EPS = 1e-5
RMS_EPS = 1e-6
ATT_SCALE = (128 + 64) ** -0.5

ENGS = ("pe", "dve", "act", "pool", "sp")
DBG = {}
MARKS = []
SELF_NOSYNC = ()
EPOCH = 5000
NDMASEM = 8


class Res:
    __slots__ = ("name", "w", "r")

    def __init__(self, name):
        self.name = name
        self.w = None
        self.r = {}


class Tile:
    def __init__(self, t, name):
        self.t = t
        self.name = name
        self.res = Res(name)

    def __getitem__(self, idx):
        return self.t[idx]


class FW:
    def __init__(self, nc, stack):
        self.nc = nc
        self.stack = stack
        self.cnt = {e: 0 for e in ENGS}
        self.known = {e: {} for e in ENGS}
        self.semh = {}
        self.dma_rr = {e: 0 for e in ENGS}
        self.dma_use = {}
        self.nsem = 0
        self.last = {}
        self.eobj = {"pe": nc.tensor, "dve": nc.vector, "act": nc.scalar, "pool": nc.gpsimd, "sp": nc.sync}
        self.ninst = 0
        self.per = {}
        self.pending_inc = {}
        self.pe_mode = None
        self.pe_safe = False

    def sbuf(self, name, shape, dtype, stack=None):
        self.nuniq = getattr(self, "nuniq", 0) + 1
        name = "%s_%d" % (name, self.nuniq)
        t = (stack or self.stack).enter_context(self.nc.sbuf_tensor(name, list(shape), dtype))
        return Tile(t, name)

    def psum(self, name, shape, dtype=F32):
        t = self.stack.enter_context(self.nc.psum_tensor(name, list(shape), dtype))
        return Tile(t, name)

    def dram(self, name, shape, dtype, kind=None):
        if kind is None:
            t = self.nc.dram_tensor(name, list(shape), dtype)
        else:
            t = self.nc.dram_tensor(name, list(shape), dtype, kind=kind)
        return Tile(t, name)

    def _sem(self, key):
        h = self.semh.get(key)
        if h is None:
            h = self.stack.enter_context(self.nc.semaphore("s%d" % self.nsem))
            self.nsem += 1
            self.semh[key] = h
        return h

    def _need(self, eng, ev):
        if ev is None:
            return
        key, val = ev
        if key[0] == "c" and key[1] == eng and eng in SELF_NOSYNC:
            return
        if eng == "pe" and key[0] == "c" and key[1] == "pe" and not self.pe_safe:
            return
        if self.known[eng].get(key, 0) >= val:
            return
        self.known[eng][key] = val
        self.eobj[eng].wait_ge(self._sem(key), val)
        self.ninst += 1
        self.per[eng] = self.per.get(eng, 0) + 1

    def emit(self, eng, fn, reads=(), writes=(), dma=False, inc=True, pe_mode=None):
        rr = [x.res if isinstance(x, Tile) else x for x in reads]
        ww = [x.res if isinstance(x, Tile) else x for x in writes]
        if eng == "pe" and self.pe_safe:
            inc = True
        if pe_mode is not None:
            if False and pe_mode != self.pe_mode and self.cnt["pe"] > 0:
                assert not self.pending_inc.get("pe"), "tiling-mode switch inside an open PE group"
                c = self.cnt["pe"] - 1
                key = ("c", "pe", c // EPOCH)
                self.eobj["pe"].wait_ge(self._sem(key), c % EPOCH + 1)
                self.ninst += 1
                self.ndrain = getattr(self, "ndrain", 0) + 1
            self.pe_mode = pe_mode
        for r in rr:
            self._need(eng, r.w)
        for w in ww:
            self._need(eng, w.w)
            for k, v in list(w.r.items()):
                self._need(eng, (k, v))
        if dma:
            i = self.dma_rr[eng]
            self.dma_rr[eng] = (i + 1) % NDMASEM
            key = ("d", eng, i)
            used = self.dma_use.get(key, 0)
            if used:
                self._need(eng, (key, 16 * used))
            self.dma_use[key] = used + 1
            ev = (key, 16 * (used + 1))
            inc = 16
        else:
            c = self.cnt[eng]
            key = ("c", eng, c // EPOCH)
            ev = (key, c % EPOCH + 1)
            if inc:
                self.cnt[eng] = c + 1
            self.pending_inc[eng] = not inc
        if dma or inc:
            fn(self.eobj[eng]).then_inc(self._sem(key), 16 if dma else 1)
        else:
            fn(self.eobj[eng])
        self.ninst += 1
        self.per[eng] = self.per.get(eng, 0) + 1
        self.last[key] = ev
        for r in rr:
            if r.r.get(key, 0) < ev[1]:
                r.r[key] = ev[1]
        for w in ww:
            w.w = ev
            w.r = {}
        return ev

    def barrier(self):
        assert not self.pending_inc.get("pe"), "PE group left open at a barrier"
        evs = list(self.last.values())
        for ev in evs:
            self._need("sp", ev)
        ev = self.emit("sp", lambda e: e.nop())
        for eng in ENGS:
            if eng != "sp":
                self._need(eng, ev)

    def finish(self):
        for ev in list(self.last.values()):
            self._need("sp", ev)


def make_consts():
    c = {}
    c["ident"] = np.eye(128, dtype=np.float32)
    i = np.arange(128)
    c["m_causal"] = (i[None, :] >= i[:, None]).astype(np.float32)
    blk64 = (i[:, None] // 64) == (i[None, :] // 64)
    blk32 = (i[:, None] // 32) == (i[None, :] // 32)
    c["m_low64"] = (blk64 & (i[None, :] <= i[:, None])).astype(np.float32)
    c["m_slow64"] = (blk64 & (i[None, :] < i[:, None])).astype(np.float32)
    c["m_up64"] = (blk64 & (i[None, :] >= i[:, None])).astype(np.float32)
    c["m_up32"] = (blk32 & (i[None, :] >= i[:, None])).astype(np.float32)
    c["m_chunk32"] = ((i[:, None] // 32) == np.arange(4)[None, :]).astype(np.float32)
    c["m_chunk64"] = ((i[:, None] // 64) == np.arange(2)[None, :]).astype(np.float32)
    t = np.arange(S)
    c["scan32"] = np.broadcast_to((t % 32 != 0).astype(np.float32)[None, :], (128, S)).copy()
    c["scan64"] = np.broadcast_to((t % 64 != 0).astype(np.float32)[None, :], (128, S)).copy()
    half = 32
    inv = (10000.0 ** (-np.arange(half, dtype=np.float32) / half)).astype(np.float32)
    rp = np.zeros((128, 4), np.float32)
    rp[:64, 0] = np.concatenate([inv, inv])
    rp[:64, 1] = np.concatenate([-np.ones(32), np.ones(32)])
    c["ropep"] = rp
    ic = np.zeros((128, 4, 16), np.float32)
    for gi, win in enumerate((2, 4, 8, 16)):
        ic[:, gi, :] = 1.0 / np.minimum(np.arange(16) + 1, win)
    c["invcnt"] = ic.reshape(128, 64)
    sel = np.zeros((128, 4, 128), np.float32)
    for h in range(4):
        sel[h, h, :] = 1.0
    c["sel4"] = sel.reshape(128, 512)
    c["ones"] = np.ones((128, 128), np.float32)
    selx = np.zeros((128, 4, 128), np.float32)
    for base in (0, 32, 64):
        for h in range(4):
            selx[base + h, h, :] = 1.0
    c["sel4x"] = selx.reshape(128, 512)
    c["m_sup64"] = (blk64 & (i[None, :] > i[:, None])).astype(np.float32)
    return c


CONST_SHAPES = {k: v.shape for k, v in make_consts().items()}


class View:
    __slots__ = ("tile", "ap")

    def __init__(self, tile, ap):
        self.tile = tile
        self.ap = ap

    def __getitem__(self, idx):
        return View(self.tile, self.ap[idx])

    def bitcast(self, dt):
        return View(self.tile, self.ap.bitcast(dt))

    def rearrange(self, s, **kw):
        return View(self.tile, self.ap.rearrange(s, **kw))

    def bcast(self, shape):
        return View(self.tile, self.ap.broadcast_to(list(shape)))


def V(tile):
    return View(tile, tile.t.ap() if hasattr(tile.t, "ap") and callable(tile.t.ap) else tile.t[:])


def _ap(x):
    return x.ap if isinstance(x, View) else x


def _tiles(*xs):
    return [x.tile for x in xs if isinstance(x, View)]


def _r32(n):
    return 32 if n <= 32 else (64 if n <= 64 else 128)


def _pe_mode(ap, is_tr):
    shp = list(ap.shape)
    k = shp[0]
    m = 1
    for d in shp[1:]:
        m *= d
    return (_r32(k), _r32(m), str(ap.dtype) == str(F32), is_tr and str(ap.dtype) == str(F32))


class Ops:
    def __init__(self, fw):
        self.fw = fw

    def mm(self, out, lhsT, rhs, start=True, stop=True, inc=None):
        if inc is None:
            inc = stop
        return self.fw.emit("pe", lambda e: e.matmul(out.ap, lhsT.ap, rhs.ap, start=start, stop=stop),
                            reads=_tiles(lhsT, rhs), writes=_tiles(out), inc=inc, pe_mode=_pe_mode(lhsT.ap, False))

    def tr(self, out, in_, ident, inc=True):
        return self.fw.emit("pe", lambda e: e.transpose(out.ap, in_.ap, ident.ap),
                            reads=_tiles(in_, ident), writes=_tiles(out), inc=inc, pe_mode=_pe_mode(in_.ap, True))

    def act(self, out, in_, func, bias=None, scale=None, accum=None):
        kw = {}
        if bias is not None:
            kw["bias"] = _ap(bias)
        if scale is not None:
            kw["scale"] = _ap(scale)
        if accum is not None:
            kw["accum_out"] = accum.ap
        return self.fw.emit("act", lambda e: e.activation(out.ap, in_.ap, func, **kw),
                            reads=_tiles(in_, bias, scale), writes=_tiles(out, accum))

    def ts(self, out, in0, s1, s2, op0, op1=None, eng="dve", accum=None):
        kw = {}
        if op1 is not None:
            kw["op1"] = op1
        if accum is not None:
            kw["accum_out"] = accum.ap
        return self.fw.emit(eng, lambda e: e.tensor_scalar(out.ap, in0.ap, _ap(s1), _ap(s2), op0, **kw),
                            reads=_tiles(in0, s1, s2), writes=_tiles(out, accum))

    def tt(self, out, a, b, op, eng="dve"):
        return self.fw.emit(eng, lambda e: e.tensor_tensor(out.ap, a.ap, b.ap, op),
                            reads=_tiles(a, b), writes=_tiles(out))

    def stt(self, out, in0, scalar, in1, op0, op1, accum=None):
        kw = {}
        if accum is not None:
            kw["accum_out"] = accum.ap
        return self.fw.emit("dve", lambda e: e.scalar_tensor_tensor(out.ap, in0.ap, _ap(scalar), in1.ap, op0, op1, **kw),
                            reads=_tiles(in0, scalar, in1), writes=_tiles(out, accum))

    def copy(self, out, in_, eng="dve"):
        if eng == "act":
            return self.act(out, in_, AF.Copy)
        return self.fw.emit(eng, lambda e: e.tensor_copy(out.ap, in_.ap), reads=_tiles(in_), writes=_tiles(out))

    def recip(self, out, in_):
        return self.fw.emit("dve", lambda e: e.reciprocal(out.ap, in_.ap), reads=_tiles(in_), writes=_tiles(out))

    def scan(self, out, d0, d1, init, op0, op1):
        return self.fw.emit("dve", lambda e: e.tensor_tensor_scan(out.ap, d0.ap, d1.ap, _ap(init), op0, op1),
                            reads=_tiles(d0, d1, init), writes=_tiles(out))

    def memset(self, out, val, eng="dve"):
        return self.fw.emit(eng, lambda e: e.memset(out.ap, val), writes=_tiles(out))

    def dma(self, out, in_, eng="sp", slow=False):
        kw = {"allow_slow_non_contiguous": True} if slow else {}
        return self.fw.emit(eng, lambda e: e.dma_start(out=out.ap, in_=in_.ap, **kw),
                            reads=_tiles(in_), writes=_tiles(out), dma=True)

    def wrap(self, out, in_, shift):
        return self.fw.emit("dve", lambda e: e.add_range_wrap(out.ap, in_.ap, shift, math.pi, 2 * math.pi),
                            reads=_tiles(in_), writes=_tiles(out))


def V(tile):
    return View(tile, tile.t[:])


WEIGHT_SPECS = [
    ("w_in", (2, 1024, 9160)), ("mla_q_norm", (2, 256)), ("mla_w_uq", (2, 256, 768)),
    ("mla_kv_norm", (2, 128)), ("mla_w_ukv", (2, 128, 1024)), ("gdn_conv", (2, 4, 1536)),
    ("gdn_a_log", (2, 4)), ("gdn_dt_bias", (2, 4)), ("gdn_norm", (2, 128)), ("pool_w", (2, 4, 128, 128)),
    ("pool_scale", (2, 512)), ("hgrn_lb_logits", (2, 512)), ("hgrn_norm", (2, 128)),
    ("w_branch", (2, 4, 512, 1024)), ("w_out", (2, 1024, 1024)), ("ln_mix_g", (2, 1024)),
    ("ln_mix_b", (2, 1024)), ("ffn_w_gate", (1, 1024, 2816)), ("ffn_w_up", (1, 1024, 2816)),
    ("ffn_w_down", (1, 2816, 1024)), ("moe_router", (1, 1024, 8)), ("moe_w_gate", (1, 8, 1024, 3584)),
    ("moe_w_up", (1, 8, 1024, 3584)), ("moe_w_down", (1, 8, 3584, 1024)), ("ln_ffn_g", (2, 1024)),
    ("ln_ffn_b", (2, 1024)), ("moe_router_T", (1, 8, 1024)),
]


def weight_array(inputs, name):
    if name == "moe_router_T":
        return np.ascontiguousarray(np.asarray(inputs["moe_router"], dtype=np.float32).transpose(0, 2, 1))
    return np.ascontiguousarray(inputs[name], dtype=np.float32)


class Kern:
    def __init__(self, nc, stack, nseq=NSEQ, dbg=None):
        self.nc = nc
        self.fw = fw = FW(nc, stack)
        self.o = Ops(fw)
        self.nseq = nseq
        self.dbg = dbg or {}
        self.x_in = V(fw.dram("x", [nseq, S, D], F32, kind="ExternalInput"))
        self.pos = V(fw.dram("positions", [nseq, S], I32, kind="ExternalInput"))
        self.W = {}
        for name, shp in WEIGHT_SPECS:
            self.W[name] = V(fw.dram(name, list(shp), F32, kind="ExternalInput"))
        self.Cd = {}
        for name, shp in CONST_SHAPES.items():
            self.Cd[name] = V(fw.dram("c_" + name, list(shp), F32, kind="ExternalInput"))
        self.out = V(fw.dram("out", [nseq, S, D], F32, kind="ExternalOutput"))
        self.xres = V(fw.dram("xres", [nseq, S, D], F32))
        self.pb = [V(fw.psum("pb%d" % i, [128, 512], F32)) for i in range(8)]
        self.prr = 0
        sb = fw.sbuf
        o = self.o
        C = self.C = {}
        for name in ("ident", "m_causal", "m_up64", "m_sup64", "m_up32", "ones"):
            C[name] = V(sb("k_" + name, [128, 128], F32))
            o.dma(C[name], self.Cd[name])
        for name, w in (("m_chunk32", 4), ("m_chunk64", 2), ("ropep", 4), ("invcnt", 64)):
            C[name] = V(sb("k_" + name, [128, w], F32))
            o.dma(C[name], self.Cd[name])
        C["scan32"] = V(sb("k_scan32", [128, S], F32))
        o.dma(C["scan32"], self.Cd["scan32"])
        C["scan64x"] = V(sb("k_scan64x", [68, S], F32))
        o.dma(C["scan64x"], self.Cd["scan64"][0:68, :])
        C["sel4x"] = V(sb("k_sel4x", [68, 512], F32))
        o.dma(C["sel4x"], self.Cd["sel4x"][0:68, :])
        C["ident_b"] = V(sb("k_ident_b", [128, 128], BF16))
        o.copy(C["ident_b"], C["ident"])
        C["ones_b"] = V(sb("k_ones_b", [128, 128], BF16))
        o.copy(C["ones_b"], C["ones"])
        C["causal_b"] = V(sb("k_causal_b", [128, 128], BF16))
        o.copy(C["causal_b"], C["m_causal"])
        self.gates = V(sb("moe_gates", [128, NT, 8], F32))
        self.xT = V(sb("xT", [128, 8, S], BF16))
        self.yT = [V(sb("yT%d" % m, [128, 4, S], BF16)) for m in range(4)]

    def ps(self, lo=0, hi=5):
        n = hi - lo
        b = self.pb[lo + self.prr % n]
        self.prr += 1
        return b

    def scope(self):
        return _Scope(self)

    def load_xT(self, src):
        o = self.o
        with self.scope() as sc:
            xin = [V(sc.sbuf("xa_in%d" % i, [128, D], F32)) for i in range(2)]
            xb = [V(sc.sbuf("xa_b%d" % i, [128, D], BF16)) for i in range(2)]
            for ti in range(NT):
                a, b = xin[ti % 2], xb[ti % 2]
                o.dma(a, src[ti * 128:(ti + 1) * 128, :])
                o.copy(b, a, eng="act")
                self.transpose_into_xT(b, ti)

    def transpose_into_xT(self, xb, ti):
        o = self.o
        p = self.ps().bitcast(BF16)
        for k in range(8):
            o.tr(p[:, k * 128:(k + 1) * 128], xb[:, k * 128:(k + 1) * 128], self.C["ident_b"], inc=(k == 7))
        o.copy(self.xT[:, :, ti * 128:(ti + 1) * 128], p.rearrange("p (k t) -> p k t", k=8))

    def wload(self, dst, src):
        return self.o.dma(dst, src, eng="pool")

    def w_in_cols(self, l, c0, n):
        return self.W["w_in"][l, :, c0:c0 + n].rearrange("(k p) n -> p k n", p=128)

    def proj_fm(self, wt, ncols, cb, blocks=range(4), bw=512):
        o = self.o
        for tb in blocks:
            ps = self.ps()
            for k in range(8):
                o.mm(ps[0:ncols, 0:bw], wt[:, k, 0:ncols], self.xT[:, k, tb * bw:(tb + 1) * bw],
                     start=(k == 0), stop=(k == 7))
            cb(ps[0:ncols, 0:bw], tb)

    def proj_tm(self, wt, ncols, cb, tiles=range(NT)):
        o = self.o
        for ti in tiles:
            ps = self.ps()
            for k in range(8):
                o.mm(ps[:, 0:ncols], self.xT[:, k, ti * 128:(ti + 1) * 128], wt[:, k, 0:ncols],
                     start=(k == 0), stop=(k == 7))
            cb(ps[:, 0:ncols], ti)

    def gated_norm_out(self, sc_tiles, oT, wg, ng_col, func, dst):
        o = self.o
        sets = [sc_tiles[i:i + 4] for i in range(0, len(sc_tiles), 4)]
        n = len(sets)
        for tb0 in range(0, 4, n):
            items = [(sets[i], slice((tb0 + i) * 512, (tb0 + i + 1) * 512)) for i in range(n)]
            for (sq, rs, gs, tmp), blk in items:
                o.act(sq, oT[:, blk], AF.Square)
            pss = []
            for (sq, rs, gs, tmp), blk in items:
                p = self.ps()
                o.mm(p, self.C["ones_b"], sq)
                pss.append(p)
            pgs = []
            for (sq, rs, gs, tmp), blk in items:
                pg = self.ps()
                for k in range(8):
                    o.mm(pg, wg[:, k, :], self.xT[:, k, blk], start=(k == 0), stop=(k == 7))
                pgs.append(pg)
            for i, ((sq, rs, gs, tmp), blk) in enumerate(items):
                o.act(rs, pss[i], AF.Sqrt, bias=RMS_EPS, scale=1.0 / 128)
            for i, ((sq, rs, gs, tmp), blk) in enumerate(items):
                o.act(gs, pgs[i], func)
            for (sq, rs, gs, tmp), blk in items:
                o.recip(rs, rs)
            for (sq, rs, gs, tmp), blk in items:
                o.stt(tmp, oT[:, blk], ng_col, rs, ALU.mult, ALU.mult)
            for (sq, rs, gs, tmp), blk in items:
                o.tt(dst[:, blk], tmp, gs, ALU.mult)


class _Scope:
    def __init__(self, kern):
        self.k = kern
        self.stack = ExitStack()

    def __enter__(self):
        self.stack.__enter__()
        return self

    def sbuf(self, name, shape, dtype):
        return self.k.fw.sbuf(name, shape, dtype, stack=self.stack)

    def __exit__(self, *a):
        if a[0] is None:
            self.k.fw.barrier()
        return self.stack.__exit__(*a)


class KernMix(Kern):
    def pool_mixer(self, l):
        o = self.o
        C = self.C
        with self.scope() as sc:
            wt = V(sc.sbuf("pl_w", [128, 8, 512], BF16))
            self.wload(wt, self.w_in_cols(l, OFF["pu"], 512))
            pw = V(sc.sbuf("pl_pw", [128, 4, 128], BF16))
            self.wload(pw, self.W["pool_w"][l].rearrange("g c d -> c g d"))
            psc = V(sc.sbuf("pl_sc", [128, 4], F32))
            o.dma(psc, self.W["pool_scale"][l].rearrange("(g p) -> p g", p=128), slow=True)
            u = V(sc.sbuf("pl_u", [128, 4, S], F32))
            tmp = [V(sc.sbuf("pl_t%d" % i, [128, S], F32)) for i in range(2)]
            pooled = V(sc.sbuf("pl_pooled", [128, S], BF16))
            t16 = V(sc.sbuf("pl_t16", [128, 16], F32))
            for gi in range(4):
                self.proj_fm(wt[:, :, gi * 128:(gi + 1) * 128], 128,
                             lambda ps, tb, gi=gi: o.copy(u[:, gi, tb * 512:(tb + 1) * 512], ps, eng="act"))
            for gi in range(4):
                win = 2 ** (gi + 1)
                cur = u[:, gi, :]
                sh = 1
                for step in range(gi + 1):
                    nxt = tmp[step % 2]
                    o.tt(nxt[:, sh:S], cur[:, sh:S], cur[:, 0:S - sh], ALU.add)
                    o.copy(nxt[:, 0:sh], cur[:, 0:sh])
                    cur = nxt
                    sh *= 2
                o.stt(pooled[:, :], cur, 1.0 / win, u[:, gi, :], ALU.mult, ALU.subtract)
                o.tt(t16, cur[:, 0:16], C["invcnt"][:, gi * 16:(gi + 1) * 16], ALU.mult)
                o.tt(pooled[:, 0:16], t16, u[:, gi, 0:16], ALU.subtract)
                for tb in range(4):
                    blk = slice(tb * 512, (tb + 1) * 512)
                    p = self.ps()
                    o.mm(p, pw[:, gi, :], pooled[:, blk])
                    o.ts(self.yT[2][:, gi, blk], p, psc[:, gi:gi + 1], None, ALU.mult)

    def hgrn_prep(self, l):
        o = self.o
        if hasattr(self, "_lb"):
            return
        sb = self.fw.sbuf
        lg = V(sb("hg_lg", [128, 2, 4], F32))
        o.dma(lg, self.W["hgrn_lb_logits"].rearrange("l (h p) -> p l h", p=128), slow=True)
        lb = V(sb("hg_lb", [128, 2, 4], F32))
        oml = V(sb("hg_oml", [128, 2, 4], F32))
        noml = V(sb("hg_noml", [128, 2, 4], F32))
        d = V(sb("hg_d", [128, 4], F32))
        o.tt(d, lg[:, 1, :], lg[:, 0, :], ALU.subtract)
        o.memset(lb[:, 0, :], 0.0)
        o.act(lb[:, 1, :], d, AF.Sigmoid)
        o.ts(oml, lb, -1.0, 1.0, ALU.mult, ALU.add)
        o.ts(noml, oml, -1.0, None, ALU.mult)
        self._lb, self._oml, self._noml = lb, oml, noml
        ng = V(sb("hg_ng", [128, 2], F32))
        o.dma(ng, self.W["hgrn_norm"].rearrange("l p -> p l"), slow=True)
        self._hng = ng

    def hgrn_mixer(self, l):
        o = self.o
        C = self.C
        self.hgrn_prep(l)
        for h in range(4):
            with self.scope() as sc:
                sb = sc.sbuf
                wq = V(sb("h_wq", [128, 8, 128], BF16))
                wf = V(sb("h_wf", [128, 8, 128], BF16))
                wi = V(sb("h_wi", [128, 8, 128], BF16))
                wg = V(sb("h_wg", [128, 8, 128], BF16))
                self.wload(wq, self.w_in_cols(l, OFF["hq"] + h * 128, 128))
                self.wload(wf, self.w_in_cols(l, OFF["hf"] + h * 128, 128))
                self.wload(wi, self.w_in_cols(l, OFF["hi"] + h * 128, 128))
                self.wload(wg, self.w_in_cols(l, OFF["hg"] + h * 128, 128))
                lbc = self._lb[:, l, h:h + 1]
                omlc = self._oml[:, l, h:h + 1]
                nomlc = self._noml[:, l, h:h + 1]
                q0T = V(sb("h_q0T", [128, S], BF16))
                qmT = V(sb("h_qmT", [128, S], BF16))
                kmT = V(sb("h_kmT", [128, S], BF16))
                khT = V(sb("h_khT", [128, S], BF16))
                vsb = V(sb("h_v", [128, NT, 128], BF16))
                oT = V(sb("h_oT", [128, S], F32))
                eb = V(sb("h_eb", [128, 64], F32))
                with self.scope() as s1:
                    s1b = s1.sbuf
                    qs = V(s1b("h_qs", [128, S], F32))
                    kT = V(s1b("h_kT", [128, S], F32))
                    lf = V(s1b("h_lf", [128, S], F32))
                    b = V(s1b("h_b", [128, S], F32))
                    e = V(s1b("h_e", [128, S], F32))
                    self.proj_fm(wq, 128, lambda ps, tb: o.act(qs[:, tb * 512:(tb + 1) * 512], ps, AF.Silu))

                    def f_cb(ps, tb):
                        blk = slice(tb * 512, (tb + 1) * 512)
                        o.act(e[:, blk], ps, AF.Sigmoid)
                        o.ts(lf[:, blk], e[:, blk], omlc, lbc, ALU.mult, ALU.add)
                        o.ts(kT[:, blk], e[:, blk], nomlc, omlc, ALU.mult, ALU.add)
                        o.act(lf[:, blk], lf[:, blk], AF.Ln)
                    self.proj_fm(wf, 128, f_cb)
                    self.proj_tm(wi, 128, lambda ps, ti: o.copy(vsb[:, ti, :], ps, eng="act"))
                    o.scan(b, C["scan32"], lf, 0.0, ALU.mult, ALU.add)
                    b3 = b.rearrange("p (c t) -> p c t", t=32)
                    o.act(e, b, AF.Exp)
                    e3 = e.rearrange("p (c t) -> p c t", t=32)
                    o.copy(eb, e3[:, :, 31])
                    o.tt(q0T, qs, e, ALU.mult)
                    lf3 = lf.rearrange("p (c t) -> p c t", t=32)
                    o.tt(lf3, b3, b3[:, :, 15:16].bcast([128, 64, 32]), ALU.subtract)
                    o.act(e, lf, AF.Exp)
                    o.tt(qmT, qs, e, ALU.mult)
                    o.act(e, lf, AF.Exp, scale=-1.0)
                    o.tt(kmT, kT, e, ALU.mult)
                    o.tt(lf3, b3, b3[:, :, 31:32].bcast([128, 64, 32]), ALU.subtract)
                    o.act(e, lf, AF.Exp, scale=-1.0)
                    o.tt(khT, kT, e, ALU.mult)
                att = V(sb("h_att", [128, NT, 128], BF16))
                khm = V(sb("h_khm", [128, NT, 4, 128], BF16))
                Sts = [V(sb("h_S%d" % i, [128, 128], F32)) for i in range(2)]
                Sb = [V(sb("h_Sb%d" % i, [128, 128], BF16)) for i in range(2)]
                g_sq = V(sb("h_gsq", [128, 512], BF16))
                g_rs = V(sb("h_grs", [128, 512], F32))
                g_gs = V(sb("h_ggs", [128, 512], F32))
                g_tmp = V(sb("h_gtmp", [128, 512], F32))
                g2 = (V(sb("h_gsq2", [128, 512], BF16)), V(sb("h_grs2", [128, 512], F32)),
                      V(sb("h_ggs2", [128, 512], F32)), V(sb("h_gtmp2", [128, 512], F32)))
                for ti in range(NT):
                    tl = slice(ti * 128, (ti + 1) * 128)
                    p = self.ps(0, 4)
                    o.mm(p[:, 0:128], kmT[:, tl], qmT[:, tl])
                    o.tt(att[:, ti, :], p[:, 0:128], C["m_up32"], ALU.mult)
                    pt = self.ps(0, 4).bitcast(BF16)
                    o.tr(pt[:, 0:128], khT[:, tl], C["ident_b"])
                    for c in range(4):
                        o.act(khm[:, ti, c, :], pt[:, 0:128], AF.Copy, scale=C["m_chunk32"][:, c:c + 1])
                o.memset(Sts[0], 0.0)
                o.memset(Sb[0], 0.0)
                cur = 0
                pOs = [self.pb[4], self.pb[5]]
                pSs = [self.pb[6], self.pb[7]]
                def emit_pS(ti):
                    for c in range(4):
                        o.mm(pSs[ti % 2][:, c * 128:(c + 1) * 128], khm[:, ti, c, :], vsb[:, ti, :])

                emit_pS(0)
                for ti in range(NT):
                    tl = slice(ti * 128, (ti + 1) * 128)
                    pO, pS = pOs[ti % 2], pSs[ti % 2]
                    if ti + 1 < NT:
                        emit_pS(ti + 1)
                    o.mm(pO[:, 0:128], vsb[:, ti, :], att[:, ti, :], start=True, stop=False)
                    for c in range(4):
                        cs = slice(ti * 128 + c * 32, ti * 128 + (c + 1) * 32)
                        o.mm(pO[:, c * 32:(c + 1) * 32], Sb[cur], q0T[:, cs], start=False, stop=(c == 3), inc=True)
                        o.stt(Sts[1 - cur], Sts[cur], eb[:, ti * 4 + c:ti * 4 + c + 1], pS[:, c * 128:(c + 1) * 128], ALU.mult, ALU.add)
                        cur = 1 - cur
                        o.copy(Sb[cur], Sts[cur], eng="act")
                    o.copy(oT[:, tl], pO[:, 0:128], eng="act")
                self.gated_norm_out((g_sq, g_rs, g_gs, g_tmp) + g2, oT, wg, self._hng[:, l:l + 1], AF.Sigmoid,
                                    self.yT[3][:, h, :])


class KernFull0(KernMix):
    pass


def build_debug(stages, nseq=1):
    nc = bass.Bass("TRN2", target_bir_lowering=False)
    stack = ExitStack()
    with stack:
        k = KERN_CLS(nc, stack, nseq=nseq)
        o = k.o
        dump = V(k.fw.dram("dump", [4, 128, 4, S], BF16, kind="ExternalOutput"))
        l = stages.get("layer", 0)
        k.load_xT(k.x_in[0])
        for m, name in enumerate(("mla", "gdn", "pool", "hgrn")):
            if name in stages["mixers"]:
                getattr(k, name + "_mixer")(l, 0) if name == "mla" else getattr(k, name + "_mixer")(l)
                o.dma(dump[m], k.yT[m])
        k.fw.finish()
        print("instructions:", k.fw.ninst, "sems:", k.fw.nsem, "sbuf left:", nc.sbuf_bytes_remaining)
    return nc


class KernMLA(KernMix):
    def rope_tables(self, sc, s):
        o = self.o
        C = self.C
        cosT = V(sc.sbuf("rp_cos", [64, S], BF16))
        sinT = V(sc.sbuf("rp_sin", [64, S], BF16))
        with self.scope() as s2:
            posi = V(s2.sbuf("rp_pi", [64, S], I32))
            ang = V(s2.sbuf("rp_ang", [64, S], F32))
            t = V(s2.sbuf("rp_t", [64, S], F32))
            ki = V(s2.sbuf("rp_ki", [64, S], I32))
            ang2 = V(s2.sbuf("rp_ang2", [64, S], F32))
            o.dma(posi, self.pos[s:s + 1, :].bcast([64, S]))
            o.copy(ang, posi)
            o.ts(ang, ang, C["ropep"][0:64, 0:1], None, ALU.mult)
            for shift, dst, scale in ((0.0, sinT, C["ropep"][0:64, 1:2]), (math.pi / 2, cosT, None)):
                o.ts(t, ang, shift, 1.0 / (2 * math.pi), ALU.add, ALU.mult)
                o.copy(ki, t)
                o.copy(t, ki)
                o.stt(t, t, -2 * math.pi, ang, ALU.mult, ALU.add)
                if shift != 0.0:
                    o.ts(t, t, shift, None, ALU.add)
                o.ts(ang2, t, math.pi, 2 * math.pi, ALU.is_gt, ALU.mult)
                o.tt(t, t, ang2, ALU.subtract)
                o.ts(ang2, t, -math.pi, 2 * math.pi, ALU.is_lt, ALU.mult)
                o.tt(t, t, ang2, ALU.add)
                o.ts(t, t, 3.141592, -3.141592, ALU.min, ALU.max)
                o.act(dst, t, AF.Sin, scale=scale)
        return cosT, sinT

    def mla_mixer(self, l, s):
        o = self.o
        C = self.C
        with self.scope() as sc:
            sb = sc.sbuf
            cosT, sinT = self.rope_tables(sc, s)
            wcq = V(sb("a_wcq", [128, 8, 256], BF16))
            wckv = V(sb("a_wckv", [128, 8, 128], BF16))
            wkrA = V(sb("a_wkrA", [128, 8, 64], BF16))
            wkrB = V(sb("a_wkrB", [128, 8, 64], BF16))
            wuq = V(sb("a_wuq", [128, 2, 768], BF16))
            wuqB = V(sb("a_wuqB", [128, 2, 4, 64], BF16))
            wukv = V(sb("a_wukv", [128, 1024], BF16))
            gq = V(sb("a_gq", [128, 2], F32))
            gkv = V(sb("a_gkv", [128, 1], F32))
            self.wload(wcq, self.w_in_cols(l, OFF["cq"], 256))
            self.wload(wckv, self.w_in_cols(l, OFF["ckv"], 128))
            self.wload(wkrA, self.w_in_cols(l, OFF["kr"], 64))
            self.wload(wkrB[:, :, 0:32], self.w_in_cols(l, OFF["kr"] + 32, 32))
            self.wload(wkrB[:, :, 32:64], self.w_in_cols(l, OFF["kr"], 32))
            uq = self.W["mla_w_uq"][l].rearrange("(c p) n -> p c n", p=128)
            self.wload(wuq, uq)
            for h in range(4):
                self.wload(wuqB[:, :, h, 0:32], uq[:, :, h * 192 + 160:h * 192 + 192])
                self.wload(wuqB[:, :, h, 32:64], uq[:, :, h * 192 + 128:h * 192 + 160])
            self.wload(wukv, self.W["mla_w_ukv"][l])
            o.dma(gq, self.W["mla_q_norm"][l].rearrange("(c p) -> p c", p=128), slow=True)
            o.dma(gkv, self.W["mla_kv_norm"][l].rearrange("(p o) -> p o", o=1), slow=True)
            cqn = V(sb("a_cqn", [128, 2, S], BF16))
            ckvn = V(sb("a_ckvn", [128, S], BF16))
            krT = V(sb("a_krT", [64, S], BF16))
            cqf = V(sb("a_cqf", [128, 2, 512], F32))
            sq = V(sb("a_sq", [128, 2, 512], BF16))
            rq = V(sb("a_rq", [128, 512], F32))
            t1 = V(sb("a_t1", [64, 512], F32))
            t2 = V(sb("a_t2", [64, 512], F32))

            def rope_combine(dst, psA, psB, blk):
                o.tt(t1, psA, cosT[:, blk], ALU.mult)
                o.tt(t2, psB, sinT[:, blk], ALU.mult)
                o.tt(dst, t1, t2, ALU.add)

            for tb in range(4):
                blk = slice(tb * 512, (tb + 1) * 512)
                for c in range(2):
                    ps = self.ps()
                    for k in range(8):
                        o.mm(ps, wcq[:, k, c * 128:(c + 1) * 128], self.xT[:, k, blk], start=(k == 0), stop=(k == 7))
                    o.copy(cqf[:, c, :], ps, eng="act")
                    o.act(sq[:, c, :], ps, AF.Square)
                pss = self.ps()
                for c in range(2):
                    o.mm(pss, C["ones_b"], sq[:, c, :], start=(c == 0), stop=(c == 1))
                o.act(rq, pss, AF.Sqrt, bias=RMS_EPS, scale=1.0 / 256)
                o.recip(rq, rq)
                for c in range(2):
                    o.stt(cqn[:, c, blk], cqf[:, c, :], gq[:, c:c + 1], rq, ALU.mult, ALU.mult)
                ps = self.ps()
                for k in range(8):
                    o.mm(ps, wckv[:, k, :], self.xT[:, k, blk], start=(k == 0), stop=(k == 7))
                o.copy(cqf[:, 0, :], ps, eng="act")
                o.act(sq[:, 0, :], ps, AF.Square)
                pss = self.ps()
                o.mm(pss, C["ones_b"], sq[:, 0, :])
                o.act(rq, pss, AF.Sqrt, bias=RMS_EPS, scale=1.0 / 128)
                o.recip(rq, rq)
                o.stt(ckvn[:, blk], cqf[:, 0, :], gkv[:, 0:1], rq, ALU.mult, ALU.mult)
                psA = self.ps()
                psB = self.ps()
                for k in range(8):
                    o.mm(psA[0:64, :], wkrA[:, k, :], self.xT[:, k, blk], start=(k == 0), stop=(k == 7))
                for k in range(8):
                    o.mm(psB[0:64, :], wkrB[:, k, :], self.xT[:, k, blk], start=(k == 0), stop=(k == 7))
                rope_combine(krT[:, blk], psA[0:64, :], psB[0:64, :], blk)

            qnT = V(sb("a_qnT", [128, S], BF16))
            qrT = V(sb("a_qrT", [64, S], BF16))
            knT = V(sb("a_knT", [128, S], BF16))
            vsb = V(sb("a_v", [128, NT, 128], BF16))
            pts = [V(sb("a_pt%d" % i, [128, 512], BF16)) for i in range(3)]
            rd = V(sb("a_rd", [128, 512], F32))
            pO = self.pb[5]
            pD = self.pb[6]
            for h in range(4):
                for tb in range(4):
                    blk = slice(tb * 512, (tb + 1) * 512)
                    ps = self.ps()
                    for c in range(2):
                        o.mm(ps, wuq[:, c, h * 192:h * 192 + 128], cqn[:, c, blk], start=(c == 0), stop=(c == 1))
                    o.copy(qnT[:, blk], ps, eng="act")
                    psA = self.ps()
                    psB = self.ps()
                    for c in range(2):
                        o.mm(psA[0:64, :], wuq[:, c, h * 192 + 128:h * 192 + 192], cqn[:, c, blk], start=(c == 0), stop=(c == 1))
                    for c in range(2):
                        o.mm(psB[0:64, :], wuqB[:, c, h, :], cqn[:, c, blk], start=(c == 0), stop=(c == 1))
                    rope_combine(qrT[:, blk], psA[0:64, :], psB[0:64, :], blk)
                    ps = self.ps()
                    o.mm(ps, wukv[:, h * 256:h * 256 + 128], ckvn[:, blk])
                    o.copy(knT[:, blk], ps, eng="act")
                for ti in range(NT):
                    ps = self.ps()
                    o.mm(ps[:, 0:128], ckvn[:, ti * 128:(ti + 1) * 128], wukv[:, h * 256 + 128:h * 256 + 256])
                    o.copy(vsb[:, ti, :], ps[:, 0:128], eng="act")
                it = 0
                for a in range(4):
                    nj = 4 * a + 4

                    def geom(j, a=a):
                        qlo = max(j * 128, a * 512)
                        qhi = (a + 1) * 512
                        return qlo, qhi, qhi - qlo, qlo - a * 512, slice(j * 128, (j + 1) * 128)

                    def scores(j):
                        qlo, qhi, wd, off, ks = geom(j)
                        ps = self.ps()
                        o.mm(ps[:, 0:wd], knT[:, ks], qnT[:, qlo:qhi], start=True, stop=False)
                        o.mm(ps[:, 0:wd], krT[:, ks], qrT[:, qlo:qhi], start=False, stop=True)
                        return ps

                    ps_next = scores(0)
                    for j in range(nj):
                        qlo, qhi, wd, off, ks = geom(j)
                        ps = ps_next
                        if j + 1 < nj:
                            ps_next = scores(j + 1)
                        pt = pts[it % 3]
                        it += 1
                        o.act(pt[:, 0:wd], ps[:, 0:wd], AF.Exp, scale=ATT_SCALE)
                        if j >= 4 * a:
                            o.tt(pt[:, 0:128], pt[:, 0:128], C["causal_b"], ALU.mult)
                        o.mm(pO[:, off:512], vsb[:, j, :], pt[:, 0:wd], start=(j == 0), stop=(j == nj - 1))
                        o.mm(pD[:, off:512], C["ones_b"], pt[:, 0:wd], start=(j == 0), stop=(j == nj - 1))
                    o.recip(rd, pD)
                    o.tt(self.yT[0][:, h, a * 512:(a + 1) * 512], pO, rd, ALU.mult)


class KernGDN(KernMLA):
    def gdn_mixer(self, l):
        self.fw.pe_safe = True
        try:
            self._gdn_mixer(l)
        finally:
            self.fw.pe_safe = False

    def _gdn_mixer(self, l):
        o = self.o
        C = self.C
        with self.scope() as sc:
            sb = sc.sbuf
            scal = V(sb("g_scal", [68, S], F32))
            tok = V(sb("g_tok", [128, NT, 16], F32))
            egl = V(sb("g_egl", [68, 32], F32))
            bge = V(sb("g_bge", [128, NT, 4], F32))
            with self.scope() as s2:
                w3 = V(s2.sbuf("g_w3", [128, 8, 68], BF16))
                par = V(s2.sbuf("g_par", [68, 4], F32))
                tmp = V(s2.sbuf("g_tmp", [68, 512], F32))
                edT = V(s2.sbuf("g_edT", [4, S], F32))
                o.memset(w3, 0.0)
                o.memset(par, 0.0)
                self.wload(w3[:, :, 0:4], self.w_in_cols(l, OFF["ga"], 4))
                self.wload(w3[:, :, 32:36], self.w_in_cols(l, OFF["gb"], 4))
                self.wload(w3[:, :, 64:68], self.w_in_cols(l, OFF["ga"], 4))
                for base in (0, 64):
                    o.dma(par[base:base + 4, 0:1], self.W["gdn_dt_bias"][l].rearrange("(p o) -> p o", o=1), slow=True)
                    o.dma(par[base:base + 4, 1:2], self.W["gdn_a_log"][l].rearrange("(p o) -> p o", o=1), slow=True)
                o.act(par[:, 2:3], par[:, 1:2], AF.Exp)
                o.ts(par[:, 2:3], par[:, 2:3], -1.0, None, ALU.mult)
                for tb in range(4):
                    blk = slice(tb * 512, (tb + 1) * 512)
                    ps = self.ps()
                    for k in range(8):
                        o.mm(ps[0:68, :], w3[:, k, :], self.xT[:, k, blk], start=(k == 0), stop=(k == 7))
                    o.act(tmp, ps[0:68, :], AF.Exp, bias=par[:, 0:1])
                    o.act(tmp, tmp, AF.Ln, bias=1.0)
                    o.ts(tmp, tmp, par[:, 2:3], None, ALU.mult)
                    o.copy(scal[:, blk], tmp)
                    o.act(tmp[32:36, :], ps[32:36, :], AF.Sigmoid)
                    o.copy(scal[32:36, blk], tmp[32:36, :])
                for base in (0, 64):
                    for tb in range(4):
                        blk = slice(tb * 512, (tb + 1) * 512)
                        o.scan(tmp[base:base + 4, :], C["scan64x"][base:base + 4, blk], scal[base:base + 4, blk], 0.0, ALU.mult, ALU.add)
                        o.copy(scal[base:base + 4, blk], tmp[base:base + 4, :])
                gc3 = scal[0:4, :].rearrange("p (c t) -> p c t", t=64)
                ed3 = edT.rearrange("p (c t) -> p c t", t=64)
                o.tt(ed3, gc3[:, :, 63:64].bcast([4, 32, 64]), gc3, ALU.subtract)
                o.act(edT, edT, AF.Exp)
                o.act(scal[64:68, :], scal[64:68, :], AF.Exp)
                o.copy(egl[64:68, :], scal[64:68, :].rearrange("p (c t) -> p c t", t=64)[:, :, 63])
                for ti in range(NT):
                    tl = slice(ti * 128, (ti + 1) * 128)
                    ps = self.ps()
                    o.tr(ps[:, 0:4], scal[0:4, tl], C["ident"][0:4, 0:4])
                    o.tr(ps[:, 4:8], scal[32:36, tl], C["ident"][32:36, 32:36])
                    o.tr(ps[:, 8:12], scal[64:68, tl], C["ident"][64:68, 64:68])
                    o.tr(ps[:, 12:16], edT[0:4, tl], C["ident"][0:4, 0:4])
                    o.copy(tok[:, ti, :], ps[:, 0:16])
                o.tt(bge, tok[:, :, 4:8], tok[:, :, 8:12], ALU.mult)
            ng = V(sb("g_ng", [128, 1], F32))
            o.dma(ng, self.W["gdn_norm"][l].rearrange("(p o) -> p o", o=1), slow=True)
            for h in range(4):
                self.gdn_head(l, h, scal, tok, egl, bge, ng)

    def gdn_head(self, l, h, scal, tok, egl, bge, ng):
        MARKS.append(("gdn h%d start" % h, self.nc.get_next_instruction_name()))
        o = self.o
        C = self.C
        sel = C["sel4x"]
        hs = slice(h * 128, (h + 1) * 128)
        with self.scope() as sc:
            sb = sc.sbuf
            wz = V(sb("g_wz", [128, 8, 128], BF16))
            self.wload(wz, self.w_in_cols(l, OFF["gz"] + h * 128, 128))
            qT = V(sb("g_qT", [128, S], BF16))
            kT = V(sb("g_kT", [128, S], BF16))
            vT = V(sb("g_vT", [128, S], BF16))
            with self.scope() as s1:
                s1b = s1.sbuf
                wts = []
                for i, nm in enumerate(("gq", "gk", "gv")):
                    w = V(s1b("g_w" + nm, [128, 8, 128], BF16))
                    self.wload(w, self.w_in_cols(l, OFF[nm] + h * 128, 128))
                    wts.append(w)
                cw = V(s1b("g_cw", [128, 3, 4], F32))
                for i in range(3):
                    o.dma(cw[:, i, :], self.W["gdn_conv"][l][:, i * 512 + h * 128:i * 512 + (h + 1) * 128].rearrange("t c -> c t"), slow=True)
                xin = V(s1b("g_xin", [128, S], F32))
                acc = V(s1b("g_acc", [128, S], F32))
                sq = V(s1b("g_sq", [128, 512], BF16))
                rs = V(s1b("g_rs", [128, 512], F32))
                for i, dst in enumerate((qT, kT, vT)):
                    self.fw.pe_safe = False
                    self.proj_fm(wts[i], 128, lambda ps, tb: o.copy(xin[:, tb * 512:(tb + 1) * 512], ps, eng="act"))
                    self.fw.pe_safe = True
                    o.ts(acc, xin, cw[:, i, 3:4], None, ALU.mult)
                    for d in range(1, 4):
                        o.stt(acc[:, d:S], xin[:, 0:S - d], cw[:, i, 3 - d:4 - d], acc[:, d:S], ALU.mult, ALU.add)
                    o.act(acc, acc, AF.Silu)
                    if i == 2:
                        o.copy(vT, acc)
                        continue
                    for tb in range(4):
                        blk = slice(tb * 512, (tb + 1) * 512)
                        o.act(sq, acc[:, blk], AF.Square)
                        p = self.ps()
                        o.mm(p, C["ones_b"], sq)
                        o.act(rs, p, AF.Sqrt, bias=RMS_EPS, scale=1.0)
                        o.recip(rs, rs)
                        if i == 0:
                            o.stt(dst[:, blk], acc[:, blk], 128 ** -0.5, rs, ALU.mult, ALU.mult)
                        else:
                            o.tt(dst[:, blk], acc[:, blk], rs, ALU.mult)
            MARKS.append(("gdn h%d front-done" % h, self.nc.get_next_instruction_name()))
            u = V(sb("g_u", [128, NT, 128], F32))
            wT = V(sb("g_wT", [128, S], BF16))
            qgT = V(sb("g_qgT", [128, S], BF16))
            kdec = V(sb("g_kdec", [128, NT, 128], BF16))
            qkm = V(sb("g_qkm", [128, NT, 128], BF16))
            dtab = V(sb("g_dtab", [128, 32], F32))
            oT = V(sb("g_oT", [128, S], F32))
            LT = V(sb("g_LT", [128, 128], F32))
            dd = V(sb("g_dd", [128, 128], F32))
            t1 = V(sb("g_t1", [128, 128], F32))
            G = 4
            sets = []
            for i in range(G):
                sets.append(dict(
                    PT=[V(sb("g_PTa%d" % i, [128, 128], F32)), V(sb("g_PTb%d" % i, [128, 128], F32))],
                    P=[V(sb("g_Pa%d" % i, [128, 128], F32)), V(sb("g_Pb%d" % i, [128, 128], F32))],
                    Y=V(sb("g_Y%d" % i, [128, 256], F32))))
            p = self.ps()
            o.mm(p[:, 0:32], sel[64:68, hs], egl[64:68, :])
            o.copy(dtab, p[:, 0:32])
            for base in range(0, NT, G):
                for i in range(G):
                    ti = base + i
                    T = sets[i]
                    AT, Y = T["PT"][0], T["Y"]
                    tl = slice(ti * 128, (ti + 1) * 128)
                    gcc = tok[:, ti, h:h + 1]
                    pG = self.ps()
                    o.mm(pG[:, 0:128], sel[0:4, hs], scal[0:4, tl])
                    o.mm(pG[:, 128:256], sel[32:36, hs], scal[32:36, tl])
                    o.mm(pG[:, 256:384], sel[64:68, hs], scal[64:68, tl])
                    o.ts(dd, pG[:, 0:128], gcc, 0.0, ALU.subtract, ALU.min)
                    o.act(dd, dd, AF.Exp)
                    pK = self.ps()
                    o.mm(pK[:, 0:128], kT[:, tl], kT[:, tl])
                    o.mm(pK[:, 128:256], kT[:, tl], qT[:, tl])
                    pt = self.ps().bitcast(BF16)
                    o.tr(pt[:, 0:128], kT[:, tl], C["ident_b"])
                    o.tr(pt[:, 128:256], vT[:, tl], C["ident_b"])
                    o.tt(qgT[:, tl], qT[:, tl], pG[:, 256:384], ALU.mult)
                    o.ts(Y[:, 0:128], pt[:, 128:256], tok[:, ti, 4 + h:5 + h], None, ALU.mult)
                    o.ts(Y[:, 128:256], pt[:, 0:128], bge[:, ti, h:h + 1], None, ALU.mult)
                    o.ts(kdec[:, ti, :], pt[:, 0:128], tok[:, ti, 12 + h:13 + h], None, ALU.mult)
                    o.tt(LT, dd, C["m_up64"], ALU.mult)
                    o.tt(t1, dd, C["m_sup64"], ALU.mult)
                    o.tt(qkm[:, ti, :], pK[:, 128:256], LT, ALU.mult)
                    o.tt(t1, pK[:, 0:128], t1, ALU.mult)
                    o.tt(AT, t1, pG[:, 128:256], ALU.mult)
                    pa = self.ps()
                    o.tr(pa[:, 0:128], AT, C["ident"])
                    pa2 = self.ps()
                    o.mm(pa2[:, 0:256], AT, Y)
                    o.copy(T["P"][0], pa[:, 0:128], eng="act")
                    o.tt(Y, Y, pa2[:, 0:256], ALU.subtract)
                cur = 0
                for k in range(5):
                    nxt = 1 - cur
                    pps = [self.pb[i] for i in range(G)]
                    pqs = [self.pb[4 + i] for i in range(G)]
                    for i in range(G):
                        T = sets[i]
                        o.mm(pps[i][:, 0:128], T["P"][cur], T["PT"][cur])
                        if k < 4:
                            o.mm(pps[i][:, 128:256], T["PT"][cur], T["P"][cur])
                    for i in range(G):
                        T = sets[i]
                        o.copy(T["PT"][nxt], pps[i][:, 0:128], eng="act")
                        if k < 4:
                            o.copy(T["P"][nxt], pps[i][:, 128:256], eng="act")
                    for i in range(G):
                        T = sets[i]
                        o.mm(pqs[i][:, 0:256], T["PT"][nxt], T["Y"])
                    for i in range(G):
                        T = sets[i]
                        o.tt(T["Y"], T["Y"], pqs[i][:, 0:256], ALU.add)
                    cur = nxt
                for i in range(G):
                    ti = base + i
                    T = sets[i]
                    tl = slice(ti * 128, (ti + 1) * 128)
                    wtok = T["P"][0]
                    o.copy(u[:, ti, :], T["Y"][:, 0:128], eng="act")
                    o.copy(wtok, T["Y"][:, 128:256], eng="act")
                    pw = self.ps()
                    o.tr(pw[:, 0:128], wtok, C["ident"])
                    o.copy(wT[:, tl], pw[:, 0:128], eng="act")
            MARKS.append(("gdn h%d prep-done" % h, self.nc.get_next_instruction_name()))
            Sts = [V(sb("g_S%d" % i, [128, 128], F32)) for i in range(2)]
            Sbs = [V(sb("g_Sb%d" % i, [128, 128], BF16)) for i in range(2)]
            vn = [V(sb("g_vn%d" % i, [128, 128], BF16)) for i in range(2)]
            o.memset(Sts[0], 0.0)
            o.memset(Sbs[0], 0.0)
            pO = self.pb[5]
            pSs = [self.pb[6], self.pb[6]]
            pV = self.pb[7]
            cur = 0
            for n in range(32):
                ti, half = n // 2, n % 2
                rows = slice(half * 64, half * 64 + 64)
                cols = slice(n * 64, n * 64 + 64)
                v = vn[ti % 2]
                pS = pSs[n % 2]
                St, Sb = Sts[cur], Sbs[cur]
                o.mm(pV[rows, 0:128], wT[:, cols], Sb)
                o.tt(v[rows, :], u[rows, ti, :], pV[rows, 0:128], ALU.subtract)
                oc = slice(half * 64, half * 64 + 64)
                o.mm(pO[:, oc], Sb, qgT[:, cols], start=True, stop=False, inc=True)
                o.mm(pO[:, oc], v[rows, :], qkm[rows, ti, half * 64:half * 64 + 64], start=False, stop=True)
                o.mm(pS[:, 0:128], kdec[rows, ti, :], v[rows, :])
                o.stt(Sbs[1 - cur], St, dtab[:, n:n + 1], pS[:, 0:128], ALU.mult, ALU.add)
                o.stt(Sts[1 - cur], St, dtab[:, n:n + 1], pS[:, 0:128], ALU.mult, ALU.add)
                cur = 1 - cur
                if half == 1:
                    o.copy(oT[:, ti * 128:(ti + 1) * 128], pO[:, 0:128], eng="act")
            MARKS.append(("gdn h%d scan-done" % h, self.nc.get_next_instruction_name()))
            g_sq = V(sb("g_gsq", [128, 512], BF16))
            g_rs = V(sb("g_grs", [128, 512], F32))
            g_gs = V(sb("g_ggs", [128, 512], F32))
            g_tmp = V(sb("g_gtmp", [128, 512], F32))
            self.fw.pe_safe = False
            g2 = (V(sb("g_gsq2", [128, 512], BF16)), V(sb("g_grs2", [128, 512], F32)),
                  V(sb("g_ggs2", [128, 512], F32)), V(sb("g_gtmp2", [128, 512], F32)))
            self.gated_norm_out((g_sq, g_rs, g_gs, g_tmp) + g2, oT, wz, ng[:, 0:1], AF.Silu, self.yT[1][:, h, :])
            self.fw.pe_safe = True


class KernFull(KernGDN):
    def xacc(self, ti):
        v = self.yT[ti // 4].rearrange("p a s -> p (a s)").bitcast(F32)
        return v[:, (ti % 4) * 1024:(ti % 4 + 1) * 1024]

    def bc_row(self, sc, name, src_row):
        t = V(sc.sbuf(name, [128, D], F32))
        self.o.dma(t, src_row.rearrange("(o d) -> o d", o=1).bcast([128, D]))
        return t

    def ln_tile(self, r, gbc, bbc, st, junk):
        o = self.o
        o.memset(st[:, 0:2], 0.0)
        o.act(junk, r, AF.Copy, accum=st[:, 0:1])
        o.act(junk, r, AF.Square, accum=st[:, 1:2])
        o.ts(st[:, 2:4], st[:, 0:2], 1.0 / D, None, ALU.mult)
        o.tt(st[:, 4:5], st[:, 2:3], st[:, 2:3], ALU.mult)
        o.tt(st[:, 5:6], st[:, 3:4], st[:, 4:5], ALU.subtract)
        o.act(st[:, 6:7], st[:, 5:6], AF.Sqrt, bias=LN_EPS)
        o.recip(st[:, 7:8], st[:, 6:7])
        o.ts(r, r, st[:, 2:3], st[:, 7:8], ALU.subtract, ALU.mult)
        o.tt(r, r, gbc, ALU.mult)
        o.tt(r, r, bbc, ALU.add)

    def ln_tiles(self, items, gbc, bbc):
        o = self.o
        for r, st, junk in items:
            o.memset(st[:, 0:2], 0.0)
        for r, st, junk in items:
            o.act(junk, r, AF.Copy, accum=st[:, 0:1])
        for r, st, junk in items:
            o.act(junk, r, AF.Square, accum=st[:, 1:2])
        for r, st, junk in items:
            o.ts(st[:, 2:4], st[:, 0:2], 1.0 / D, None, ALU.mult)
        for r, st, junk in items:
            o.tt(st[:, 4:5], st[:, 2:3], st[:, 2:3], ALU.mult)
        for r, st, junk in items:
            o.tt(st[:, 5:6], st[:, 3:4], st[:, 4:5], ALU.subtract)
        for r, st, junk in items:
            o.act(st[:, 6:7], st[:, 5:6], AF.Sqrt, bias=LN_EPS)
        for r, st, junk in items:
            o.recip(st[:, 7:8], st[:, 6:7])
        for r, st, junk in items:
            o.ts(r, r, st[:, 2:3], st[:, 7:8], ALU.subtract, ALU.mult)
        for r, st, junk in items:
            o.tt(r, r, gbc, ALU.mult)
        for r, st, junk in items:
            o.tt(r, r, bbc, ALU.add)

    def merge_outproj(self, l, s, moe_next):
        o = self.o
        C = self.C
        with self.scope() as sc:
            sb = sc.sbuf
            mT = V(sb("m_mT", [128, 8, S], BF16))
            with self.scope() as s1:
                wgo = [V(s1.sbuf("m_wgo%d" % i, [128, 4, 8, 128], BF16)) for i in range(2)]
                wbo = [V(s1.sbuf("m_wbo%d" % i, [128, 4, 4, 128], BF16)) for i in range(2)]
                acc = [V(s1.sbuf("m_acc%d" % i, [128, 512], F32)) for i in range(4)]
                gs = [V(s1.sbuf("m_gs%d" % i, [128, 512], F32)) for i in range(2)]
                tt_ = V(s1.sbuf("m_t", [128, 512], F32))
                it = 0
                for oc in range(8):
                    wg, wb = wgo[oc % 2], wbo[oc % 2]
                    for m in range(4):
                        self.wload(wg[:, m], self.w_in_cols(l, OFF["gate"] + m * 1024 + oc * 128, 128))
                        self.wload(wb[:, m], self.W["w_branch"][l, m][:, oc * 128:(oc + 1) * 128].rearrange("(c p) n -> p c n", p=128))
                    for m in range(4):
                        for tb in range(4):
                            blk = slice(tb * 512, (tb + 1) * 512)
                            pg = self.ps()
                            for k in range(8):
                                o.mm(pg, wg[:, m, k, :], self.xT[:, k, blk], start=(k == 0), stop=(k == 7))
                            g = gs[it % 2]
                            it += 1
                            o.act(g, pg, AF.Sigmoid)
                            pbr = self.ps()
                            for c in range(4):
                                o.mm(pbr, wb[:, m, c, :], self.yT[m][:, c, blk], start=(c == 0), stop=(c == 3))
                            if m == 0:
                                o.tt(acc[tb], g, pbr, ALU.mult)
                            else:
                                o.tt(tt_, g, pbr, ALU.mult)
                                o.tt(mT[:, oc, blk] if m == 3 else acc[tb], acc[tb], tt_, ALU.add)
            wout = V(sb("m_wout", [128, 8, D], BF16))
            self.wload(wout, self.W["w_out"][l].rearrange("(k p) n -> p k n", p=128))
            gbc = self.bc_row(sc, "m_gbc", self.W["ln_mix_g"][l])
            bbc = self.bc_row(sc, "m_bbc", self.W["ln_mix_b"][l])
            xr = [V(sb("m_xr%d" % i, [128, D], F32)) for i in range(2)]
            rr = [V(sb("m_rr%d" % i, [128, D], F32)) for i in range(2)]
            xb = [V(sb("m_xb%d" % i, [128, D], BF16)) for i in range(2)]
            junk2 = [V(sb("m_junk%d" % i, [128, D], BF16)) for i in range(2)]
            st2 = [V(sb("m_st%d" % i, [128, 8], F32)) for i in range(2)]
            src = self.x_in[s] if l == 0 else self.xres[s]
            for tp in range(0, NT, 2):
                for k_ in range(2):
                    ti = tp + k_
                    tl = slice(ti * 128, (ti + 1) * 128)
                    x_, r_ = xr[k_], rr[k_]
                    o.dma(x_, src[tl, :])
                    for half in range(2):
                        hs = slice(half * 512, (half + 1) * 512)
                        ps = self.ps()
                        for k in range(8):
                            o.mm(ps, mT[:, k, tl], wout[:, k, hs], start=(k == 0), stop=(k == 7))
                        o.stt(r_[:, hs], x_[:, hs], ALPHA, ps, ALU.mult, ALU.add)
                self.ln_tiles([(rr[0], st2[0], junk2[0]), (rr[1], st2[1], junk2[1])], gbc, bbc)
                for k_ in range(2):
                    ti = tp + k_
                    r_, b_ = rr[k_], xb[k_]
                    o.ts(self.xacc(ti), r_, ALPHA, None, ALU.mult)
                    o.copy(b_, r_, eng="act")
                    self.transpose_into_xT(b_, ti)

    def router(self):
        o = self.o
        with self.scope() as sc:
            wrb = [V(sc.sbuf("r_wrb%d" % i, [128, D], F32)) for i in range(2)]
            junk = V(sc.sbuf("r_junk", [128, D], F32))
            lgs = V(sc.sbuf("r_lgs", [128, NT, 8], F32))
            rt = V(sc.sbuf("r_rt", [128, 64], F32))
            o.memset(lgs, 0.0)
            for e in range(NEXP):
                w = wrb[e % 2]
                o.dma(w, self.W["moe_router_T"][0, e:e + 1, :].bcast([128, D]))
                for ti in range(NT):
                    o.stt(junk, self.xacc(ti), 1.0 / ALPHA, w, ALU.mult, ALU.mult, accum=lgs[:, ti, e:e + 1])
            for ti in range(NT):
                lg = lgs[:, ti, :]
                m1, m2, nm1, den = rt[:, 8:9], rt[:, 9:10], rt[:, 10:11], rt[:, 11:12]
                eq, l2, sel, ex = rt[:, 16:24], rt[:, 24:32], rt[:, 32:40], rt[:, 40:48]
                o.fw.emit("dve", lambda e_: e_.reduce_max(m1.ap, lg.ap, AX.X), reads=[lgs.tile], writes=[rt.tile])
                o.ts(eq, lg, m1, None, ALU.is_equal)
                o.stt(l2, eq, -1e30, lg, ALU.mult, ALU.add)
                o.fw.emit("dve", lambda e_: e_.reduce_max(m2.ap, l2.ap, AX.X), reads=[rt.tile], writes=[rt.tile])
                o.ts(sel, lg, m2, None, ALU.is_ge)
                o.ts(nm1, m1, -1.0, None, ALU.mult)
                o.act(ex, lg, AF.Exp, bias=nm1)
                o.tt(ex, ex, sel, ALU.mult)
                o.fw.emit("dve", lambda e_: e_.reduce_sum(den.ap, ex.ap, AX.X), reads=[rt.tile], writes=[rt.tile])
                o.recip(den, den)
                o.ts(self.gates[:, ti, :], ex, den, None, ALU.mult)

    def ffn_stage(self, l, s, last):
        o = self.o
        moe = (l % 2 == 1)
        j = l // 2
        if moe:
            self.router()
        with self.scope() as sc:
            sb = sc.sbuf
            hT = V(sb("f_hT", [128, 4, S], BF16))
            wg = [V(sb("f_wg%d" % i, [128, 8, 512], BF16)) for i in range(2)]
            wu = [V(sb("f_wu%d" % i, [128, 8, 512], BF16)) for i in range(2)]
            wd = [V(sb("f_wd%d" % i, [128, 4, D], BF16)) for i in range(2)]
            hs_ = [V(sb("f_hs%d" % i, [128, 512], F32)) for i in range(2)]
            if moe:
                jobs = [(e, fb, 4) for e in range(NEXP) for fb in range(D_FFE // 512)]
            else:
                jobs = [(None, fb, 4) for fb in range(D_FF // 512)] + [(None, D_FF // 512, (D_FF % 512) // 128)]
            it = 0
            if DBG.get("max_jobs") is not None:
                jobs = jobs[:DBG["max_jobs"]]
            for ji, (e, fb, nch) in enumerate(jobs):
                g_, u_, d_ = wg[ji % 2], wu[ji % 2], wd[ji % 2]
                f0 = fb * 512
                nf = nch * 128
                if moe:
                    Wg, Wu, Wd = self.W["moe_w_gate"][j, e], self.W["moe_w_up"][j, e], self.W["moe_w_down"][j, e]
                else:
                    Wg, Wu, Wd = self.W["ffn_w_gate"][j], self.W["ffn_w_up"][j], self.W["ffn_w_down"][j]
                self.wload(g_[:, :, 0:nf], Wg[:, f0:f0 + nf].rearrange("(k p) n -> p k n", p=128))
                self.wload(u_[:, :, 0:nf], Wu[:, f0:f0 + nf].rearrange("(k p) n -> p k n", p=128))
                self.wload(d_[:, 0:nch, :], Wd[f0:f0 + nf, :].rearrange("(c p) n -> p c n", p=128))
                for tb in range(4):
                    blk = slice(tb * 512, (tb + 1) * 512)
                    for ch in range(nch):
                        pg = self.ps()
                        for k in range(8):
                            o.mm(pg, g_[:, k, ch * 128:(ch + 1) * 128], self.xT[:, k, blk], start=(k == 0), stop=(k == 7))
                        pu = self.ps()
                        for k in range(8):
                            o.mm(pu, u_[:, k, ch * 128:(ch + 1) * 128], self.xT[:, k, blk], start=(k == 0), stop=(k == 7))
                        h_ = hs_[it % 2]
                        it += 1
                        o.act(h_, pg, AF.Silu)
                        o.tt(hT[:, ch, blk], h_, pu, ALU.mult)
                for ti in range(NT):
                    tl = slice(ti * 128, (ti + 1) * 128)
                    xa = self.xacc(ti)
                    for half in range(2):
                        hs = slice(half * 512, (half + 1) * 512)
                        ps = self.ps()
                        for ch in range(nch):
                            o.mm(ps, hT[:, ch, tl], d_[:, ch, hs], start=(ch == 0), stop=(ch == nch - 1))
                        if moe:
                            o.stt(xa[:, hs], ps, self.gates[:, ti, e:e + 1], xa[:, hs], ALU.mult, ALU.add)
                        else:
                            o.tt(xa[:, hs], xa[:, hs], ps, ALU.add)
            gbc = self.bc_row(sc, "f_gbc", self.W["ln_ffn_g"][l])
            bbc = self.bc_row(sc, "f_bbc", self.W["ln_ffn_b"][l])
            junk2 = [V(sb("f_junk%d" % i, [128, D], BF16)) for i in range(2)]
            st2 = [V(sb("f_st%d" % i, [128, 8], F32)) for i in range(2)]
            xb = [V(sb("f_xb%d" % i, [128, D], BF16)) for i in range(2)]
            dst = self.out[s] if last else self.xres[s]
            for g in range(2):
                for i in range(4):
                    pair = (g * 8 + i, g * 8 + i + 4)
                    self.ln_tiles([(self.xacc(pair[0]), st2[0], junk2[0]), (self.xacc(pair[1]), st2[1], junk2[1])],
                                  gbc, bbc)
                    for k_, ti in enumerate(pair):
                        tl = slice(ti * 128, (ti + 1) * 128)
                        xa = self.xacc(ti)
                        o.dma(dst[tl, :], xa)
                        if not last:
                            b_ = xb[k_]
                            o.copy(b_, xa, eng="act")
                            self.transpose_into_xT(b_, ti)

    def forward(self, depth=DEPTH, dbg_stop=None):
        for s in range(self.nseq):
            self.load_xT(self.x_in[s])
            for l in range(depth):
                on = DBG.get("l1") if (l == 1 and DBG.get("l1") is not None) else ("mla", "gdn", "pool", "hgrn", "merge", "ffn")
                for st in ("mla", "gdn", "pool", "hgrn", "merge", "ffn"):
                    if st not in on:
                        continue
                    MARKS.append(("s%d l%d %s" % (s, l, st), self.nc.get_next_instruction_name()))
                    if st == "mla":
                        self.mla_mixer(l, s)
                    elif st == "gdn":
                        self.gdn_mixer(l)
                    elif st == "pool":
                        self.pool_mixer(l)
                    elif st == "hgrn":
                        self.hgrn_mixer(l)
                    elif st == "merge":
                        self.merge_outproj(l, s, moe_next=False)
                    else:
                        self.ffn_stage(l, s, last=(l == depth - 1))
        MARKS.append(("end", self.nc.get_next_instruction_name()))
        self.fw.finish()


def build_full(nseq=NSEQ, depth=DEPTH):
    nc = bass.Bass("TRN2", target_bir_lowering=False)
    stack = ExitStack()
    with stack:
        k = KernFull(nc, stack, nseq=nseq)
        k.forward(depth)
        print("instructions:", k.fw.ninst, k.fw.per, "sems:", k.fw.nsem, "sbuf left:", nc.sbuf_bytes_remaining)
    return nc


_CACHE = {}


def kernel(**inputs):
    n_cores = 8
    if "nc" not in _CACHE:
        _CACHE["nc"] = build_full()
    nc = _CACHE["nc"]
    consts = make_consts()
    x = np.ascontiguousarray(inputs["x"], dtype=np.float32)
    pos = np.ascontiguousarray(inputs["positions"], dtype=np.int32)
    in_maps = []
    warr = {name: weight_array(inputs, name) for name, _ in WEIGHT_SPECS}
    for c in range(n_cores):
        m = {"x": x[c * NSEQ:(c + 1) * NSEQ], "positions": pos[c * NSEQ:(c + 1) * NSEQ]}
        for name, shp in WEIGHT_SPECS:
            m[name] = warr[name]
        for k_, v_ in consts.items():
            m["c_" + k_] = v_
        in_maps.append(m)
    res = run_bass_kernel_spmd(nc, in_maps, core_ids=list(range(n_cores)))
    out = np.concatenate([np.asarray(r["out"]) for r in res.results], axis=0)
    return out.astype(np.float32)


KERN_CLS = KernFull
```
